# Optimizing a Trainium2 kernel written in Bass

```python
import jax, jax.numpy as jnp
from jax import lax
import numpy as np

D_MODEL = 4096
BATCH = 2
SEQ = 8192
DEPTH = 2

GRID_W = 64
CTX_LEN = 256
ATTN_HEADS = 16
ATTN_KV_HEADS = 4
ATTN_GROUP = ATTN_HEADS // ATTN_KV_HEADS
HEAD_DIM = 128
ATTN_SCALE = HEAD_DIM ** -0.5
WINDOW = 128
QBLK = 128
ROPE_THETA = 10000.0
ROPE_PAIRS = HEAD_DIM // 4
MLSTM_HEADS = 8
MLSTM_QK_DIM = 128
MLSTM_V_DIM = 256
MLSTM_CHUNK = 128
KCONV = 3
ATTN_WIDTH = ATTN_HEADS * HEAD_DIM
KV_WIDTH = ATTN_KV_HEADS * HEAD_DIM
MLSTM_QK_WIDTH = MLSTM_HEADS * MLSTM_QK_DIM
MLSTM_WIDTH = MLSTM_HEADS * MLSTM_V_DIM
N_GATE_COLS = 4 * MLSTM_HEADS
COL_SIZES = (ATTN_WIDTH, KV_WIDTH, KV_WIDTH, MLSTM_QK_WIDTH, MLSTM_QK_WIDTH, MLSTM_WIDTH, MLSTM_WIDTH, N_GATE_COLS, D_MODEL, D_MODEL)
D_IN = ATTN_WIDTH + 2 * KV_WIDTH + 2 * MLSTM_QK_WIDTH + 2 * MLSTM_WIDTH + N_GATE_COLS + 2 * D_MODEL
GATE_OFFSET = ATTN_WIDTH + 2 * KV_WIDTH + 2 * MLSTM_QK_WIDTH + 2 * MLSTM_WIDTH
N_EXPERTS = 16
N_GROUPS = 4
EXPERTS_PER_GROUP = N_EXPERTS // N_GROUPS
TOP_K = 2
D_FF_EXPERT = 1024
EPS = 1e-6
NEG = -1e30

kernel_name = 'hybrid_mlstm_swa_moe_dit_trunk'


def rms_norm(x, g):
    x32 = x.astype(jnp.float32)
    y = x32 * lax.rsqrt(jnp.mean(x32 * x32, axis=-1, keepdims=True) + EPS)
    return (y * g.astype(jnp.float32)).astype(x.dtype)


def modulate(x, g, shift, scale):
    return rms_norm(x, g) * (1.0 + scale) + shift


def adaln_params(cvec, w, b):
    return jnp.split(jax.nn.silu(cvec) @ w + b, 6, axis=-1)


def axial_angles(n_lat):
    rows = n_lat // GRID_W
    inv_freq = ROPE_THETA ** (-jnp.arange(ROPE_PAIRS, dtype=jnp.float32) / ROPE_PAIRS)
    row_pos = jnp.repeat(jnp.arange(rows, dtype=jnp.float32), GRID_W)
    col_pos = jnp.tile(jnp.arange(GRID_W, dtype=jnp.float32), rows)
    return row_pos[:, None] * inv_freq, col_pos[:, None] * inv_freq


def rope_half(x, ang):
    x1, x2 = jnp.split(x, 2, axis=-1)
    cos = jnp.cos(ang)[:, None, :]
    sin = jnp.sin(ang)[:, None, :]
    return jnp.concatenate([x1 * cos - x2 * sin, x1 * sin + x2 * cos], axis=-1)


def axial_rope(x, ang_row, ang_col):
    xr, xc = jnp.split(x.astype(jnp.float32), 2, axis=-1)
    return jnp.concatenate([rope_half(xr, ang_row), rope_half(xc, ang_col)], axis=-1).astype(x.dtype)


def short_conv(t, w, b):
    y = lax.conv_general_dilated(t, w[:, None, :].astype(t.dtype), window_strides=(1,), padding='SAME',
                                 dimension_numbers=('NWC', 'WIO', 'NWC'), feature_group_count=t.shape[-1])
    return y + b.astype(t.dtype)


def mlstm_scan(q, k, v, i_pre, f_pre):
    B, T, H, _ = q.shape
    L = MLSTM_CHUNK
    nc = T // L
    f32 = jnp.float32

    def vec_chunks(t):
        return jnp.transpose(t.astype(f32).reshape(B, nc, L, H, -1), (1, 0, 3, 2, 4))

    def gate_chunks(t):
        return jnp.transpose(t.astype(f32).reshape(B, nc, L, H), (1, 0, 3, 2))

    tril = jnp.tril(jnp.ones((L, L), dtype=bool))

    def step(carry, xs):
        C, n, m = carry
        qc, kc, vc, ic, lf = xs
        b = jnp.cumsum(lf, axis=-1)
        D = jnp.where(tril, b[..., :, None] - b[..., None, :] + ic[..., None, :], NEG)
        m_inter = b + m[..., None]
        m_t = jnp.maximum(m_inter, jnp.max(D, axis=-1))
        P = jnp.exp(D - m_t[..., None])
        a = jnp.exp(m_inter - m_t)
        S = jnp.einsum('bhtd,bhsd->bhts', qc, kc) * P
        num = jnp.einsum('bhts,bhsv->bhtv', S, vc) + a[..., None] * jnp.einsum('bhtd,bhvd->bhtv', qc, C)
        qn = jnp.sum(S, axis=-1) + a * jnp.einsum('bhtd,bhd->bht', qc, n)
        h = num / jnp.maximum(jnp.abs(qn), jnp.exp(-m_t))[..., None]
        b_end = b[..., -1]
        g = b_end[..., None] - b + ic
        m_new = jnp.maximum(b_end + m, jnp.max(g, axis=-1))
        w = jnp.exp(g - m_new[..., None])
        a_end = jnp.exp(b_end + m - m_new)
        C_new = a_end[..., None, None] * C + jnp.einsum('bhsv,bhsd->bhvd', vc * w[..., None], kc)
        n_new = a_end[..., None] * n + jnp.einsum('bhs,bhsd->bhd', w, kc)
        return (C_new, n_new, m_new), h

    init = (jnp.zeros((B, H, v.shape[-1], q.shape[-1]), f32), jnp.zeros((B, H, q.shape[-1]), f32), jnp.zeros((B, H), f32))
    xs = (vec_chunks(q), vec_chunks(k), vec_chunks(v), gate_chunks(i_pre), gate_chunks(jax.nn.log_sigmoid(f_pre.astype(f32))))
    _, h = lax.scan(step, init, xs)
    return jnp.transpose(h, (1, 0, 3, 2, 4)).reshape(B, T, H, v.shape[-1])


def windowed_attention(q, k, v, k_ctx, v_ctx, sink):
    B, N = q.shape[:2]
    nb = N // QBLK
    n_band = 3 * QBLK
    n_ctx = k_ctx.shape[1]
    qb = q.reshape(B, nb, QBLK, ATTN_KV_HEADS, ATTN_GROUP, HEAD_DIM)

    def band(t):
        tp = jnp.pad(t, ((0, 0), (QBLK, QBLK), (0, 0), (0, 0))).reshape(B, nb + 2, QBLK, ATTN_KV_HEADS, HEAD_DIM)
        return jnp.concatenate([tp[:, :-2], tp[:, 1:-1], tp[:, 2:]], axis=2)

    kb, vb = band(k), band(v)
    s_loc = jnp.einsum('bnqhgd,bnkhd->bnhgqk', qb, kb).astype(jnp.float32) * ATTN_SCALE
    s_ctx = jnp.einsum('bnqhgd,bchd->bnhgqc', qb, k_ctx).astype(jnp.float32) * ATTN_SCALE
    blk = jnp.arange(nb)[:, None]
    q_pos = blk * QBLK + jnp.arange(QBLK)[None, :]
    k_pos = (blk - 1) * QBLK + jnp.arange(n_band)[None, :]
    valid = ((jnp.abs(k_pos[:, None, :] - q_pos[:, :, None]) <= WINDOW)
             & (k_pos >= 0)[:, None, :] & (k_pos < N)[:, None, :])
    s_loc = jnp.where(valid[None, :, None, None], s_loc, NEG)
    s_sink = jnp.broadcast_to(sink.astype(jnp.float32).reshape(1, 1, ATTN_KV_HEADS, ATTN_GROUP, 1, 1), s_loc.shape[:-1] + (1,))
    p = jax.nn.softmax(jnp.concatenate([s_loc, s_ctx, s_sink], axis=-1), axis=-1).astype(v.dtype)
    o = (jnp.einsum('bnhgqk,bnkhd->bnqhgd', p[..., :n_band], vb)
         + jnp.einsum('bnhgqc,bchd->bnqhgd', p[..., n_band:n_band + n_ctx], v_ctx))
    return o.reshape(B, N, ATTN_WIDTH)


def context_attention(q, k, v, sink):
    B, C = q.shape[:2]
    qg = q.reshape(B, C, ATTN_KV_HEADS, ATTN_GROUP, HEAD_DIM)
    s = jnp.einsum('bqhgd,bkhd->bhgqk', qg, k).astype(jnp.float32) * ATTN_SCALE
    s_sink = jnp.broadcast_to(sink.astype(jnp.float32).reshape(1, ATTN_KV_HEADS, ATTN_GROUP, 1, 1), s.shape[:-1] + (1,))
    p = jax.nn.softmax(jnp.concatenate([s, s_sink], axis=-1), axis=-1)[..., :C].astype(v.dtype)
    return jnp.einsum('bhgqk,bkhd->bqhgd', p, v).reshape(B, C, ATTN_WIDTH)


def moe(h, w_router, b_router, w_gate, w_up, w_down):
    aff = jax.nn.sigmoid((h @ w_router).astype(jnp.float32))
    biased = aff + b_router.astype(jnp.float32)
    grp = biased.reshape(biased.shape[:-1] + (N_GROUPS, EXPERTS_PER_GROUP))
    grp_score = jnp.sum(lax.top_k(grp, TOP_K)[0], axis=-1)
    best = jnp.argmax(grp_score, axis=-1)
    in_group = (jnp.arange(N_EXPERTS) // EXPERTS_PER_GROUP) == best[..., None]
    _, idx = lax.top_k(jnp.where(in_group, biased, NEG), TOP_K)
    wts = jnp.take_along_axis(aff, idx, axis=-1)
    wts = wts / jnp.sum(wts, axis=-1, keepdims=True)
    combine = jnp.sum(jax.nn.one_hot(idx, N_EXPERTS, dtype=jnp.float32) * wts[..., None], axis=-2).astype(h.dtype)
    y = jnp.zeros_like(h)
    for e in range(N_EXPERTS):
        act = jax.nn.silu(h @ w_gate[e]) * (h @ w_up[e])
        y = y + combine[..., e:e + 1] * (act @ w_down[e])
    return y


def mixer(hc, hl, ang_row, ang_col, w_in, b_in, g_q, g_k, sink, conv_w, conv_b, g_mh,
          w_br_attn, w_br_mlstm, w_out, need_ctx):
    B, n_ctx = hc.shape[:2]
    proj = jnp.concatenate([hc, hl], axis=1) @ w_in + b_in
    T = proj.shape[1]
    splits = np.cumsum(COL_SIZES)[:-1].tolist()
    a_q, a_k, a_v, m_q, m_k, m_v, m_o, m_gates, gate_attn, gate_mlstm = jnp.split(proj, splits, axis=-1)

    a_q = rms_norm(a_q.reshape(B, T, ATTN_HEADS, HEAD_DIM), g_q)
    a_k = rms_norm(a_k.reshape(B, T, ATTN_KV_HEADS, HEAD_DIM), g_k)
    a_v = a_v.reshape(B, T, ATTN_KV_HEADS, HEAD_DIM)
    q_lat = axial_rope(a_q[:, n_ctx:], ang_row, ang_col)
    k_lat = axial_rope(a_k[:, n_ctx:], ang_row, ang_col)
    k_ctx, v_ctx = a_k[:, :n_ctx], a_v[:, :n_ctx]
    attn_lat = windowed_attention(q_lat, k_lat, a_v[:, n_ctx:], k_ctx, v_ctx, sink)

    qk = jnp.concatenate([m_q, m_k], axis=-1)
    qk = jax.nn.silu(jnp.concatenate([short_conv(qk[:, :n_ctx], conv_w, conv_b),
                                      short_conv(qk[:, n_ctx:], conv_w, conv_b)], axis=1))
    m_q, m_k = jnp.split(qk, 2, axis=-1)
    m_q = m_q.reshape(B, T, MLSTM_HEADS, MLSTM_QK_DIM)
    m_k = m_k.reshape(B, T, MLSTM_HEADS, MLSTM_QK_DIM) * (MLSTM_QK_DIM ** -0.5)
    m_v = m_v.reshape(B, T, MLSTM_HEADS, MLSTM_V_DIM)
    i_fwd, f_fwd, i_bwd, f_bwd = jnp.split(m_gates, 4, axis=-1)

    def rev(t):
        return jnp.concatenate([jnp.flip(t[:, :n_ctx], axis=1), jnp.flip(t[:, n_ctx:], axis=1)], axis=1)

    h_fwd = mlstm_scan(m_q, m_k, m_v, i_fwd, f_fwd)
    h_bwd = rev(mlstm_scan(rev(m_q), rev(m_k), rev(m_v), rev(i_bwd), rev(f_bwd)))
    h_m = jax.nn.sigmoid(m_o.astype(jnp.float32)).reshape(B, T, MLSTM_HEADS, MLSTM_V_DIM) * (h_fwd + h_bwd)
    h_m = rms_norm(h_m, g_mh.reshape(MLSTM_HEADS, MLSTM_V_DIM)).reshape(B, T, MLSTM_WIDTH).astype(proj.dtype)

    def merge(attn, hm, ga, gm):
        return (jax.nn.sigmoid(ga) * (attn @ w_br_attn) + jax.nn.sigmoid(gm) * (hm @ w_br_mlstm)) @ w_out

    out_lat = merge(attn_lat, h_m[:, n_ctx:], gate_attn[:, n_ctx:], gate_mlstm[:, n_ctx:])
    if not need_ctx:
        return out_lat, None
    attn_ctx = context_attention(a_q[:, :n_ctx], k_ctx, v_ctx, sink)
    out_ctx = merge(attn_ctx, h_m[:, :n_ctx], gate_attn[:, :n_ctx], gate_mlstm[:, :n_ctx])
    return out_lat, out_ctx


def setup_inputs(seed: int = 0) -> dict:
    key = jax.random.key(seed)
    ks = jax.random.split(key, 24)
    nrm = jax.random.normal
    f_bias = jnp.linspace(3.0, 6.0, MLSTM_HEADS)
    b_in = 0.02 * nrm(ks[9], (DEPTH, D_IN))
    b_in = b_in.at[:, GATE_OFFSET + MLSTM_HEADS:GATE_OFFSET + 2 * MLSTM_HEADS].add(f_bias)
    b_in = b_in.at[:, GATE_OFFSET + 3 * MLSTM_HEADS:GATE_OFFSET + 4 * MLSTM_HEADS].add(f_bias)
    return {
        'x': nrm(ks[0], (BATCH, SEQ, D_MODEL)),
        'c': nrm(ks[1], (BATCH, D_MODEL)),
        'ctx': nrm(ks[2], (BATCH, CTX_LEN, D_MODEL)),
        'c_ctx': nrm(ks[3], (D_MODEL,)),
        'w_ada': nrm(ks[4], (DEPTH, D_MODEL, 6 * D_MODEL)) * (0.5 * D_MODEL ** -0.5),
        'b_ada': 0.01 * nrm(ks[5], (DEPTH, 6 * D_MODEL)),
        'g_mix': 1.0 + 0.02 * nrm(ks[6], (DEPTH, D_MODEL)),
        'g_ffn': 1.0 + 0.02 * nrm(ks[7], (DEPTH, D_MODEL)),
        'w_in': nrm(ks[8], (DEPTH, D_MODEL, D_IN)) * D_MODEL ** -0.5,
        'b_in': b_in,
        'g_q': 1.0 + 0.02 * nrm(ks[10], (DEPTH, HEAD_DIM)),
        'g_k': 1.0 + 0.02 * nrm(ks[11], (DEPTH, HEAD_DIM)),
        'sink': 0.5 * nrm(ks[12], (DEPTH, ATTN_HEADS)),
        'conv_w': nrm(ks[13], (DEPTH, KCONV, 2 * MLSTM_QK_WIDTH)) * KCONV ** -0.5,
        'conv_b': 0.02 * nrm(ks[14], (DEPTH, 2 * MLSTM_QK_WIDTH)),
        'g_mh': 1.0 + 0.02 * nrm(ks[15], (DEPTH, MLSTM_WIDTH)),
        'w_br_attn': nrm(ks[16], (DEPTH, ATTN_WIDTH, D_MODEL)) * ATTN_WIDTH ** -0.5,
        'w_br_mlstm': nrm(ks[17], (DEPTH, MLSTM_WIDTH, D_MODEL)) * MLSTM_WIDTH ** -0.5,
        'w_out': nrm(ks[18], (DEPTH, D_MODEL, D_MODEL)) * D_MODEL ** -0.5,
        'w_router': nrm(ks[19], (D_MODEL, N_EXPERTS)) * D_MODEL ** -0.5,
        'b_router': 0.01 * nrm(ks[20], (N_EXPERTS,)),
        'w_gate': nrm(ks[21], (DEPTH, N_EXPERTS, D_MODEL, D_FF_EXPERT)) * D_MODEL ** -0.5,
        'w_up': nrm(ks[22], (DEPTH, N_EXPERTS, D_MODEL, D_FF_EXPERT)) * D_MODEL ** -0.5,
        'w_down': nrm(ks[23], (DEPTH, N_EXPERTS, D_FF_EXPERT, D_MODEL)) * D_FF_EXPERT ** -0.5,
    }


def reference(x, c, ctx, c_ctx, w_ada, b_ada, g_mix, g_ffn, w_in, b_in, g_q, g_k, sink, conv_w, conv_b,
              g_mh, w_br_attn, w_br_mlstm, w_out, w_router, b_router, w_gate, w_up, w_down):
    n_lat = x.shape[1]
    n_ctx = ctx.shape[1]
    ang_row, ang_col = axial_angles(n_lat)
    for l in range(DEPTH):
        need_ctx = l < DEPTH - 1
        sh_m, sc_m, gt_m, sh_f, sc_f, gt_f = adaln_params(c, w_ada[l], b_ada[l])
        csh_m, csc_m, cgt_m, csh_f, csc_f, cgt_f = adaln_params(c_ctx, w_ada[l], b_ada[l])
        hl = modulate(x, g_mix[l], sh_m[:, None], sc_m[:, None])
        hc = modulate(ctx, g_mix[l], csh_m, csc_m)
        out_lat, out_ctx = mixer(hc, hl, ang_row, ang_col, w_in[l], b_in[l], g_q[l], g_k[l], sink[l],
                                 conv_w[l], conv_b[l], g_mh[l], w_br_attn[l], w_br_mlstm[l], w_out[l], need_ctx)
        x = x + gt_m[:, None] * out_lat
        hl = modulate(x, g_ffn[l], sh_f[:, None], sc_f[:, None])
        if need_ctx:
            ctx = ctx + cgt_m * out_ctx
            hc = modulate(ctx, g_ffn[l], csh_f, csc_f)
            y = moe(jnp.concatenate([hc, hl], axis=1), w_router, b_router, w_gate[l], w_up[l], w_down[l])
            ctx = ctx + cgt_f * y[:, :n_ctx]
            x = x + gt_f[:, None] * y[:, n_ctx:]
        else:
            x = x + gt_f[:, None] * moe(hl, w_router, b_router, w_gate[l], w_up[l], w_down[l])
    return x
```

```python
from contextlib import ExitStack
import os
import numpy as np
import concourse.bass as bass
import concourse.mybir as mybir
from concourse.bass_utils import run_bass_kernel_spmd

F32 = mybir.dt.float32
BF16 = mybir.dt.bfloat16
ALU = mybir.AluOpType
AF = mybir.ActivationFunctionType
AX = mybir.AxisListType
EPS = 1e-6
NEG = -1e30

CFG_FULL = dict(D=4096, NLAT=8192, CTX=256, DEPTH=2, NH=16, NKV=4, MH=8, DK=128, DV=256, NE=16, NG=4, FF=1024, GRID_W=64)


class T:
    psum = False
    def __init__(s, h):
        s.h = h; s.lw = None; s.rd = {}
    def __getitem__(s, i):
        return s.h[i]


class TP(T):
    psum = True
    def __init__(s, h, shape, dt):
        T.__init__(s, h)
        v = h[:]
        if dt != F32:
            v = v.bitcast(dt)
        n = 1
        for d in shape[1:]:
            n *= d
        v = v[0:shape[0], 0:n]
        if len(shape) == 3:
            v = v.rearrange("p (a b) -> p a b", a=shape[1])
        s.view = v
    def __getitem__(s, i):
        return s.view[i]


class KB:
    NDS = 12
    def __init__(s, nc):
        s.nc = nc
        s.eng = {'pe': nc.tensor, 'act': nc.scalar, 'dve': nc.vector, 'pool': nc.gpsimd, 'sp': nc.sync}
        s.sem = {e: nc.alloc_semaphore('s_' + e) for e in ['pe', 'act', 'dve', 'pool']}
        s.cnt = {e: 0 for e in s.sem}
        s.seen = {e: {} for e in s.eng}
        s.dq = ['sp', 'pool']
        s.dsem = {q: [nc.alloc_semaphore(f'd_{q}{i}') for i in range(s.NDS)] for q in s.dq}
        s.dcnt = {q: [0] * s.NDS for q in s.dq}
        s.dnext = {q: 0 for q in s.dq}
        s.uid = 0

    def semobj(s, key):
        return s.sem[key[1]] if key[0] == 'c' else s.dsem[key[1]][key[2]]

    def need(s, e, ev):
        if ev is None:
            return
        key, val = ev
        if val <= 0 or s.seen[e].get(key, 0) >= val:
            return
        if key == ('c', 'pe') and e == 'pe':
            return
        s.eng[e].wait_ge(s.semobj(key), val)
        s.seen[e][key] = val

    def wait(s, e, R=(), W=()):
        for t in R:
            s.need(e, t.lw)
            if t.psum:
                for k, v in t.rd.items():
                    if k != ('c', e):
                        s.need(e, (k, v))
        for t in W:
            s.need(e, t.lw)
            for k, v in t.rd.items():
                s.need(e, (k, v))

    def done(s, e, ins, R=(), W=()):
        s.cnt[e] += 1
        ins.then_inc(s.sem[e], 1)
        key = ('c', e); val = s.cnt[e]
        for t in W:
            t.lw = (key, val); t.rd = {}
        for t in R:
            t.rd[key] = val

    def op(s, e, f, R=(), W=()):
        s.wait(e, R, W)
        ins = f(s.eng[e])
        s.done(e, ins, R, W)
        return ins

    def dma(s, q, out, in_, R=(), W=()):
        s.wait(q, R, W)
        k = s.dnext[q]; s.dnext[q] = (k + 1) % s.NDS
        key = ('d', q, k)
        s.need(q, (key, s.dcnt[q][k]))
        ins = s.eng[q].dma_start(out=out, in_=in_)
        s.dcnt[q][k] += 16
        ins.then_inc(s.dsem[q][k], 16)
        val = s.dcnt[q][k]
        for t in W:
            t.lw = (key, val); t.rd = {}
        for t in R:
            t.rd[key] = val

    def barrier(s):
        for e in s.eng:
            for c in s.sem:
                s.need(e, (('c', c), s.cnt[c]))
            for q in s.dq:
                for k in range(s.NDS):
                    s.need(e, (('d', q, k), s.dcnt[q][k]))

    def name(s, p):
        s.uid += 1
        return f"{p}_{s.uid}"


def build(cfg, need_out_ctx=False):
    D = cfg['D']; NLAT = cfg['NLAT']; CTX = cfg['CTX']; DEPTH = cfg['DEPTH']
    NH = cfg['NH']; NKV = cfg['NKV']; MH = cfg['MH']; DK = cfg['DK']; DV = cfg['DV']
    NE = cfg['NE']; NG = cfg['NG']; FF = cfg['FF']
    HD = 128
    GRP = NH // NKV
    AW = NH * HD; KW = NKV * HD; QKW = MH * DK; MW = MH * DV; NGC = 4 * MH
    DIN = AW + 2 * KW + 2 * QKW + 2 * MW + NGC + 2 * D
    Tn = CTX + NLAT; TT = Tn // 128; CT = CTX // 128; KC = D // 128
    NB = NLAT // 128
    EPG = NE // NG
    o_aq = 0; o_ak = AW; o_av = AW + KW; o_mq = AW + 2 * KW; o_mk = o_mq + QKW; o_mv = o_mk + QKW
    o_mo = o_mv + MW; o_mg = o_mo + MW; o_ga = o_mg + NGC; o_gm = o_ga + D

    nc = bass.Bass("TRN2", target_bir_lowering=False)
    kb = KB(nc)

    def din(name, shape, dt=F32):
        return nc.dram_tensor(name, list(shape), dt, kind="ExternalInput").ap()

    def dsc(name, shape, dt):
        return nc.dram_tensor(name, list(shape), dt, kind="Internal").ap()

    x_in = din("x", [NLAT, D]); ctx_in = din("ctx", [CTX, D]); cc_in = din("cc", [2, D])
    w_ada = din("w_ada", [DEPTH, D, 6 * D]); b_ada = din("b_ada", [DEPTH, 6 * D])
    g_mix = din("g_mix", [DEPTH, D]); g_ffn = din("g_ffn", [DEPTH, D])
    w_in = din("w_in", [DEPTH, D, DIN]); b_in = din("b_in", [DEPTH, DIN])
    g_q = din("g_q", [DEPTH, HD]); g_k = din("g_k", [DEPTH, HD]); sink = din("sink", [DEPTH, NH])
    conv_w = din("conv_w", [DEPTH, 3, 2 * QKW]); conv_b = din("conv_b", [DEPTH, 2 * QKW])
    g_mh = din("g_mh", [DEPTH, MW])
    w_ba = din("w_br_attn", [DEPTH, AW, D]); w_bm = din("w_br_mlstm", [DEPTH, MW, D]); w_o = din("w_out", [DEPTH, D, D])
    w_r = din("w_router", [D, NE]); b_r = din("b_router", [1, NE])
    w_g = din("w_gate", [DEPTH, NE, D, FF]); w_u = din("w_up", [DEPTH, NE, D, FF]); w_d = din("w_down", [DEPTH, NE, FF, D])
    rope_cs = din("rope_cs", [NLAT, 128])
    cst = din("cst", [128, 5 * 128])
    y_out = nc.dram_tensor("y", [NLAT, D], F32, kind="ExternalOutput").ap()

    xs = dsc("xs", [Tn, D], F32)
    hT_d = dsc("hT_d", [128, KC, Tn], BF16)
    mod_d = dsc("mod_d", [DEPTH, 2, 6 * D], F32)
    def wblk(name, R_, C_, BW, lead=()):
        return dsc(name, list(lead) + [128, C_ // BW, R_ // 128, BW], BF16)
    tm_cols = [("aq", o_aq, AW), ("ak", o_ak, KW), ("av", o_av, KW), ("mv", o_mv, MW), ("mo", o_mo, MW), ("mg", o_mg, NGC)]
    fm_cols = [("mqk", o_mq, 2 * QKW), ("ga", o_ga, D), ("gm", o_gm, D)]
    NWS = min(2, DEPTH)
    WS = []
    for wl in range(NWS):
        sfx = f"_{wl}"
        WS.append(dict(
            win_tm={n: (wblk("wtm_" + n + sfx, D, cw, min(512, cw)), c0, cw, min(512, cw)) for (n, c0, cw) in tm_cols},
            win_fm={n: (wblk("wfm_" + n + sfx, D, cw, 128), c0, cw, 128) for (n, c0, cw) in fm_cols},
            wba_b=wblk("wba_b" + sfx, AW, D, 128), wbm_b=wblk("wbm_b" + sfx, MW, D, 128), wo_b=wblk("wo_b" + sfx, D, D, 512),
            wg_b=wblk("wg_b" + sfx, D, FF, 128, [NE]), wu_b=wblk("wu_b" + sfx, D, FF, 128, [NE]), wd_b=wblk("wd_b" + sfx, FF, D, 512, [NE])))
    aq_d = dsc("aq_d", [Tn, AW], BF16); ak_d = dsc("ak_d", [Tn, KW], BF16); av_d = dsc("av_d", [Tn, KW], BF16)
    mv_d = dsc("mv_d", [Tn, MW], BF16); mo_d = dsc("mo_d", [Tn, MW], BF16); mg_d = dsc("mg_d", [Tn, NGC], F32)
    mqk_d = dsc("mqk_d", [2 * QKW, Tn], BF16); mqkc_d = dsc("mqkc_d", [2 * QKW, Tn], BF16)
    sga_d = dsc("sga_d", [D, Tn], BF16); sgm_d = dsc("sgm_d", [D, Tn], BF16)
    qT_d = dsc("qT_d", [128, NH, Tn], BF16); kT_d = dsc("kT_d", [128, NKV, Tn], BF16)
    atT_d = dsc("atT_d", [128, NH, Tn], BF16)
    hf_d = dsc("hf_d", [Tn, MW], BF16); hb_d = dsc("hb_d", [Tn, MW], BF16)
    hmT_d = dsc("hmT_d", [128, MW // 128, Tn], BF16)

    def phase():
        kb.barrier()

    class Ph:
        def __enter__(s):
            s.es = ExitStack(); s.es.__enter__(); return s
        def __exit__(s, *a):
            kb.barrier(); return s.es.__exit__(*a)
        def sb(s, shape, dt=F32, nm="t"):
            return T(s.es.enter_context(nc.sbuf_tensor(kb.name(nm), list(shape), dt)))
        def ps(s, shape, dt=F32, nm="p"):
            return TP(s.es.enter_context(nc.psum_tensor(kb.name(nm), [128, 512], F32)), list(shape), dt)

    pes = ExitStack(); pes.__enter__()
    pes.enter_context(nc.allow_non_contiguous_dma(reason="strided layout transforms"))
    def psb(shape, dt=F32, nm="c"):
        return T(pes.enter_context(nc.sbuf_tensor(kb.name(nm), list(shape), dt)))
    cst_f = psb([128, 5 * 128], F32, "cstf")
    ident_f = None
    ident_b = psb([128, 128], BF16, "idb")
    maskT = psb([128, 3 * 128], F32, "maskT")
    tri_f = psb([128, 128], F32, "trif")
    tri_b = psb([128, 128], F32, "trib")
    tri_fb = psb([128, 128], BF16, "trifb")
    tri_bb = psb([128, 128], BF16, "tribb")
    ones_f = psb([128, 128], F32, "onesf")
    ones_b = psb([128, 128], BF16, "onesb")
    kb.dma('sp', cst_f[:], cst[:, :], W=[cst_f])
    kb.op('dve', lambda v: v.tensor_copy(out=ident_b[:], in_=cst_f[:, 0:128]), R=[cst_f], W=[ident_b])
    kb.op('dve', lambda v: v.tensor_copy(out=maskT[:], in_=cst_f[:, 128:512]), R=[cst_f], W=[maskT])
    kb.op('dve', lambda v: v.tensor_copy(out=tri_f[:], in_=cst_f[:, 512:640]), R=[cst_f], W=[tri_f])
    kb.op('dve', lambda v: v.memset(ones_f[:], 1.0), W=[ones_f])
    kb.op('dve', lambda v: v.memset(ones_b[:], 1.0), W=[ones_b])
    kb.op('dve', lambda v: v.tensor_tensor(out=tri_b[:], in0=ones_f[:], in1=tri_f[:], op=ALU.subtract), R=[ones_f, tri_f], W=[tri_b])
    kb.op('dve', lambda v: v.tensor_tensor(out=tri_b[:], in0=tri_b[:], in1=cst_f[:, 0:128], op=ALU.add), R=[tri_b, cst_f], W=[tri_b])
    kb.op('dve', lambda v: v.tensor_copy(out=tri_fb[:], in_=tri_f[:]), R=[tri_f], W=[tri_fb])
    kb.op('dve', lambda v: v.tensor_copy(out=tri_bb[:], in_=tri_b[:]), R=[tri_b], W=[tri_bb])
    ident_f = cst_f

    rr = [0]
    def cast_eng():
        rr[0] += 1
        return ['dve', 'act', 'pool'][rr[0] % 3]

    def copy_on(e, out, in_, R, W):
        if e == 'act':
            kb.op('act', lambda a: a.copy(out=out, in_=in_), R=R, W=W)
        else:
            kb.op(e, lambda v: v.tensor_copy(out=out, in_=in_), R=R, W=W)

    CVW = 1024
    cvbuf = [(psb([128, CVW], F32, "cf"), psb([128, CVW], BF16, "cb")) for _ in range(2)]
    cvi = [0]

    def cvt(src, dst, R_, C_, BW, eng):
        CC = min(C_, CVW)
        for r0 in range(0, R_, 128):
            k = r0 // 128
            for c0 in range(0, C_, CC):
                cw = min(CC, C_ - c0)
                f, b = cvbuf[cvi[0] % 2]; cvi[0] += 1
                kb.dma('sp', f[:, 0:cw], src[r0:r0 + 128, c0:c0 + cw], W=[f])
                copy_on(eng if eng else cast_eng(), b[:, 0:cw], f[:, 0:cw], [f], [b])
                kb.dma('pool', dst[:, c0 // BW:(c0 + cw) // BW, k, :], b[:, 0:cw].rearrange("p (a b) -> p a b", b=BW), R=[b])
                yield

    def convert_gen(l, eng=None):
        ws = WS[l % NWS]
        for n, (dst, c0, cw, BW) in list(ws['win_tm'].items()) + list(ws['win_fm'].items()):
            yield from cvt(w_in[l][:, c0:c0 + cw], dst, D, cw, BW, eng)
        yield from cvt(w_ba[l], ws['wba_b'], AW, D, 128, eng)
        yield from cvt(w_bm[l], ws['wbm_b'], MW, D, 128, eng)
        yield from cvt(w_o[l], ws['wo_b'], D, D, 512, eng)
        for e in range(NE):
            yield from cvt(w_g[l, e], ws['wg_b'][e], D, FF, 128, eng)
            yield from cvt(w_u[l, e], ws['wu_b'][e], D, FF, 128, eng)
            yield from cvt(w_d[l, e], ws['wd_b'][e], FF, D, 512, eng)

    pending = []

    def cv_tick(n=1):
        for _ in range(n):
            if not pending:
                return
            try:
                next(pending[0])
            except StopIteration:
                pending.pop(0)

    def cv_flush():
        while pending:
            cv_tick()
        kb.barrier()

    kb.dma('sp', xs[0:CTX, :], ctx_in[:, :])
    for r0 in range(0, NLAT, 1024):
        r1 = min(NLAT, r0 + 1024)
        kb.dma('sp', xs[CTX + r0:CTX + r1, :], x_in[r0:r1, :])
    kb.barrier()

    def adaln(l):
        with Ph() as ph:
            ct = ph.sb([128, 2, KC], F32, "ct")
            for r in range(2):
                kb.dma('sp', ct[:, r, :], cc_in[r, :].rearrange("(k p) -> p k", p=128), W=[ct])
            sg = ph.sb([128, 2, KC], F32, "sg")
            kb.op('act', lambda a: a.activation(out=sg[:], in_=ct[:], func=AF.Sigmoid), R=[ct], W=[sg])
            kb.op('dve', lambda v: v.tensor_tensor(out=ct[:], in0=ct[:], in1=sg[:], op=ALU.mult), R=[ct, sg], W=[ct])
            BW = 2048 if (6 * D) % 2048 == 0 else 1024
            NJ = BW // 512
            wt = [ph.sb([128, BW], F32, "wt") for _ in range(3)]
            pss = [ph.ps([2, 512], F32, "pa") for _ in range(NJ)]
            ob = ph.sb([2, BW], F32, "ob"); bb = ph.sb([2, BW], F32, "bb")
            NBLK = (6 * D) // BW
            i = 0
            for nb in range(NBLK):
                kb.dma('sp', bb[:], b_ada[l:l + 1, nb * BW:(nb + 1) * BW].to_broadcast([2, BW]), W=[bb])
                for kc in range(KC):
                    w = wt[i % 3]; i += 1
                    kb.dma('sp', w[:], w_ada[l, kc * 128:(kc + 1) * 128, nb * BW:(nb + 1) * BW], W=[w])
                    kb.wait('pe', R=[w, ct], W=pss if kc == 0 else [])
                    for j in range(NJ):
                        ins = nc.tensor.matmul(pss[j][:], lhsT=ct[:, :, kc], rhs=w[:, j * 512:(j + 1) * 512], start=(kc == 0), stop=(kc == KC - 1))
                    kb.done('pe', ins, R=[w, ct], W=pss if kc == KC - 1 else [])
                for j in range(NJ):
                    kb.op('dve', lambda v: v.tensor_tensor(out=ob[:, j * 512:(j + 1) * 512], in0=pss[j][:], in1=bb[:, j * 512:(j + 1) * 512], op=ALU.add), R=[pss[j], bb], W=[ob])
                kb.dma('pool', mod_d[l, :, nb * BW:(nb + 1) * BW], ob[:], R=[ob])

    def load_rep(ph_t, src_row_ap, n):
        kb.dma('sp', ph_t[:, 0:n], src_row_ap.to_broadcast([128, n]), W=[ph_t])

    def norm_phase(l, which):
        last = (l == DEPTH - 1)
        gsrc = g_mix if which == 0 else g_ffn
        so = 0 if which == 0 else 3
        with Ph() as ph:
            G = [ph.sb([128, D], F32, "G") for _ in range(2)]
            S = [ph.sb([128, D], F32, "S") for _ in range(2)]
            gr = ph.sb([128, D], F32, "gr")
            load_rep(gr, gsrc[l:l + 1, :], D)
            for r in range(2):
                load_rep(S[r], mod_d[l, r:r + 1, so * D:(so + 1) * D], D)
                load_rep(G[r], mod_d[l, r:r + 1, (so + 1) * D:(so + 2) * D], D)
                kb.op('dve', lambda v: v.scalar_tensor_tensor(out=G[r][:], in0=G[r][:], scalar=1.0, in1=gr[:], op0=ALU.add, op1=ALU.mult), R=[G[r], gr], W=[G[r]])
            xt = [ph.sb([128, D], F32, "xt") for _ in range(2)]
            junk = ph.sb([128, D], BF16, "junk")
            hb = [ph.sb([128, D], BF16 if which == 0 else F32, "hb") for _ in range(2 if which == 0 else 1)]
            hTt = [ph.sb([128, KC, 128], BF16, "hTt") for _ in range(2)]
            st = [ph.sb([128, 4], F32, "st") for _ in range(2)]
            if which == 0:
                ptr = [ph.ps([128, 8, 128], BF16, "ptr") for _ in range(2)]
                NPT = 8
            else:
                ptr = [ph.ps([128, 4, 128], F32, "ptr") for _ in range(2)]
                NPT = 4
                hTf = [ph.sb([128, KC, 128], F32, "hTf") for _ in range(1)]
                wr = ph.sb([128, KC, NE], F32, "wr")
                if 'a' not in os.environ.get("KX", ""):
                    kb.dma('sp', wr[:], w_r.rearrange("(k p) e -> p k e", p=128), W=[wr])
                brr = ph.sb([128, NE], F32, "brr")
                if 'a' not in os.environ.get("KX", ""):
                    load_rep(brr, b_r[0:1, :], NE)
                pl = [ph.ps([128, NE], F32, "pl") for _ in range(2)]
                rt = {k: ph.sb([128, NE], F32, "r" + k) for k in ['aff', 'bi', 'sel', 'm2', 'oh', 'w']}
                ps6 = ph.sb([128, NG, 6], F32, "ps6"); gs = ph.sb([128, NG], F32, "gs"); gm1 = ph.sb([128, 4], F32, "gm1")
                goh = ph.sb([128, NG], F32, "goh")
            t0 = CT if (last and which == 1) else 0
            for t in range(t0, TT):
                r = 1 if t < CT else 0
                x_ = xt[t % 2]; h_ = hb[t % len(hb)]; s_ = st[t % 2]; hT_ = hTt[t % 2]
                kb.dma('sp', x_[:], xs[t * 128:(t + 1) * 128, :], W=[x_])
                kb.op('act', lambda a: a.activation(out=junk[:], in_=x_[:], func=AF.Square, accum_out=s_[:, 0:1]), R=[x_], W=[junk, s_])
                kb.op('dve', lambda v: v.tensor_scalar(out=s_[:, 1:2], in0=s_[:, 0:1], scalar1=1.0 / D, scalar2=EPS, op0=ALU.mult, op1=ALU.add), R=[s_], W=[s_])
                kb.op('act', lambda a: a.activation(out=s_[:, 2:3], in_=s_[:, 1:2], func=AF.Sqrt), R=[s_], W=[s_])
                kb.op('dve', lambda v: v.reciprocal(out=s_[:, 3:4], in_=s_[:, 2:3]), R=[s_], W=[s_])
                kb.op('dve', lambda v: v.scalar_tensor_tensor(out=x_[:], in0=x_[:], scalar=s_[:, 3:4], in1=G[r][:], op0=ALU.mult, op1=ALU.mult), R=[x_, s_, G[r]], W=[x_])
                kb.op('pool', lambda v: v.tensor_tensor(out=h_[:], in0=x_[:], in1=S[r][:], op=ALU.add), R=[x_, S[r]], W=[h_])
                idn = ident_b[:] if which == 0 else ident_f[:, 0:128]
                idt = ident_b if which == 0 else cst_f
                for k0 in range(0, KC, NPT):
                    p_ = ptr[(k0 // NPT) % 2]
                    kb.wait('pe', R=[h_, idt], W=[p_])
                    for k in range(k0, min(KC, k0 + NPT)):
                        if which == 0:
                            ins = nc.tensor.transpose(out=p_[:, k - k0, :], in_=h_[:, k * 128:(k + 1) * 128], identity=idn)
                        else:
                            ins = nc.tensor.matmul(p_[:, k - k0, :], lhsT=h_[:, k * 128:(k + 1) * 128], rhs=idn, start=True, stop=True)
                    kb.done('pe', ins, R=[h_, idt], W=[p_])
                    n = min(KC, k0 + NPT) - k0
                    kb.op('act', lambda a: a.copy(out=hT_[:, k0:k0 + n, :], in_=p_[:, 0:n, :]), R=[p_], W=[hT_])
                    if which == 1 and 'b' not in os.environ.get("KX", ""):
                        hf_ = hTf[0]
                        kb.op('dve', lambda v: v.tensor_copy(out=hf_[:, k0:k0 + n, :], in_=p_[:, 0:n, :]), R=[p_], W=[hf_])
                kb.dma('pool', hT_d[:, :, t * 128:(t + 1) * 128], hT_[:], R=[hT_])
                if which == 1 and os.environ.get("KNOROUTE") != "2":
                    hf_ = hTf[0]; pl_ = pl[t % 2]
                    kb.wait('pe', R=[hf_, wr], W=[pl_])
                    for k in range(KC):
                        ins = nc.tensor.matmul(pl_[:], lhsT=hf_[:, k, :], rhs=wr[:, k, :], start=(k == 0), stop=(k == KC - 1))
                    kb.done('pe', ins, R=[hf_, wr], W=[pl_])
                    if os.environ.get("KNOROUTE"):
                        continue
                    aff = rt['aff']; bi = rt['bi']; sel = rt['sel']; m2 = rt['m2']; oh = rt['oh']; w_ = rt['w']
                    kb.op('act', lambda a: a.activation(out=aff[:], in_=pl_[:], func=AF.Sigmoid), R=[pl_], W=[aff])
                    kb.op('dve', lambda v: v.tensor_tensor(out=bi[:], in0=aff[:], in1=brr[:], op=ALU.add), R=[aff, brr], W=[bi])
                    big = bi[:].rearrange("p (g e) -> p g e", g=NG)
                    pi = 0
                    for a_ in range(EPG):
                        for b_ in range(a_ + 1, EPG):
                            kb.op('dve', lambda v: v.tensor_tensor(out=ps6[:, :, pi], in0=big[:, :, a_], in1=big[:, :, b_], op=ALU.add), R=[bi], W=[ps6])
                            pi += 1
                    kb.op('dve', lambda v: v.tensor_reduce(out=gs[:], in_=ps6[:], axis=AX.X, op=ALU.max), R=[ps6], W=[gs])
                    kb.op('dve', lambda v: v.tensor_reduce(out=gm1[:, 0:1], in_=gs[:], axis=AX.X, op=ALU.max), R=[gs], W=[gm1])
                    kb.op('dve', lambda v: v.tensor_scalar(out=goh[:], in0=gs[:], scalar1=gm1[:, 0:1], scalar2=None, op0=ALU.is_ge), R=[gs, gm1], W=[goh])
                    kb.op('dve', lambda v: v.tensor_tensor(out=m2[:].rearrange("p (g e) -> p g e", g=NG), in0=big, in1=goh[:].unsqueeze(2).to_broadcast([128, NG, EPG]), op=ALU.mult), R=[bi, goh], W=[m2])
                    kb.op('dve', lambda v: v.tensor_scalar(out=goh[:], in0=goh[:], scalar1=-1.0, scalar2=1e30, op0=ALU.add, op1=ALU.mult), R=[goh], W=[goh])
                    kb.op('dve', lambda v: v.tensor_tensor(out=m2[:].rearrange("p (g e) -> p g e", g=NG), in0=m2[:].rearrange("p (g e) -> p g e", g=NG), in1=goh[:].unsqueeze(2).to_broadcast([128, NG, EPG]), op=ALU.add), R=[m2, goh], W=[m2])
                    kb.op('dve', lambda v: v.tensor_reduce(out=gm1[:, 1:2], in_=m2[:], axis=AX.X, op=ALU.max), R=[m2], W=[gm1])
                    kb.op('dve', lambda v: v.tensor_scalar(out=sel[:], in0=m2[:], scalar1=gm1[:, 1:2], scalar2=None, op0=ALU.is_ge), R=[m2, gm1], W=[sel])
                    kb.op('dve', lambda v: v.scalar_tensor_tensor(out=m2[:], in0=sel[:], scalar=-1e30, in1=m2[:], op0=ALU.mult, op1=ALU.add), R=[sel, m2], W=[m2])
                    kb.op('dve', lambda v: v.tensor_reduce(out=gm1[:, 2:3], in_=m2[:], axis=AX.X, op=ALU.max), R=[m2], W=[gm1])
                    kb.op('dve', lambda v: v.tensor_scalar(out=oh[:], in0=m2[:], scalar1=gm1[:, 2:3], scalar2=None, op0=ALU.is_ge), R=[m2, gm1], W=[oh])
                    kb.op('dve', lambda v: v.tensor_tensor(out=sel[:], in0=sel[:], in1=oh[:], op=ALU.add), R=[sel, oh], W=[sel])
                    kb.op('dve', lambda v: v.tensor_tensor(out=w_[:], in0=sel[:], in1=aff[:], op=ALU.mult), R=[sel, aff], W=[w_])
                    kb.op('dve', lambda v: v.tensor_reduce(out=gm1[:, 3:4], in_=w_[:], axis=AX.X, op=ALU.add), R=[w_], W=[gm1])
                    kb.op('dve', lambda v: v.reciprocal(out=gm1[:, 3:4], in_=gm1[:, 3:4]), R=[gm1], W=[gm1])
                    kb.op('dve', lambda v: v.tensor_scalar(out=comb[:, t, :], in0=w_[:], scalar1=gm1[:, 3:4], scalar2=None, op0=ALU.mult), R=[w_, gm1], W=[comb])

    comb = psb([128, TT, NE], F32, "comb")

    def proj_phase(l):
        last = (l == DEPTH - 1)
        NTB = 512
        tm_groups = [("aq", aq_d, BF16), ("ak", ak_d, BF16), ("av", av_d, BF16), ("mv", mv_d, BF16), ("mo", mo_d, BF16), ("mg", mg_d, F32)]
        fm_groups = [("mqk", mqk_d, 0), ("ga", sga_d, 1), ("gm", sgm_d, 1)]
        with Ph() as ph:
            hTb = [ph.sb([128, KC, NTB], BF16, "hTb") for _ in range(1)]
            wtm = [ph.sb([128, KC, 512], BF16, "wtm") for _ in range(2)]
            brep = [ph.sb([128, 512], F32, "brep") for _ in range(2)]
            bcol = [ph.sb([128, 1], F32, "bcol") for _ in range(2)]
            pp = [ph.ps([128, 512], F32, "pp") for _ in range(4)]
            ot = [ph.sb([128, 512], BF16, "ot") for _ in range(3)]
            otf = [ph.sb([128, 512], F32, "otf") for _ in range(2)]
            wi = 0; pi = 0; oi = 0
            for tb0 in range(0, Tn, NTB):
                nt = min(NTB, Tn - tb0)
                hT_ = hTb[0]
                kb.dma('sp', hT_[:, :, 0:nt], hT_d[:, :, tb0:tb0 + nt], W=[hT_])
                for (gn, dst, dt) in tm_groups:
                    wsrc, c0, cw, BW = WS[l % NWS]['win_tm'][gn]
                    for n0 in range(0, cw, BW):
                        nw = BW
                        w_ = wtm[wi % 2]; br_ = brep[wi % 2]; wi += 1
                        kb.dma('sp', w_[:, :, 0:nw], wsrc[:, n0 // BW, :, :], W=[w_])
                        kb.dma('sp', br_[:, 0:nw], b_in[l:l + 1, c0 + n0:c0 + n0 + nw].to_broadcast([128, nw]), W=[br_])
                        for tt in range(nt // 128):
                            p_ = pp[pi % 4]; pi += 1
                            kb.wait('pe', R=[hT_, w_], W=[p_])
                            for k in range(KC):
                                ins = nc.tensor.matmul(p_[:, 0:nw], lhsT=hT_[:, k, tt * 128:(tt + 1) * 128], rhs=w_[:, k, 0:nw], start=(k == 0), stop=(k == KC - 1))
                            kb.done('pe', ins, R=[hT_, w_], W=[p_])
                            if dt == BF16:
                                o_ = ot[oi % 3]; oi += 1
                            else:
                                o_ = otf[oi % 2]; oi += 1
                            kb.op('dve', lambda v: v.tensor_tensor(out=o_[:, 0:nw], in0=p_[:, 0:nw], in1=br_[:, 0:nw], op=ALU.add), R=[p_, br_], W=[o_])
                            kb.dma('pool', dst[tb0 + tt * 128:tb0 + (tt + 1) * 128, n0:n0 + nw], o_[:, 0:nw], R=[o_])
                for (gn, dst, sg) in fm_groups:
                    wsrc, c0, cw, BW = WS[l % NWS]['win_fm'][gn]
                    for n0 in range(0, cw, 128):
                        w_ = wtm[wi % 2]; bc_ = bcol[wi % 2]; wi += 1
                        kb.dma('sp', w_[:, :, 0:128], wsrc[:, n0 // 128, :, :], W=[w_])
                        kb.dma('sp', bc_[:], b_in[l, c0 + n0:c0 + n0 + 128].rearrange("(p o) -> p o", o=1), W=[bc_])
                        p_ = pp[pi % 4]; pi += 1
                        kb.wait('pe', R=[hT_, w_], W=[p_])
                        for k in range(KC):
                            ins = nc.tensor.matmul(p_[:, 0:nt], lhsT=w_[:, k, 0:128], rhs=hT_[:, k, 0:nt], start=(k == 0), stop=(k == KC - 1))
                        kb.done('pe', ins, R=[hT_, w_], W=[p_])
                        o_ = ot[oi % 3]; oi += 1
                        kb.op('act', lambda a: a.activation(out=o_[:, 0:nt], in_=p_[:, 0:nt], func=(AF.Sigmoid if sg else AF.Identity), bias=bc_[:, 0:1], scale=1.0), R=[p_, bc_], W=[o_])
                        kb.dma('pool', dst[n0:n0 + 128, tb0:tb0 + nt], o_[:, 0:nt], R=[o_])

    def conv_phase(l):
        with Ph() as ph:
            xi = [ph.sb([128, Tn], BF16, "cxi") for _ in range(2)]
            ya = [ph.sb([128, Tn], F32, "cya") for _ in range(2)]
            yo = [ph.sb([128, Tn], BF16, "cyo") for _ in range(2)]
            cw = [ph.sb([128, 4], F32, "ccw") for _ in range(2)]
            for i, c0 in enumerate(range(0, 2 * QKW, 128)):
                x_ = xi[i % 2]; y_ = ya[i % 2]; o_ = yo[i % 2]; w_ = cw[i % 2]
                kb.dma('sp', x_[:], mqk_d[c0:c0 + 128, :], W=[x_])
                kb.dma('sp', w_[:, 0:3], conv_w[l, :, c0:c0 + 128].rearrange("j p -> p j"), W=[w_])
                kb.dma('sp', w_[:, 3:4], conv_b[l, c0:c0 + 128].rearrange("(p o) -> p o", o=1), W=[w_])
                kb.op('dve', lambda v: v.tensor_scalar(out=y_[:], in0=x_[:], scalar1=w_[:, 1:2], scalar2=w_[:, 3:4], op0=ALU.mult, op1=ALU.add), R=[x_, w_], W=[y_])
                for (s0, s1) in [(0, CTX), (CTX, Tn)]:
                    kb.op('dve', lambda v: v.scalar_tensor_tensor(out=y_[:, s0 + 1:s1], in0=x_[:, s0:s1 - 1], scalar=w_[:, 0:1], in1=y_[:, s0 + 1:s1], op0=ALU.mult, op1=ALU.add), R=[x_, w_, y_], W=[y_])
                    kb.op('dve', lambda v: v.scalar_tensor_tensor(out=y_[:, s0:s1 - 1], in0=x_[:, s0 + 1:s1], scalar=w_[:, 2:3], in1=y_[:, s0:s1 - 1], op0=ALU.mult, op1=ALU.add), R=[x_, w_, y_], W=[y_])
                sc = 1.0 if c0 < QKW else DK ** -0.5
                kb.op('act', lambda a: a.activation(out=o_[:], in_=y_[:], func=AF.Sigmoid), R=[y_], W=[o_])
                kb.op('dve', lambda v: v.scalar_tensor_tensor(out=o_[:], in0=y_[:], scalar=sc, in1=o_[:], op0=ALU.mult, op1=ALU.mult), R=[y_, o_], W=[o_])
                kb.dma('pool', mqkc_d[c0:c0 + 128, :], o_[:], R=[o_])

    def qk_phase(l):
        last = (l == DEPTH - 1)
        with Ph() as ph:
            gq = ph.sb([128, HD], F32, "gq"); gk = ph.sb([128, HD], F32, "gk")
            load_rep(gq, g_q[l:l + 1, :], HD); load_rep(gk, g_k[l:l + 1, :], HD)
            NQ = NH + NKV
            qi = [ph.sb([128, NQ, HD], BF16, "qi") for _ in range(2)]
            sq = ph.sb([128, NQ, HD], F32, "sq")
            xn = ph.sb([128, NQ, HD], F32, "xn")
            tmp = ph.sb([128, NQ, 2, 32], F32, "tmp"); tmp2 = ph.sb([128, NQ, 2, 32], F32, "tmp2")
            qo = [ph.sb([128, NQ, HD], BF16, "qo") for _ in range(2)]
            st = [ph.sb([128, 4, NQ], F32, "st") for _ in range(2)]
            cs = [ph.sb([128, 128], F32, "cs") for _ in range(2)]
            ptr = [ph.ps([128, 8, 128], BF16, "ptr") for _ in range(2)]
            oT = [ph.sb([128, NQ, 128], BF16, "oT") for _ in range(2)]
            for t in range(TT):
                q_ = qi[t % 2]; s_ = st[t % 2]; c_ = cs[t % 2]; o_ = qo[t % 2]; oT_ = oT[t % 2]
                kb.dma('sp', q_[:, 0:NH, :], aq_d[t * 128:(t + 1) * 128, :].rearrange("p (h d) -> p h d", d=HD), W=[q_])
                kb.dma('sp', q_[:, NH:NQ, :], ak_d[t * 128:(t + 1) * 128, :].rearrange("p (h d) -> p h d", d=HD), W=[q_])
                kb.op('pool', lambda v: v.tensor_tensor(out=sq[:], in0=q_[:], in1=q_[:], op=ALU.mult), R=[q_], W=[sq])
                kb.op('dve', lambda v: v.tensor_reduce(out=s_[:, 0, :], in_=sq[:], axis=AX.X, op=ALU.add), R=[sq], W=[s_])
                kb.op('dve', lambda v: v.tensor_scalar(out=s_[:, 1, :], in0=s_[:, 0, :], scalar1=1.0 / HD, scalar2=EPS, op0=ALU.mult, op1=ALU.add), R=[s_], W=[s_])
                kb.op('act', lambda a: a.activation(out=s_[:, 2, :], in_=s_[:, 1, :], func=AF.Sqrt), R=[s_], W=[s_])
                kb.op('dve', lambda v: v.reciprocal(out=s_[:, 3, :], in_=s_[:, 2, :]), R=[s_], W=[s_])
                kb.op('dve', lambda v: v.tensor_tensor(out=xn[:], in0=q_[:], in1=s_[:, 3, :].unsqueeze(2).to_broadcast([128, NQ, HD]), op=ALU.mult), R=[q_, s_], W=[xn])
                lat = t >= CT
                dstq = o_ if not lat else xn
                kb.op('dve', lambda v: v.tensor_tensor(out=dstq[:, 0:NH, :], in0=xn[:, 0:NH, :], in1=gq[:].unsqueeze(1).to_broadcast([128, NH, HD]), op=ALU.mult), R=[xn, gq], W=[dstq])
                kb.op('dve', lambda v: v.tensor_tensor(out=dstq[:, NH:NQ, :], in0=xn[:, NH:NQ, :], in1=gk[:].unsqueeze(1).to_broadcast([128, NKV, HD]), op=ALU.mult), R=[xn, gk], W=[dstq])
                if lat:
                    kb.dma('sp', c_[:], rope_cs[(t - CT) * 128:(t - CT + 1) * 128, :], W=[c_])
                    xv = xn[:].rearrange("p h (a b c) -> p h a b c", a=2, b=2)
                    ov = o_[:].rearrange("p h (a b c) -> p h a b c", a=2, b=2)
                    x1 = xv[:, :, :, 0, :]; x2 = xv[:, :, :, 1, :]
                    COS = c_[:, 0:64].rearrange("p (a c) -> p a c", a=2).unsqueeze(1).to_broadcast([128, NQ, 2, 32])
                    SIN = c_[:, 64:128].rearrange("p (a c) -> p a c", a=2).unsqueeze(1).to_broadcast([128, NQ, 2, 32])
                    kb.op('dve', lambda v: v.tensor_tensor(out=tmp[:], in0=x1, in1=COS, op=ALU.mult), R=[xn, c_], W=[tmp])
                    kb.op('pool', lambda v: v.tensor_tensor(out=tmp2[:], in0=x2, in1=SIN, op=ALU.mult), R=[xn, c_], W=[tmp2])
                    kb.op('dve', lambda v: v.tensor_tensor(out=ov[:, :, :, 0, :], in0=tmp[:], in1=tmp2[:], op=ALU.subtract), R=[tmp, tmp2], W=[o_])
                    kb.op('dve', lambda v: v.tensor_tensor(out=tmp[:], in0=x1, in1=SIN, op=ALU.mult), R=[xn, c_], W=[tmp])
                    kb.op('pool', lambda v: v.tensor_tensor(out=tmp2[:], in0=x2, in1=COS, op=ALU.mult), R=[xn, c_], W=[tmp2])
                    kb.op('dve', lambda v: v.tensor_tensor(out=ov[:, :, :, 1, :], in0=tmp[:], in1=tmp2[:], op=ALU.add), R=[tmp, tmp2], W=[o_])
                for k0 in range(0, NQ, 8):
                    p_ = ptr[(k0 // 8) % 2]
                    n = min(NQ, k0 + 8) - k0
                    kb.wait('pe', R=[o_, ident_b], W=[p_])
                    for k in range(k0, k0 + n):
                        ins = nc.tensor.transpose(out=p_[:, k - k0, :], in_=o_[:, k, :], identity=ident_b[:])
                    kb.done('pe', ins, R=[o_, ident_b], W=[p_])
                    kb.op('act', lambda a: a.copy(out=oT_[:, k0:k0 + n, :], in_=p_[:, 0:n, :]), R=[p_], W=[oT_])
                kb.dma('pool', qT_d[:, :, t * 128:(t + 1) * 128], oT_[:, 0:NH, :], R=[oT_])
                kb.dma('pool', kT_d[:, :, t * 128:(t + 1) * 128], oT_[:, NH:NQ, :], R=[oT_])

    def attn_phase(l):
        last = (l == DEPTH - 1)
        scale = HD ** -0.5
        with Ph() as ph:
            ES = ph.sb([128, NH], F32, "ES")
            load_rep(ES, sink[l:l + 1, :], NH)
            kb.op('act', lambda a: a.activation(out=ES[:], in_=ES[:], func=AF.Exp), R=[ES], W=[ES])
            kc = ph.sb([128, NKV, CTX], BF16, "kc")
            kb.dma('sp', kc[:], kT_d[:, :, 0:CTX], W=[kc])
            vc = ph.sb([128, CT, KW], BF16, "vc")
            kb.dma('sp', vc[:], av_d[0:CTX, :].rearrange("(c p) n -> p c n", p=128), W=[vc])
            qb = [ph.sb([128, NH, 128], BF16, "qb") for _ in range(2)]
            kw = [ph.sb([128, NKV, 384], BF16, "kw") for _ in range(2)]
            vw = [ph.sb([128, 3, KW], BF16, "vw") for _ in range(2)]
            pS = [ph.ps([128, 512], F32, "pS") for _ in range(2)]
            pN = [ph.ps([128, 512], F32, "pN") for _ in range(2)]
            pD = [ph.ps([128, 512], F32, "pD") for _ in range(2)]
            sm = [ph.sb([128, 512], F32, "sm") for _ in range(2)]
            pT = [ph.sb([128, 512], BF16, "pT") for _ in range(3)]
            dn = [ph.sb([128, 512], F32, "dn") for _ in range(2)]
            ob = [ph.sb([128, NH, 128], BF16, "ob") for _ in range(2)]
            blocks = ([] if last else [('c', c) for c in range(CT)]) + [('l', n) for n in range(NB)]
            si = 0; pi = 0
            for bi_, (kind, n) in enumerate(blocks):
                q_ = qb[bi_ % 2]; k_ = kw[bi_ % 2]; v_ = vw[bi_ % 2]; o_ = ob[bi_ % 2]
                tok0 = n * 128 if kind == 'c' else CTX + n * 128
                kb.dma('sp', q_[:], qT_d[:, :, tok0:tok0 + 128], W=[q_])
                chunks = []
                if kind == 'l':
                    lo = max(0, n - 1); hi = min(NB - 1, n + 1)
                    kb.dma('sp', k_[:, :, (lo - n + 1) * 128:(hi - n + 2) * 128], kT_d[:, :, CTX + lo * 128:CTX + (hi + 1) * 128], W=[k_])
                    kb.dma('sp', v_[:, lo - n + 1:hi - n + 2, :], av_d[CTX + lo * 128:CTX + (hi + 1) * 128, :].rearrange("(c p) n -> p c n", p=128), W=[v_])
                    for j in range(lo - n + 1, hi - n + 2):
                        chunks.append((k_, j, v_, j, j))
                for c in range(CT):
                    chunks.append((kc, c, vc, c, None))
                for g in range(NKV):
                    pN_ = pN[g % 2]; pD_ = pD[g % 2]
                    qg = q_[:, g * GRP:(g + 1) * GRP, :].rearrange("p h q -> p (h q)")
                    NQC = GRP * 128
                    for ci, (kt, kj, vt, vj, mj) in enumerate(chunks):
                        pS_ = pS[si % 2]; si += 1
                        kb.wait('pe', R=[kt, q_], W=[pS_])
                        ins = nc.tensor.matmul(pS_[:, 0:NQC], lhsT=kt[:, g, kj * 128:(kj + 1) * 128], rhs=qg, start=True, stop=True)
                        kb.done('pe', ins, R=[kt, q_], W=[pS_])
                        p_ = pT[pi % 3]; pi += 1
                        if mj is not None:
                            s_ = sm[ci % 2]
                            kb.op('dve', lambda v: v.tensor_tensor(out=s_[:, 0:NQC].rearrange("p (h q) -> p h q", h=GRP), in0=pS_[:, 0:NQC].rearrange("p (h q) -> p h q", h=GRP), in1=maskT[:, mj * 128:(mj + 1) * 128].unsqueeze(1).to_broadcast([128, GRP, 128]), op=ALU.add), R=[pS_, maskT], W=[s_])
                            kb.op('act', lambda a: a.activation(out=p_[:, 0:NQC], in_=s_[:, 0:NQC], func=AF.Exp, scale=scale), R=[s_], W=[p_])
                        else:
                            kb.op('act', lambda a: a.activation(out=p_[:, 0:NQC], in_=pS_[:, 0:NQC], func=AF.Exp, scale=scale), R=[pS_], W=[p_])
                        first = (ci == 0); lastc = (ci == len(chunks) - 1)
                        kb.wait('pe', R=[p_, vt, ones_b], W=[pN_, pD_] if first else [])
                        nc.tensor.matmul(pN_[:, 0:NQC], lhsT=vt[:, vj, g * HD:(g + 1) * HD], rhs=p_[:, 0:NQC], start=first, stop=lastc)
                        ins = nc.tensor.matmul(pD_[:, 0:NQC], lhsT=ones_b[:, :], rhs=p_[:, 0:NQC], start=first, stop=lastc)
                        kb.done('pe', ins, R=[p_, vt, ones_b], W=[pN_, pD_] if lastc else [])
                    d_ = dn[g % 2]
                    kb.op('dve', lambda v: v.tensor_tensor(out=d_[:, 0:NQC].rearrange("p (h q) -> p h q", h=GRP), in0=pD_[:, 0:NQC].rearrange("p (h q) -> p h q", h=GRP), in1=ES[:, g * GRP:(g + 1) * GRP].unsqueeze(2).to_broadcast([128, GRP, 128]), op=ALU.add), R=[pD_, ES], W=[d_])
                    kb.op('dve', lambda v: v.reciprocal(out=d_[:, 0:NQC], in_=d_[:, 0:NQC]), R=[d_], W=[d_])
                    kb.op('dve', lambda v: v.tensor_tensor(out=o_[:, g * GRP:(g + 1) * GRP, :].rearrange("p h q -> p (h q)"), in0=pN_[:, 0:NQC], in1=d_[:, 0:NQC], op=ALU.mult), R=[pN_, d_], W=[o_])
                kb.dma('pool', atT_d[:, :, tok0:tok0 + 128], o_[:], R=[o_])

    def mlstm_phase(l):
        last = (l == DEPTH - 1)
        NCH = TT
        M2 = 2 * MH
        order_f = list(range(NCH))
        order_b = list(range(CT - 1, -1, -1)) + list(range(NCH - 1, CT - 1, -1))
        with Ph() as ph:
            CS = ph.sb([128, NCH, M2], F32, "CS"); IA = ph.sb([128, NCH, M2], F32, "IA")
            EA = ph.sb([128, NCH, M2], F32, "EA"); WA = ph.sb([128, NCH, M2], F32, "WA")
            Um = [ph.sb([MH, NCH], F32, "Um") for _ in range(2)]; Be = [ph.sb([MH, NCH], F32, "Be") for _ in range(2)]
            Mm = [ph.sb([MH, NCH], F32, "Mm") for _ in range(2)]; Mp = [ph.sb([MH, NCH + 1], F32, "Mp") for _ in range(2)]
            Aa = [ph.sb([MH, NCH], F32, "Aa") for _ in range(2)]
            Mb = ph.sb([128, M2, NCH], F32, "Mb"); Ab = ph.sb([128, M2, NCH], F32, "Ab")
            with ExitStack() as es2:
                def sb2(shape, dt=F32, nm="g"):
                    return T(es2.enter_context(nc.sbuf_tensor(kb.name(nm), list(shape), dt)))
                def ps2(shape, dt=F32, nm="gp"):
                    return TP(es2.enter_context(nc.psum_tensor(kb.name(nm), [128, 512], F32)), list(shape), dt)
                gt = [sb2([128, NGC], F32, "gt") for _ in range(2)]
                spl = [sb2([128, M2], F32, "spl") for _ in range(2)]
                uu = [sb2([128, M2], F32, "uu") for _ in range(2)]
                pc = [ps2([128, M2], F32, "pc") for _ in range(2)]
                pu = [ps2([MH, 2, 128], F32, "pu") for _ in range(2)]
                pb = [ps2([MH, 2], F32, "pb") for _ in range(2)]
                for t in range(NCH):
                    g_ = gt[t % 2]; s_ = spl[t % 2]; u_ = uu[t % 2]; pc_ = pc[t % 2]; pu_ = pu[t % 2]; pb_ = pb[t % 2]
                    kb.dma('sp', g_[:], mg_d[t * 128:(t + 1) * 128, :], W=[g_])
                    kb.op('act', lambda a: a.activation(out=s_[:], in_=g_[:, M2:2 * M2], func=AF.Exp, scale=-1.0), R=[g_], W=[s_])
                    kb.op('act', lambda a: a.activation(out=s_[:], in_=s_[:], func=AF.Ln, bias=1.0, scale=1.0), R=[s_], W=[s_])
                    kb.wait('pe', R=[s_, tri_f, tri_b], W=[pc_])
                    nc.tensor.matmul(pc_[:, 0:MH], lhsT=tri_f[:], rhs=s_[:, 0:MH], start=True, stop=True)
                    ins = nc.tensor.matmul(pc_[:, MH:M2], lhsT=tri_b[:], rhs=s_[:, MH:M2], start=True, stop=True)
                    kb.done('pe', ins, R=[s_, tri_f, tri_b], W=[pc_])
                    kb.op('dve', lambda v: v.tensor_copy(out=CS[:, t, :], in_=pc_[:]), R=[pc_], W=[CS])
                    kb.op('dve', lambda v: v.tensor_copy(out=IA[:, t, :], in_=g_[:, 0:M2]), R=[g_], W=[IA])
                    kb.op('dve', lambda v: v.tensor_tensor(out=u_[:], in0=g_[:, 0:M2], in1=pc_[:], op=ALU.add), R=[g_, pc_], W=[u_])
                    kb.wait('pe', R=[u_, s_, cst_f, ones_f], W=[pu_, pb_])
                    for d in range(2):
                        nc.tensor.matmul(pu_[:, d, :], lhsT=u_[:, d * MH:(d + 1) * MH], rhs=ident_f[:, 0:128], start=True, stop=True)
                    for d in range(2):
                        ins = nc.tensor.matmul(pb_[:, d:d + 1], lhsT=s_[:, d * MH:(d + 1) * MH], rhs=ones_f[:, 0:1], start=True, stop=True)
                    kb.done('pe', ins, R=[u_, s_, cst_f, ones_f], W=[pu_, pb_])
                    for d in range(2):
                        kb.op('dve', lambda v: v.tensor_reduce(out=Um[d][:, t:t + 1], in_=pu_[:, d, :], axis=AX.X, op=ALU.max), R=[pu_], W=[Um[d]])
                        kb.op('dve', lambda v: v.tensor_scalar(out=Be[d][:, t:t + 1], in0=pb_[:, d:d + 1], scalar1=-1.0, scalar2=None, op0=ALU.mult), R=[pb_], W=[Be[d]])
                for d, order in enumerate([order_f, order_b]):
                    kb.op('dve', lambda v: v.memset(Mp[d][:], 0.0), W=[Mp[d]])
                    for k, c in enumerate(order):
                        kb.op('dve', lambda v: v.tensor_tensor(out=Mm[d][:, c:c + 1], in0=Mp[d][:, k:k + 1], in1=Um[d][:, c:c + 1], op=ALU.max), R=[Mp[d], Um[d]], W=[Mm[d]])
                        kb.op('dve', lambda v: v.tensor_tensor(out=Aa[d][:, c:c + 1], in0=Mp[d][:, k:k + 1], in1=Mm[d][:, c:c + 1], op=ALU.subtract), R=[Mp[d], Mm[d]], W=[Aa[d]])
                        kb.op('dve', lambda v: v.tensor_tensor(out=Mp[d][:, k + 1:k + 2], in0=Be[d][:, c:c + 1], in1=Mm[d][:, c:c + 1], op=ALU.add), R=[Be[d], Mm[d]], W=[Mp[d]])
                    kb.op('act', lambda a: a.activation(out=Aa[d][:], in_=Aa[d][:], func=AF.Exp), R=[Aa[d]], W=[Aa[d]])
                X = sb2([MH, MH, NCH], F32, "X")
                pbc = ps2([128, 512], F32, "pbc")
                HPB = max(1, 512 // NCH)
                for d in range(2):
                    for (src, dstb) in [(Mm[d], Mb), (Aa[d], Ab)]:
                        kb.op('dve', lambda v: v.tensor_tensor(out=X[:], in0=ident_f[0:MH, 0:MH].unsqueeze(2).to_broadcast([MH, MH, NCH]), in1=src[:].unsqueeze(1).to_broadcast([MH, MH, NCH]), op=ALU.mult), R=[cst_f, src], W=[X])
                        for r0 in range(0, MH, HPB):
                            nr = min(HPB, MH - r0)
                            kb.wait('pe', R=[X, ones_f], W=[pbc])
                            ins = nc.tensor.matmul(pbc[:, 0:nr * NCH], lhsT=ones_f[0:MH, :], rhs=X[:, r0:r0 + nr, :].rearrange("q r c -> q (r c)"), start=True, stop=True)
                            kb.done('pe', ins, R=[X, ones_f], W=[pbc])
                            kb.op('dve', lambda v: v.tensor_copy(out=dstb[:, d * MH + r0:d * MH + r0 + nr, :].rearrange("p r c -> p (r c)"), in_=pbc[:, 0:nr * NCH]), R=[pbc], W=[dstb])
                kb.op('dve', lambda v: v.tensor_tensor(out=EA[:], in0=CS[:], in1=Mb[:].rearrange("p r c -> p c r"), op=ALU.subtract), R=[CS, Mb], W=[EA])
                kb.op('act', lambda a: a.activation(out=EA[:], in_=EA[:], func=AF.Exp), R=[EA], W=[EA])
                kb.op('act', lambda a: a.activation(out=WA[:], in_=IA[:], func=AF.Exp), R=[IA], W=[WA])
                kb.op('dve', lambda v: v.tensor_tensor(out=WA[:], in0=WA[:], in1=EA[:], op=ALU.mult), R=[WA, EA], W=[WA])
                kb.barrier()
            qT = [ph.sb([128, MH, 128], BF16, "mq") for _ in range(2)]
            kT = [ph.sb([128, MH, 128], BF16, "mk") for _ in range(2)]
            va = [ph.sb([128, MH, DV + 1], BF16, "va") for _ in range(2)]
            for v_ in va:
                kb.op('dve', lambda v: v.memset(v_[:], 1.0), W=[v_])
            Cn = [ph.sb([128, DV + 1], F32, "Cn") for _ in range(MH)]
            Cb = [ph.sb([128, DV + 1], BF16, "Cb") for _ in range(MH)]
            pk = [ph.ps([128, 128], BF16, "pk") for _ in range(2)]
            pst = [ph.ps([128, 128], F32, "pst") for _ in range(2)]
            pnum = [ph.ps([128, DV + 1], F32, "pnum") for _ in range(2)]
            pup = [ph.ps([128, DV + 1], F32, "pup") for _ in range(2)]
            ksc = [ph.sb([128, 128], BF16, "ksc") for _ in range(2)]
            Sp = [ph.sb([128, 128], BF16, "Sp") for _ in range(2)]
            dd = [ph.sb([128, 2], F32, "dd") for _ in range(2)]
            Ho = [ph.sb([128, MH, DV], BF16, "Ho") for _ in range(2)]
            it = 0
            for d, order in enumerate([order_f, order_b]):
                tri = tri_fb if d == 0 else tri_bb
                hdst = hf_d if d == 0 else hb_d
                for h in range(MH):
                    kb.op('dve', lambda v: v.memset(Cn[h][:], 0.0), W=[Cn[h]])
                for k, c in enumerate(order):
                    q_ = qT[k % 2]; k_ = kT[k % 2]; v_ = va[k % 2]; H_ = Ho[k % 2]
                    kb.dma('sp', q_[:], mqkc_d[0:QKW, c * 128:(c + 1) * 128].rearrange("(h d) t -> d h t", d=DK), W=[q_])
                    kb.dma('sp', k_[:], mqkc_d[QKW:2 * QKW, c * 128:(c + 1) * 128].rearrange("(h d) t -> d h t", d=DK), W=[k_])
                    kb.dma('sp', v_[:, :, 0:DV], mv_d[c * 128:(c + 1) * 128, :].rearrange("p (h v) -> p h v", v=DV), W=[v_])
                    skip_out = last and c < CT
                    for h in range(MH):
                        r = d * MH + h
                        i2 = it % 2; it += 1
                        kb.op('dve', lambda v: v.tensor_scalar(out=Cn[h][:], in0=Cn[h][:], scalar1=Ab[:, r, c:c + 1], scalar2=None, op0=ALU.mult), R=[Cn[h], Ab], W=[Cn[h]])
                        kb.op('pool', lambda v: v.tensor_copy(out=Cb[h][:], in_=Cn[h][:]), R=[Cn[h]], W=[Cb[h]])
                        kb.wait('pe', R=[k_, ident_b], W=[pk[i2]])
                        ins = nc.tensor.transpose(out=pk[i2][:], in_=k_[:, h, :], identity=ident_b[:])
                        kb.done('pe', ins, R=[k_, ident_b], W=[pk[i2]])
                        kb.op('dve', lambda v: v.tensor_scalar(out=ksc[i2][:], in0=pk[i2][:], scalar1=WA[:, c, r:r + 1], scalar2=None, op0=ALU.mult), R=[pk[i2], WA], W=[ksc[i2]])
                        if not skip_out:
                            kb.wait('pe', R=[k_, q_], W=[pst[i2]])
                            ins = nc.tensor.matmul(pst[i2][:], lhsT=k_[:, h, :], rhs=q_[:, h, :], start=True, stop=True)
                            kb.done('pe', ins, R=[k_, q_], W=[pst[i2]])
                            kb.op('dve', lambda v: v.scalar_tensor_tensor(out=Sp[i2][:], in0=pst[i2][:], scalar=WA[:, c, r:r + 1], in1=tri[:], op0=ALU.mult, op1=ALU.mult), R=[pst[i2], WA, tri], W=[Sp[i2]])
                            kb.wait('pe', R=[Sp[i2], v_, q_, Cb[h]], W=[pnum[i2]])
                            nc.tensor.matmul(pnum[i2][:], lhsT=Sp[i2][:], rhs=v_[:, h, :], start=True, stop=False)
                            ins = nc.tensor.matmul(pnum[i2][:], lhsT=q_[:, h, :], rhs=Cb[h][:], start=False, stop=True)
                            kb.done('pe', ins, R=[Sp[i2], v_, q_, Cb[h]], W=[pnum[i2]])
                            kb.op('dve', lambda v: v.tensor_scalar(out=dd[i2][:, 0:1], in0=pnum[i2][:, DV:DV + 1], scalar1=-1.0, scalar2=None, op0=ALU.mult), R=[pnum[i2]], W=[dd[i2]])
                            kb.op('dve', lambda v: v.tensor_tensor(out=dd[i2][:, 0:1], in0=dd[i2][:, 0:1], in1=pnum[i2][:, DV:DV + 1], op=ALU.max), R=[pnum[i2], dd[i2]], W=[dd[i2]])
                            kb.op('dve', lambda v: v.tensor_tensor(out=dd[i2][:, 0:1], in0=dd[i2][:, 0:1], in1=EA[:, c, r:r + 1], op=ALU.max), R=[dd[i2], EA], W=[dd[i2]])
                            kb.op('dve', lambda v: v.reciprocal(out=dd[i2][:, 1:2], in_=dd[i2][:, 0:1]), R=[dd[i2]], W=[dd[i2]])
                            kb.op('act', lambda a: a.activation(out=H_[:, h, :], in_=pnum[i2][:, 0:DV], func=AF.Copy, scale=dd[i2][:, 1:2]), R=[pnum[i2], dd[i2]], W=[H_])
                        kb.wait('pe', R=[ksc[i2], v_], W=[pup[i2]])
                        ins = nc.tensor.matmul(pup[i2][:], lhsT=ksc[i2][:], rhs=v_[:, h, :], start=True, stop=True)
                        kb.done('pe', ins, R=[ksc[i2], v_], W=[pup[i2]])
                        kb.op('dve', lambda v: v.tensor_tensor(out=Cn[h][:], in0=Cn[h][:], in1=pup[i2][:], op=ALU.add), R=[Cn[h], pup[i2]], W=[Cn[h]])
                    if not skip_out:
                        kb.dma('pool', hdst[c * 128:(c + 1) * 128, :].rearrange("p (h v) -> p h v", v=DV), H_[:], R=[H_])

    def hm_phase(l):
        last = (l == DEPTH - 1)
        MC = MW // 128
        with Ph() as ph:
            gm = ph.sb([128, MW], F32, "gmh")
            load_rep(gm, g_mh[l:l + 1, :], MW)
            hf = [ph.sb([128, MW], BF16, "hf") for _ in range(2)]; hb = [ph.sb([128, MW], BF16, "hb") for _ in range(2)]
            mo = [ph.sb([128, MW], BF16, "mo") for _ in range(2)]
            sg = ph.sb([128, MW], F32, "sg"); hs = ph.sb([128, MW], F32, "hs"); sq = ph.sb([128, MW], F32, "sq")
            ho = [ph.sb([128, MW], BF16, "ho") for _ in range(2)]
            st = [ph.sb([128, 4, MH], F32, "st") for _ in range(2)]
            ptr = [ph.ps([128, 8, 128], BF16, "ptr") for _ in range(2)]
            oT = [ph.sb([128, MC, 128], BF16, "oT") for _ in range(2)]
            for t in range(CT if last else 0, TT):
                f_ = hf[t % 2]; b_ = hb[t % 2]; m_ = mo[t % 2]; o_ = ho[t % 2]; s_ = st[t % 2]; oT_ = oT[t % 2]
                kb.dma('sp', f_[:], hf_d[t * 128:(t + 1) * 128, :], W=[f_])
                kb.dma('sp', b_[:], hb_d[t * 128:(t + 1) * 128, :], W=[b_])
                kb.dma('sp', m_[:], mo_d[t * 128:(t + 1) * 128, :], W=[m_])
                kb.op('act', lambda a: a.activation(out=sg[:], in_=m_[:], func=AF.Sigmoid), R=[m_], W=[sg])
                kb.op('pool', lambda v: v.tensor_tensor(out=hs[:], in0=f_[:], in1=b_[:], op=ALU.add), R=[f_, b_], W=[hs])
                kb.op('dve', lambda v: v.tensor_tensor(out=hs[:], in0=hs[:], in1=sg[:], op=ALU.mult), R=[hs, sg], W=[hs])
                kb.op('pool', lambda v: v.tensor_tensor(out=sq[:], in0=hs[:], in1=hs[:], op=ALU.mult), R=[hs], W=[sq])
                kb.op('dve', lambda v: v.tensor_reduce(out=s_[:, 0, :], in_=sq[:].rearrange("p (h v) -> p h v", v=DV), axis=AX.X, op=ALU.add), R=[sq], W=[s_])
                kb.op('dve', lambda v: v.tensor_scalar(out=s_[:, 1, :], in0=s_[:, 0, :], scalar1=1.0 / DV, scalar2=EPS, op0=ALU.mult, op1=ALU.add), R=[s_], W=[s_])
                kb.op('act', lambda a: a.activation(out=s_[:, 2, :], in_=s_[:, 1, :], func=AF.Sqrt), R=[s_], W=[s_])
                kb.op('dve', lambda v: v.reciprocal(out=s_[:, 3, :], in_=s_[:, 2, :]), R=[s_], W=[s_])
                kb.op('dve', lambda v: v.tensor_tensor(out=hs[:].rearrange("p (h v) -> p h v", v=DV), in0=hs[:].rearrange("p (h v) -> p h v", v=DV), in1=s_[:, 3, :].unsqueeze(2).to_broadcast([128, MH, DV]), op=ALU.mult), R=[hs, s_], W=[hs])
                kb.op('dve', lambda v: v.tensor_tensor(out=o_[:], in0=hs[:], in1=gm[:], op=ALU.mult), R=[hs, gm], W=[o_])
                for k0 in range(0, MC, 8):
                    p_ = ptr[(k0 // 8) % 2]
                    n = min(MC, k0 + 8) - k0
                    kb.wait('pe', R=[o_, ident_b], W=[p_])
                    for k in range(k0, k0 + n):
                        ins = nc.tensor.transpose(out=p_[:, k - k0, :], in_=o_[:, k * 128:(k + 1) * 128], identity=ident_b[:])
                    kb.done('pe', ins, R=[o_, ident_b], W=[p_])
                    kb.op('act', lambda a: a.copy(out=oT_[:, k0:k0 + n, :], in_=p_[:, 0:n, :]), R=[p_], W=[oT_])
                kb.dma('pool', hmT_d[:, :, t * 128:(t + 1) * 128], oT_[:], R=[oT_])

    def merge_phase(l):
        last = (l == DEPTH - 1)
        NTB = 512
        AC = AW // 128; MC = MW // 128
        with Ph() as ph:
            gts = [ph.sb([128, 512], F32, "gts") for _ in range(3)]
            aT = ph.sb([128, AC, NTB], BF16, "aT"); mT = ph.sb([128, MC, NTB], BF16, "mT")
            wa = [ph.sb([128, AC, 128], BF16, "wa") for _ in range(2)]; wm = [ph.sb([128, MC, 128], BF16, "wm") for _ in range(2)]
            ga = [ph.sb([128, NTB], BF16, "ga") for _ in range(2)]; gmm = [ph.sb([128, NTB], BF16, "gmm") for _ in range(2)]
            pa = [ph.ps([128, NTB], F32, "pa") for _ in range(2)]; pm = [ph.ps([128, NTB], F32, "pm") for _ in range(2)]
            t1 = [ph.sb([128, NTB], F32, "t1") for _ in range(2)]
            mg = ph.sb([128, KC, NTB], BF16, "mg")
            wo = [ph.sb([128, KC, 512], BF16, "wo") for _ in range(2)]
            po = [ph.ps([128, 512], F32, "po") for _ in range(2)]
            xt = [ph.sb([128, 512], F32, "xt") for _ in range(3)]
            wi = 0; xi = 0; pi = 0
            tbs = list(range(0, Tn, NTB))
            for tb0 in tbs:
                nt = min(NTB, Tn - tb0)
                kb.dma('sp', aT[:, :, 0:nt], atT_d[:, :, tb0:tb0 + nt], W=[aT])
                kb.dma('sp', mT[:, :, 0:nt], hmT_d[:, :, tb0:tb0 + nt], W=[mT])
                for j in range(KC):
                    a_ = wa[j % 2]; m_ = wm[j % 2]; ga_ = ga[j % 2]; gm_ = gmm[j % 2]; pa_ = pa[j % 2]; pm_ = pm[j % 2]; t_ = t1[j % 2]
                    kb.dma('sp', a_[:], WS[l % NWS]['wba_b'][:, j, :, :], W=[a_])
                    kb.dma('sp', m_[:], WS[l % NWS]['wbm_b'][:, j, :, :], W=[m_])
                    kb.dma('sp', ga_[:, 0:nt], sga_d[j * 128:(j + 1) * 128, tb0:tb0 + nt], W=[ga_])
                    kb.dma('sp', gm_[:, 0:nt], sgm_d[j * 128:(j + 1) * 128, tb0:tb0 + nt], W=[gm_])
                    kb.wait('pe', R=[a_, aT], W=[pa_])
                    for k in range(AC):
                        ins = nc.tensor.matmul(pa_[:, 0:nt], lhsT=a_[:, k, :], rhs=aT[:, k, 0:nt], start=(k == 0), stop=(k == AC - 1))
                    kb.done('pe', ins, R=[a_, aT], W=[pa_])
                    kb.wait('pe', R=[m_, mT], W=[pm_])
                    for k in range(MC):
                        ins = nc.tensor.matmul(pm_[:, 0:nt], lhsT=m_[:, k, :], rhs=mT[:, k, 0:nt], start=(k == 0), stop=(k == MC - 1))
                    kb.done('pe', ins, R=[m_, mT], W=[pm_])
                    kb.op('dve', lambda v: v.tensor_tensor(out=t_[:, 0:nt], in0=pa_[:, 0:nt], in1=ga_[:, 0:nt], op=ALU.mult), R=[pa_, ga_], W=[t_])
                    kb.op('dve', lambda v: v.tensor_tensor(out=mg[:, j, 0:nt], in0=pm_[:, 0:nt], in1=gm_[:, 0:nt], op=ALU.mult), R=[pm_, gm_], W=[mg])
                    kb.op('pool', lambda v: v.tensor_tensor(out=mg[:, j, 0:nt], in0=mg[:, j, 0:nt], in1=t_[:, 0:nt], op=ALU.add), R=[mg, t_], W=[mg])
                for n0 in range(0, D, 512):
                    w_ = wo[wi % 2]; wi += 1
                    kb.dma('sp', w_[:], WS[l % NWS]['wo_b'][:, n0 // 512, :, :], W=[w_])
                    for tt in range(nt // 128):
                        tg = (tb0 // 128) + tt
                        if last and tg < CT:
                            continue
                        r = 1 if tg < CT else 0
                        p_ = po[pi % 2]; pi += 1
                        x_ = xt[xi % 3]; xi += 1
                        kb.dma('sp', x_[:], xs[tg * 128:(tg + 1) * 128, n0:n0 + 512], W=[x_])
                        g_ = gts[xi % 3]
                        kb.dma('sp', g_[:], mod_d[l, r:r + 1, 2 * D + n0:2 * D + n0 + 512].to_broadcast([128, 512]), W=[g_])
                        kb.wait('pe', R=[mg, w_], W=[p_])
                        for k in range(KC):
                            ins = nc.tensor.matmul(p_[:], lhsT=mg[:, k, tt * 128:(tt + 1) * 128], rhs=w_[:, k, :], start=(k == 0), stop=(k == KC - 1))
                        kb.done('pe', ins, R=[mg, w_], W=[p_])
                        t_ = t1[pi % 2]
                        kb.op('dve', lambda v: v.tensor_tensor(out=t_[:, 0:512], in0=p_[:], in1=g_[:], op=ALU.mult), R=[p_, g_], W=[t_])
                        kb.op('pool', lambda v: v.tensor_tensor(out=x_[:], in0=x_[:], in1=t_[:, 0:512], op=ALU.add), R=[x_, t_], W=[x_])
                        kb.dma('pool', xs[tg * 128:(tg + 1) * 128, n0:n0 + 512], x_[:], R=[x_])

    def moe_phase(l):
        last = (l == DEPTH - 1)
        NTB = 512
        FC = FF // 128
        with Ph() as ph:
            gts = [ph.sb([128, 512], F32, "gts") for _ in range(2)]
            hTb = ph.sb([128, KC, NTB], BF16, "hTb")
            yacc = ph.sb([128, NTB // 128, D], F32, "yacc")
            wg = [ph.sb([128, KC, 128], BF16, "wg") for _ in range(2)]; wu = [ph.sb([128, KC, 128], BF16, "wu") for _ in range(2)]
            wd = [ph.sb([128, FC, 512], BF16, "wd") for _ in range(2)]
            pg = [ph.ps([128, NTB], F32, "pg") for _ in range(2)]; pu = [ph.ps([128, NTB], F32, "pu") for _ in range(2)]
            pdn = [ph.ps([128, 512], F32, "pdn") for _ in range(4)]
            sg = [ph.sb([128, NTB], F32, "sg") for _ in range(2)]
            aT = [ph.sb([128, FC, NTB], BF16, "aT") for _ in range(2)]
            xt = [ph.sb([128, 512], F32, "xt") for _ in range(2)]
            gi = 0; di = 0; xi = 0
            t_start = CTX if last else 0
            for tb0 in range(t_start, Tn, NTB):
                nt = min(NTB, Tn - tb0)
                ntl = nt // 128
                kb.dma('sp', hTb[:, :, 0:nt], hT_d[:, :, tb0:tb0 + nt], W=[hTb])
                kb.op('pool', lambda v: v.memset(yacc[:], 0.0), W=[yacc])
                for e in range(NE):
                    a_ = aT[e % 2]
                    for j in range(FC):
                        g_ = wg[gi % 2]; u_ = wu[gi % 2]; pg_ = pg[gi % 2]; pu_ = pu[gi % 2]; s_ = sg[gi % 2]; gi += 1
                        kb.dma('sp', g_[:], WS[l % NWS]['wg_b'][e, :, j, :, :], W=[g_])
                        cv_tick()
                        kb.dma('sp', u_[:], WS[l % NWS]['wu_b'][e, :, j, :, :], W=[u_])
                        kb.wait('pe', R=[g_, hTb], W=[pg_])
                        for k in range(KC):
                            ins = nc.tensor.matmul(pg_[:, 0:nt], lhsT=g_[:, k, :], rhs=hTb[:, k, 0:nt], start=(k == 0), stop=(k == KC - 1))
                        kb.done('pe', ins, R=[g_, hTb], W=[pg_])
                        kb.wait('pe', R=[u_, hTb], W=[pu_])
                        for k in range(KC):
                            ins = nc.tensor.matmul(pu_[:, 0:nt], lhsT=u_[:, k, :], rhs=hTb[:, k, 0:nt], start=(k == 0), stop=(k == KC - 1))
                        kb.done('pe', ins, R=[u_, hTb], W=[pu_])
                        kb.op('act', lambda a: a.activation(out=s_[:, 0:nt], in_=pg_[:, 0:nt], func=AF.Sigmoid), R=[pg_], W=[s_])
                        kb.op('dve', lambda v: v.tensor_tensor(out=s_[:, 0:nt], in0=s_[:, 0:nt], in1=pg_[:, 0:nt], op=ALU.mult), R=[s_, pg_], W=[s_])
                        kb.op('dve', lambda v: v.tensor_tensor(out=a_[:, j, 0:nt], in0=s_[:, 0:nt], in1=pu_[:, 0:nt], op=ALU.mult), R=[s_, pu_], W=[a_])
                    for n0 in range(0, D, 512):
                        d_ = wd[(di // max(1, ntl)) % 2]
                        kb.dma('sp', d_[:], WS[l % NWS]['wd_b'][e, :, n0 // 512, :, :], W=[d_])
                        cv_tick()
                        for tt in range(ntl):
                            tg = tb0 // 128 + tt
                            p_ = pdn[di % 4]; di += 1
                            kb.wait('pe', R=[a_, d_], W=[p_])
                            for k in range(FC):
                                ins = nc.tensor.matmul(p_[:], lhsT=a_[:, k, tt * 128:(tt + 1) * 128], rhs=d_[:, k, :], start=(k == 0), stop=(k == FC - 1))
                            kb.done('pe', ins, R=[a_, d_], W=[p_])
                            kb.op('dve', lambda v: v.scalar_tensor_tensor(out=yacc[:, tt, n0:n0 + 512], in0=p_[:], scalar=comb[:, tg, e:e + 1], in1=yacc[:, tt, n0:n0 + 512], op0=ALU.mult, op1=ALU.add), R=[p_, comb, yacc], W=[yacc])
                for tt in range(ntl):
                    tg = tb0 // 128 + tt
                    r = 1 if tg < CT else 0
                    for n0 in range(0, D, 512):
                        x_ = xt[xi % 2]; g_ = gts[xi % 2]; xi += 1
                        kb.dma('sp', x_[:], xs[tg * 128:(tg + 1) * 128, n0:n0 + 512], W=[x_])
                        kb.dma('sp', g_[:], mod_d[l, r:r + 1, 5 * D + n0:5 * D + n0 + 512].to_broadcast([128, 512]), W=[g_])
                        kb.op('dve', lambda v: v.tensor_tensor(out=yacc[:, tt, n0:n0 + 512], in0=yacc[:, tt, n0:n0 + 512], in1=g_[:], op=ALU.mult), R=[yacc, g_], W=[yacc])
                        kb.op('pool', lambda v: v.tensor_tensor(out=x_[:], in0=x_[:], in1=yacc[:, tt, n0:n0 + 512], op=ALU.add), R=[x_, yacc], W=[x_])
                        if last:
                            kb.dma('pool', y_out[(tg - CT) * 128:(tg - CT + 1) * 128, n0:n0 + 512], x_[:], R=[x_])
                        else:
                            kb.dma('pool', xs[tg * 128:(tg + 1) * 128, n0:n0 + 512], x_[:], R=[x_])

    import os
    stop = int(os.environ.get("KSTOP", "999"))
    phs = []
    def convert_layer(l):
        if l == 0 or not os.environ.get("KOVL", "1") == "1":
            pending.append(convert_gen(l))
        cv_flush()
        if l + 1 < DEPTH and os.environ.get("KOVL", "1") == "1":
            pending.append(convert_gen(l + 1, 'pool'))

    for l in range(DEPTH):
        phs += [lambda l=l: convert_layer(l), lambda l=l: adaln(l), lambda l=l: norm_phase(l, 0), lambda l=l: proj_phase(l),
                lambda l=l: conv_phase(l), lambda l=l: qk_phase(l), lambda l=l: attn_phase(l), lambda l=l: mlstm_phase(l),
                lambda l=l: hm_phase(l), lambda l=l: merge_phase(l), lambda l=l: norm_phase(l, 1), lambda l=l: moe_phase(l)]
    for i, p in enumerate(phs):
        if i < stop:
            p()
    kb.barrier()
    dbg = [d for d in os.environ.get("KDBG", "").split(",") if d]
    scr = dict(xs=xs, hT_d=hT_d, mod_d=mod_d, aq_d=aq_d, ak_d=ak_d, av_d=av_d, mv_d=mv_d, mo_d=mo_d, mg_d=mg_d, mqk_d=mqk_d,
               mqkc_d=mqkc_d, sga_d=sga_d, sgm_d=sgm_d, qT_d=qT_d, kT_d=kT_d, atT_d=atT_d, hf_d=hf_d, hb_d=hb_d, hmT_d=hmT_d)
    for d in dbg:
        src = scr[d]
        o = nc.dram_tensor("dbg_" + d, list(src.shape), src.dtype, kind="ExternalOutput").ap()
        if len(src.shape) == 3:
            for i in range(src.shape[0]):
                kb.dma('sp', o[i], src[i])
        else:
            kb.dma('sp', o[:, :], src[:, :])
    if "comb" in os.environ.get("KDBG2", ""):
        o = nc.dram_tensor("dbg_comb", [128, TT * NE], F32, kind="ExternalOutput").ap()
        kb.dma('sp', o[:, :], comb[:].rearrange("p t e -> p (t e)"), R=[comb])
    kb.barrier()
    pes.__exit__(None, None, None)
    return nc


def host_consts(cfg):
    NLAT = cfg['NLAT']; GW = cfg['GRID_W']
    rp = 32
    inv = (10000.0 ** (-np.arange(rp, dtype=np.float32) / rp)).astype(np.float32)
    t = np.arange(NLAT)
    ar = (t // GW).astype(np.float32)[:, None] * inv
    ac = (t % GW).astype(np.float32)[:, None] * inv
    rope = np.concatenate([np.cos(ar), np.cos(ac), np.sin(ar), np.sin(ac)], axis=1).astype(np.float32)
    ident = np.eye(128, dtype=np.float32)
    i = np.arange(128)[None, :]; j = np.arange(128)[:, None]
    m = []
    for c in (-1, 0, 1):
        rel = (c * 128 + j) - i
        m.append(np.where(np.abs(rel) <= 128, 0.0, NEG).astype(np.float32))
    trif = (j <= i).astype(np.float32)
    cst = np.concatenate([ident] + m + [trif], axis=1).astype(np.float32)
    return rope, cst


def make_in_maps(cfg, inp):
    D = cfg['D']; MH = cfg['MH']; NH = cfg['NH']; NKV = cfg['NKV']
    AW = NH * 128; KW = NKV * 128; QKW = MH * cfg['DK']; MW = MH * cfg['DV']
    o_mg = AW + 2 * KW + 2 * QKW + 2 * MW
    perm = np.arange(inp['w_in'].shape[-1])
    g = np.arange(4 * MH).reshape(4, MH)
    perm[o_mg:o_mg + 4 * MH] = o_mg + np.concatenate([g[0], g[2], g[1], g[3]])
    w_in = np.ascontiguousarray(inp['w_in'][:, :, perm]); b_in = np.ascontiguousarray(inp['b_in'][:, perm])
    rope, cst = host_consts(cfg)
    f = lambda a: np.ascontiguousarray(np.asarray(a, dtype=np.float32))
    maps = []
    for b in range(inp['x'].shape[0]):
        maps.append({
            'x': f(inp['x'][b]), 'ctx': f(inp['ctx'][b]), 'cc': f(np.stack([inp['c'][b], inp['c_ctx']], 0)),
            'w_ada': f(inp['w_ada']), 'b_ada': f(inp['b_ada']), 'g_mix': f(inp['g_mix']), 'g_ffn': f(inp['g_ffn']),
            'w_in': f(w_in), 'b_in': f(b_in), 'g_q': f(inp['g_q']), 'g_k': f(inp['g_k']), 'sink': f(inp['sink']),
            'conv_w': f(inp['conv_w']), 'conv_b': f(inp['conv_b']), 'g_mh': f(inp['g_mh']),
            'w_br_attn': f(inp['w_br_attn']), 'w_br_mlstm': f(inp['w_br_mlstm']), 'w_out': f(inp['w_out']),
            'w_router': f(inp['w_router']), 'b_router': f(np.asarray(inp['b_router']).reshape(1, -1)),
            'w_gate': f(inp['w_gate']), 'w_up': f(inp['w_up']), 'w_down': f(inp['w_down']),
            'rope_cs': rope, 'cst': cst,
        })
    return maps


def run(cfg, inp, trace=False):
    nc = build(cfg)
    maps = make_in_maps(cfg, inp)
    res = run_bass_kernel_spmd(nc, maps, core_ids=list(range(len(maps))), trace=trace)
    out = np.stack([res.results[b]['y'] for b in range(len(maps))], 0).astype(np.float32)
    return out, res


def kernel(**inputs):
    inp = {k: np.asarray(v) for k, v in inputs.items()}
    out, _ = run(CFG_FULL, inp)
    return out
```

```python
from contextlib import ExitStack
import os
import numpy as np
import concourse.bass as bass
import concourse.mybir as mybir
from concourse.bass_utils import run_bass_kernel_spmd

F32 = mybir.dt.float32
BF16 = mybir.dt.bfloat16
ALU = mybir.AluOpType
AF = mybir.ActivationFunctionType
AX = mybir.AxisListType
EPS = 1e-6
NEG = -1e30

CFG_FULL = dict(D=4096, NLAT=8192, CTX=256, DEPTH=2, NH=16, NKV=4, MH=8, DK=128, DV=256, NE=16, NG=4, FF=1024, GRID_W=64)


class T:
    psum = False
    def __init__(s, h):
        s.h = h; s.lw = None; s.rd = {}
    def __getitem__(s, i):
        return s.h[i]


class TP(T):
    psum = True
    def __init__(s, h, shape, dt):
        T.__init__(s, h)
        v = h[:]
        if dt != F32:
            v = v.bitcast(dt)
        n = 1
        for d in shape[1:]:
            n *= d
        v = v[0:shape[0], 0:n]
        if len(shape) == 3:
            v = v.rearrange("p (a b) -> p a b", a=shape[1])
        s.view = v
    def __getitem__(s, i):
        return s.view[i]


class KB:
    NDS = 12
    def __init__(s, nc):
        s.nc = nc
        s.eng = {'pe': nc.tensor, 'act': nc.scalar, 'dve': nc.vector, 'pool': nc.gpsimd, 'sp': nc.sync}
        s.sem = {e: nc.alloc_semaphore('s_' + e) for e in ['pe', 'act', 'dve', 'pool']}
        s.cnt = {e: 0 for e in s.sem}
        s.seen = {e: {} for e in s.eng}
        s.dq = ['sp', 'pool']
        s.dsem = {q: [nc.alloc_semaphore(f'd_{q}{i}') for i in range(s.NDS)] for q in s.dq}
        s.dcnt = {q: [0] * s.NDS for q in s.dq}
        s.dnext = {q: 0 for q in s.dq}
        s.uid = 0

    def semobj(s, key):
        return s.sem[key[1]] if key[0] == 'c' else s.dsem[key[1]][key[2]]

    def need(s, e, ev):
        if ev is None:
            return
        key, val = ev
        if val <= 0 or s.seen[e].get(key, 0) >= val:
            return
        if key == ('c', 'pe') and e == 'pe':
            return
        s.eng[e].wait_ge(s.semobj(key), val)
        s.seen[e][key] = val

    def wait(s, e, R=(), W=()):
        for t in R:
            s.need(e, t.lw)
            if t.psum:
                for k, v in t.rd.items():
                    if k != ('c', e):
                        s.need(e, (k, v))
        for t in W:
            s.need(e, t.lw)
            for k, v in t.rd.items():
                s.need(e, (k, v))

    def done(s, e, ins, R=(), W=()):
        s.cnt[e] += 1
        ins.then_inc(s.sem[e], 1)
        key = ('c', e); val = s.cnt[e]
        for t in W:
            t.lw = (key, val); t.rd = {}
        for t in R:
            t.rd[key] = val

    def op(s, e, f, R=(), W=()):
        s.wait(e, R, W)
        ins = f(s.eng[e])
        s.done(e, ins, R, W)
        return ins

    def dma(s, q, out, in_, R=(), W=()):
        s.wait(q, R, W)
        k = s.dnext[q]; s.dnext[q] = (k + 1) % s.NDS
        key = ('d', q, k)
        s.need(q, (key, s.dcnt[q][k]))
        ins = s.eng[q].dma_start(out=out, in_=in_)
        s.dcnt[q][k] += 16
        ins.then_inc(s.dsem[q][k], 16)
        val = s.dcnt[q][k]
        for t in W:
            t.lw = (key, val); t.rd = {}
        for t in R:
            t.rd[key] = val

    def barrier(s):
        for e in s.eng:
            for c in s.sem:
                s.need(e, (('c', c), s.cnt[c]))
            for q in s.dq:
                for k in range(s.NDS):
                    s.need(e, (('d', q, k), s.dcnt[q][k]))

    def name(s, p):
        s.uid += 1
        return f"{p}_{s.uid}"


def build(cfg, need_out_ctx=False):
    D = cfg['D']; NLAT = cfg['NLAT']; CTX = cfg['CTX']; DEPTH = cfg['DEPTH']
    NH = cfg['NH']; NKV = cfg['NKV']; MH = cfg['MH']; DK = cfg['DK']; DV = cfg['DV']
    NE = cfg['NE']; NG = cfg['NG']; FF = cfg['FF']
    HD = 128
    GRP = NH // NKV
    AW = NH * HD; KW = NKV * HD; QKW = MH * DK; MW = MH * DV; NGC = 4 * MH
    DIN = AW + 2 * KW + 2 * QKW + 2 * MW + NGC + 2 * D
    Tn = CTX + NLAT; TT = Tn // 128; CT = CTX // 128; KC = D // 128
    NB = NLAT // 128
    EPG = NE // NG
    o_aq = 0; o_ak = AW; o_av = AW + KW; o_mq = AW + 2 * KW; o_mk = o_mq + QKW; o_mv = o_mk + QKW
    o_mo = o_mv + MW; o_mg = o_mo + MW; o_ga = o_mg + NGC; o_gm = o_ga + D

    nc = bass.Bass("TRN2", target_bir_lowering=False)
    kb = KB(nc)

    def din(name, shape, dt=F32):
        return nc.dram_tensor(name, list(shape), dt, kind="ExternalInput").ap()

    def dsc(name, shape, dt):
        return nc.dram_tensor(name, list(shape), dt, kind="Internal").ap()

    x_in = din("x", [NLAT, D]); ctx_in = din("ctx", [CTX, D]); cc_in = din("cc", [2, D])
    w_ada = din("w_ada", [DEPTH, D, 6 * D]); b_ada = din("b_ada", [DEPTH, 6 * D])
    g_mix = din("g_mix", [DEPTH, D]); g_ffn = din("g_ffn", [DEPTH, D])
    w_in = din("w_in", [DEPTH, D, DIN]); b_in = din("b_in", [DEPTH, DIN])
    g_q = din("g_q", [DEPTH, HD]); g_k = din("g_k", [DEPTH, HD]); sink = din("sink", [DEPTH, NH])
    conv_w = din("conv_w", [DEPTH, 3, 2 * QKW]); conv_b = din("conv_b", [DEPTH, 2 * QKW])
    g_mh = din("g_mh", [DEPTH, MW])
    w_ba = din("w_br_attn", [DEPTH, AW, D]); w_bm = din("w_br_mlstm", [DEPTH, MW, D]); w_o = din("w_out", [DEPTH, D, D])
    w_r = din("w_router", [D, NE]); b_r = din("b_router", [1, NE])
    w_g = din("w_gate", [DEPTH, NE, D, FF]); w_u = din("w_up", [DEPTH, NE, D, FF]); w_d = din("w_down", [DEPTH, NE, FF, D])
    rope_cs = din("rope_cs", [NLAT, 128])
    cst = din("cst", [128, 5 * 128])
    y_out = nc.dram_tensor("y", [NLAT, D], F32, kind="ExternalOutput").ap()

    xs = dsc("xs", [Tn, D], F32)
    hT_d = dsc("hT_d", [128, KC, Tn], BF16)
    mod_d = dsc("mod_d", [DEPTH, 2, 6 * D], F32)
    def wblk(name, R_, C_, BW, lead=()):
        return dsc(name, list(lead) + [128, C_ // BW, R_ // 128, BW], BF16)
    tm_cols = [("aq", o_aq, AW), ("ak", o_ak, KW), ("av", o_av, KW), ("mv", o_mv, MW), ("mo", o_mo, MW), ("mg", o_mg, NGC)]
    fm_cols = [("mqk", o_mq, 2 * QKW), ("ga", o_ga, D), ("gm", o_gm, D)]
    NWS = min(2, DEPTH)
    WS = []
    for wl in range(NWS):
        sfx = f"_{wl}"
        WS.append(dict(
            win_tm={n: (wblk("wtm_" + n + sfx, D, cw, min(512, cw)), c0, cw, min(512, cw)) for (n, c0, cw) in tm_cols},
            win_fm={n: (wblk("wfm_" + n + sfx, D, cw, 128), c0, cw, 128) for (n, c0, cw) in fm_cols},
            wba_b=wblk("wba_b" + sfx, AW, D, 128), wbm_b=wblk("wbm_b" + sfx, MW, D, 128), wo_b=wblk("wo_b" + sfx, D, D, 512),
            wg_b=wblk("wg_b" + sfx, D, FF, 128, [NE]), wu_b=wblk("wu_b" + sfx, D, FF, 128, [NE]), wd_b=wblk("wd_b" + sfx, FF, D, 512, [NE])))
    aq_d = dsc("aq_d", [Tn, AW], BF16); ak_d = dsc("ak_d", [Tn, KW], BF16); av_d = dsc("av_d", [Tn, KW], BF16)
    mv_d = dsc("mv_d", [Tn, MW], BF16); mo_d = dsc("mo_d", [Tn, MW], BF16); mg_d = dsc("mg_d", [Tn, NGC], F32)
    mqk_d = dsc("mqk_d", [2 * QKW, Tn], BF16); mqkc_d = dsc("mqkc_d", [2 * QKW, Tn], BF16)
    sga_d = dsc("sga_d", [D, Tn], BF16); sgm_d = dsc("sgm_d", [D, Tn], BF16)
    qT_d = dsc("qT_d", [128, NH, Tn], BF16); kT_d = dsc("kT_d", [128, NKV, Tn], BF16)
    atT_d = dsc("atT_d", [128, NH, Tn], BF16)
    hf_d = dsc("hf_d", [Tn, MW], BF16); hb_d = dsc("hb_d", [Tn, MW], BF16)
    hmT_d = dsc("hmT_d", [128, MW // 128, Tn], BF16)

    def phase():
        kb.barrier()

    class Ph:
        def __enter__(s):
            s.es = ExitStack(); s.es.__enter__(); return s
        def __exit__(s, *a):
            kb.barrier(); return s.es.__exit__(*a)
        def sb(s, shape, dt=F32, nm="t"):
            return T(s.es.enter_context(nc.sbuf_tensor(kb.name(nm), list(shape), dt)))
        def ps(s, shape, dt=F32, nm="p"):
            return TP(s.es.enter_context(nc.psum_tensor(kb.name(nm), [128, 512], F32)), list(shape), dt)

    pes = ExitStack(); pes.__enter__()
    pes.enter_context(nc.allow_non_contiguous_dma(reason="strided layout transforms"))
    def psb(shape, dt=F32, nm="c"):
        return T(pes.enter_context(nc.sbuf_tensor(kb.name(nm), list(shape), dt)))
    cst_f = psb([128, 5 * 128], F32, "cstf")
    ident_f = None
    ident_b = psb([128, 128], BF16, "idb")
    maskT = psb([128, 3 * 128], F32, "maskT")
    tri_f = psb([128, 128], F32, "trif")
    tri_b = psb([128, 128], F32, "trib")
    tri_fb = psb([128, 128], BF16, "trifb")
    tri_bb = psb([128, 128], BF16, "tribb")
    ones_f = psb([128, 128], F32, "onesf")
    ones_b = psb([128, 128], BF16, "onesb")
    kb.dma('sp', cst_f[:], cst[:, :], W=[cst_f])
    kb.op('dve', lambda v: v.tensor_copy(out=ident_b[:], in_=cst_f[:, 0:128]), R=[cst_f], W=[ident_b])
    kb.op('dve', lambda v: v.tensor_copy(out=maskT[:], in_=cst_f[:, 128:512]), R=[cst_f], W=[maskT])
    kb.op('dve', lambda v: v.tensor_copy(out=tri_f[:], in_=cst_f[:, 512:640]), R=[cst_f], W=[tri_f])
    kb.op('dve', lambda v: v.memset(ones_f[:], 1.0), W=[ones_f])
    kb.op('dve', lambda v: v.memset(ones_b[:], 1.0), W=[ones_b])
    kb.op('dve', lambda v: v.tensor_tensor(out=tri_b[:], in0=ones_f[:], in1=tri_f[:], op=ALU.subtract), R=[ones_f, tri_f], W=[tri_b])
    kb.op('dve', lambda v: v.tensor_tensor(out=tri_b[:], in0=tri_b[:], in1=cst_f[:, 0:128], op=ALU.add), R=[tri_b, cst_f], W=[tri_b])
    kb.op('dve', lambda v: v.tensor_copy(out=tri_fb[:], in_=tri_f[:]), R=[tri_f], W=[tri_fb])
    kb.op('dve', lambda v: v.tensor_copy(out=tri_bb[:], in_=tri_b[:]), R=[tri_b], W=[tri_bb])
    ident_f = cst_f

    rr = [0]
    def cast_eng():
        rr[0] += 1
        return ['dve', 'act', 'pool'][rr[0] % 3]

    def copy_on(e, out, in_, R, W):
        if e == 'act':
            kb.op('act', lambda a: a.copy(out=out, in_=in_), R=R, W=W)
        else:
            kb.op(e, lambda v: v.tensor_copy(out=out, in_=in_), R=R, W=W)

    CVW = 1024
    cvbuf = [(psb([128, CVW], F32, "cf"), psb([128, CVW], BF16, "cb")) for _ in range(3)]
    cvi = [0]

    def cvt(src, dst, R_, C_, BW, eng):
        CC = min(C_, CVW)
        for r0 in range(0, R_, 128):
            k = r0 // 128
            for c0 in range(0, C_, CC):
                cw = min(CC, C_ - c0)
                f, b = cvbuf[cvi[0] % 3]; cvi[0] += 1
                kb.dma('sp', f[:, 0:cw], src[r0:r0 + 128, c0:c0 + cw], W=[f])
                copy_on(eng if eng else cast_eng(), b[:, 0:cw], f[:, 0:cw], [f], [b])
                kb.dma('pool', dst[:, c0 // BW:(c0 + cw) // BW, k, :], b[:, 0:cw].rearrange("p (a b) -> p a b", b=BW), R=[b])
                yield

    ready = {}

    def convert_gen(l, eng=None, parts="AB"):
        ws = WS[l % NWS]
        if "A" in parts:
            for n, (dst, c0, cw, BW) in list(ws['win_tm'].items()) + list(ws['win_fm'].items()):
                yield from cvt(w_in[l][:, c0:c0 + cw], dst, D, cw, BW, eng)
        if "B" in parts:
            yield from cvt(w_ba[l], ws['wba_b'], AW, D, 128, eng)
            yield from cvt(w_bm[l], ws['wbm_b'], MW, D, 128, eng)
            yield from cvt(w_o[l], ws['wo_b'], D, D, 512, eng)
            ready[(l, 'm')] = True
            for e in range(NE):
                yield from cvt(w_g[l, e], ws['wg_b'][e], D, FF, 128, eng)
                yield from cvt(w_u[l, e], ws['wu_b'][e], D, FF, 128, eng)
                yield from cvt(w_d[l, e], ws['wd_b'][e], FF, D, 512, eng)

    pending = []

    def cv_tick(n=1):
        for _ in range(n):
            if not pending:
                return
            try:
                next(pending[0])
            except StopIteration:
                pending.pop(0)

    def cv_flush():
        while pending:
            cv_tick()
        kb.barrier()

    kb.dma('sp', xs[0:CTX, :], ctx_in[:, :])
    for r0 in range(0, NLAT, 1024):
        r1 = min(NLAT, r0 + 1024)
        kb.dma('sp', xs[CTX + r0:CTX + r1, :], x_in[r0:r1, :])
    kb.barrier()

    def adaln(l):
        with Ph() as ph:
            ct = ph.sb([128, 2, KC], F32, "ct")
            for r in range(2):
                kb.dma('sp', ct[:, r, :], cc_in[r, :].rearrange("(k p) -> p k", p=128), W=[ct])
            sg = ph.sb([128, 2, KC], F32, "sg")
            kb.op('act', lambda a: a.activation(out=sg[:], in_=ct[:], func=AF.Sigmoid), R=[ct], W=[sg])
            kb.op('dve', lambda v: v.tensor_tensor(out=ct[:], in0=ct[:], in1=sg[:], op=ALU.mult), R=[ct, sg], W=[ct])
            BW = 2048 if (6 * D) % 2048 == 0 else 1024
            NJ = BW // 512
            wt = [ph.sb([128, BW], F32, "wt") for _ in range(3)]
            pss = [ph.ps([2, 512], F32, "pa") for _ in range(NJ)]
            ob = ph.sb([2, BW], F32, "ob"); bb = ph.sb([2, BW], F32, "bb")
            NBLK = (6 * D) // BW
            i = 0
            for nb in range(NBLK):
                kb.dma('sp', bb[:], b_ada[l:l + 1, nb * BW:(nb + 1) * BW].to_broadcast([2, BW]), W=[bb])
                for kc in range(KC):
                    w = wt[i % 3]; i += 1
                    kb.dma('sp', w[:], w_ada[l, kc * 128:(kc + 1) * 128, nb * BW:(nb + 1) * BW], W=[w])
                    kb.wait('pe', R=[w, ct], W=pss if kc == 0 else [])
                    for j in range(NJ):
                        ins = nc.tensor.matmul(pss[j][:], lhsT=ct[:, :, kc], rhs=w[:, j * 512:(j + 1) * 512], start=(kc == 0), stop=(kc == KC - 1))
                    kb.done('pe', ins, R=[w, ct], W=pss if kc == KC - 1 else [])
                for j in range(NJ):
                    kb.op('dve', lambda v: v.tensor_tensor(out=ob[:, j * 512:(j + 1) * 512], in0=pss[j][:], in1=bb[:, j * 512:(j + 1) * 512], op=ALU.add), R=[pss[j], bb], W=[ob])
                kb.dma('pool', mod_d[l, :, nb * BW:(nb + 1) * BW], ob[:], R=[ob])

    def load_rep(ph_t, src_row_ap, n):
        kb.dma('sp', ph_t[:, 0:n], src_row_ap.to_broadcast([128, n]), W=[ph_t])

    def norm_phase(l, which):
        last = (l == DEPTH - 1)
        gsrc = g_mix if which == 0 else g_ffn
        so = 0 if which == 0 else 3
        with Ph() as ph:
            G = [ph.sb([128, D], F32, "G") for _ in range(2)]
            S = [ph.sb([128, D], F32, "S") for _ in range(2)]
            gr = ph.sb([128, D], F32, "gr")
            load_rep(gr, gsrc[l:l + 1, :], D)
            for r in range(2):
                load_rep(S[r], mod_d[l, r:r + 1, so * D:(so + 1) * D], D)
                load_rep(G[r], mod_d[l, r:r + 1, (so + 1) * D:(so + 2) * D], D)
                kb.op('dve', lambda v: v.scalar_tensor_tensor(out=G[r][:], in0=G[r][:], scalar=1.0, in1=gr[:], op0=ALU.add, op1=ALU.mult), R=[G[r], gr], W=[G[r]])
            xt = [ph.sb([128, D], F32, "xt") for _ in range(2)]
            junk = ph.sb([128, D], BF16, "junk")
            hb = [ph.sb([128, D], BF16 if which == 0 else F32, "hb") for _ in range(2 if which == 0 else 1)]
            hTt = [ph.sb([128, KC, 128], BF16, "hTt") for _ in range(2)]
            st = [ph.sb([128, 4], F32, "st") for _ in range(2)]
            if which == 0:
                ptr = [ph.ps([128, 8, 128], BF16, "ptr") for _ in range(2)]
                NPT = 8
            else:
                ptr = [ph.ps([128, 4, 128], F32, "ptr") for _ in range(2)]
                NPT = 4
                hTf = [ph.sb([128, KC, 128], F32, "hTf") for _ in range(1)]
                wr = ph.sb([128, KC, NE], F32, "wr")
                if 'a' not in os.environ.get("KX", ""):
                    kb.dma('sp', wr[:], w_r.rearrange("(k p) e -> p k e", p=128), W=[wr])
                brr = ph.sb([128, NE], F32, "brr")
                if 'a' not in os.environ.get("KX", ""):
                    load_rep(brr, b_r[0:1, :], NE)
                pl = [ph.ps([128, NE], F32, "pl") for _ in range(2)]
                rt = {k: ph.sb([128, NE], F32, "r" + k) for k in ['aff', 'bi', 'sel', 'm2', 'oh', 'w']}
                ps6 = ph.sb([128, NG, 6], F32, "ps6"); gs = ph.sb([128, NG], F32, "gs"); gm1 = ph.sb([128, 4], F32, "gm1")
                goh = ph.sb([128, NG], F32, "goh")
            t0 = CT if (last and which == 1) else 0
            for t in range(t0, TT):
                r = 1 if t < CT else 0
                x_ = xt[t % 2]; h_ = hb[t % len(hb)]; s_ = st[t % 2]; hT_ = hTt[t % 2]
                kb.dma('sp', x_[:], xs[t * 128:(t + 1) * 128, :], W=[x_])
                kb.op('act', lambda a: a.activation(out=junk[:], in_=x_[:], func=AF.Square, accum_out=s_[:, 0:1]), R=[x_], W=[junk, s_])
                kb.op('dve', lambda v: v.tensor_scalar(out=s_[:, 1:2], in0=s_[:, 0:1], scalar1=1.0 / D, scalar2=EPS, op0=ALU.mult, op1=ALU.add), R=[s_], W=[s_])
                kb.op('act', lambda a: a.activation(out=s_[:, 2:3], in_=s_[:, 1:2], func=AF.Sqrt), R=[s_], W=[s_])
                kb.op('dve', lambda v: v.reciprocal(out=s_[:, 3:4], in_=s_[:, 2:3]), R=[s_], W=[s_])
                kb.op('dve', lambda v: v.scalar_tensor_tensor(out=x_[:], in0=x_[:], scalar=s_[:, 3:4], in1=G[r][:], op0=ALU.mult, op1=ALU.mult), R=[x_, s_, G[r]], W=[x_])
                kb.op('pool', lambda v: v.tensor_tensor(out=h_[:], in0=x_[:], in1=S[r][:], op=ALU.add), R=[x_, S[r]], W=[h_])
                idn = ident_b[:] if which == 0 else ident_f[:, 0:128]
                idt = ident_b if which == 0 else cst_f
                for k0 in range(0, KC, NPT):
                    p_ = ptr[(k0 // NPT) % 2]
                    kb.wait('pe', R=[h_, idt], W=[p_])
                    for k in range(k0, min(KC, k0 + NPT)):
                        if which == 0:
                            ins = nc.tensor.transpose(out=p_[:, k - k0, :], in_=h_[:, k * 128:(k + 1) * 128], identity=idn)
                        else:
                            ins = nc.tensor.matmul(p_[:, k - k0, :], lhsT=h_[:, k * 128:(k + 1) * 128], rhs=idn, start=True, stop=True)
                    kb.done('pe', ins, R=[h_, idt], W=[p_])
                    n = min(KC, k0 + NPT) - k0
                    kb.op('act', lambda a: a.copy(out=hT_[:, k0:k0 + n, :], in_=p_[:, 0:n, :]), R=[p_], W=[hT_])
                    if which == 1 and 'b' not in os.environ.get("KX", ""):
                        hf_ = hTf[0]
                        kb.op('dve', lambda v: v.tensor_copy(out=hf_[:, k0:k0 + n, :], in_=p_[:, 0:n, :]), R=[p_], W=[hf_])
                kb.dma('pool', hT_d[:, :, t * 128:(t + 1) * 128], hT_[:], R=[hT_])
                if which == 1 and os.environ.get("KNOROUTE") != "2":
                    hf_ = hTf[0]; pl_ = pl[t % 2]
                    kb.wait('pe', R=[hf_, wr], W=[pl_])
                    for k in range(KC):
                        ins = nc.tensor.matmul(pl_[:], lhsT=hf_[:, k, :], rhs=wr[:, k, :], start=(k == 0), stop=(k == KC - 1))
                    kb.done('pe', ins, R=[hf_, wr], W=[pl_])
                    if os.environ.get("KNOROUTE"):
                        continue
                    aff = rt['aff']; bi = rt['bi']; sel = rt['sel']; m2 = rt['m2']; oh = rt['oh']; w_ = rt['w']
                    kb.op('act', lambda a: a.activation(out=aff[:], in_=pl_[:], func=AF.Sigmoid), R=[pl_], W=[aff])
                    kb.op('dve', lambda v: v.tensor_tensor(out=bi[:], in0=aff[:], in1=brr[:], op=ALU.add), R=[aff, brr], W=[bi])
                    big = bi[:].rearrange("p (g e) -> p g e", g=NG)
                    pi = 0
                    for a_ in range(EPG):
                        for b_ in range(a_ + 1, EPG):
                            kb.op('dve', lambda v: v.tensor_tensor(out=ps6[:, :, pi], in0=big[:, :, a_], in1=big[:, :, b_], op=ALU.add), R=[bi], W=[ps6])
                            pi += 1
                    kb.op('dve', lambda v: v.tensor_reduce(out=gs[:], in_=ps6[:], axis=AX.X, op=ALU.max), R=[ps6], W=[gs])
                    kb.op('dve', lambda v: v.tensor_reduce(out=gm1[:, 0:1], in_=gs[:], axis=AX.X, op=ALU.max), R=[gs], W=[gm1])
                    kb.op('dve', lambda v: v.tensor_scalar(out=goh[:], in0=gs[:], scalar1=gm1[:, 0:1], scalar2=None, op0=ALU.is_ge), R=[gs, gm1], W=[goh])
                    kb.op('dve', lambda v: v.tensor_tensor(out=m2[:].rearrange("p (g e) -> p g e", g=NG), in0=big, in1=goh[:].unsqueeze(2).to_broadcast([128, NG, EPG]), op=ALU.mult), R=[bi, goh], W=[m2])
                    kb.op('dve', lambda v: v.tensor_scalar(out=goh[:], in0=goh[:], scalar1=-1.0, scalar2=1e30, op0=ALU.add, op1=ALU.mult), R=[goh], W=[goh])
                    kb.op('dve', lambda v: v.tensor_tensor(out=m2[:].rearrange("p (g e) -> p g e", g=NG), in0=m2[:].rearrange("p (g e) -> p g e", g=NG), in1=goh[:].unsqueeze(2).to_broadcast([128, NG, EPG]), op=ALU.add), R=[m2, goh], W=[m2])
                    kb.op('dve', lambda v: v.tensor_reduce(out=gm1[:, 1:2], in_=m2[:], axis=AX.X, op=ALU.max), R=[m2], W=[gm1])
                    kb.op('dve', lambda v: v.tensor_scalar(out=sel[:], in0=m2[:], scalar1=gm1[:, 1:2], scalar2=None, op0=ALU.is_ge), R=[m2, gm1], W=[sel])
                    kb.op('dve', lambda v: v.scalar_tensor_tensor(out=m2[:], in0=sel[:], scalar=-1e30, in1=m2[:], op0=ALU.mult, op1=ALU.add), R=[sel, m2], W=[m2])
                    kb.op('dve', lambda v: v.tensor_reduce(out=gm1[:, 2:3], in_=m2[:], axis=AX.X, op=ALU.max), R=[m2], W=[gm1])
                    kb.op('dve', lambda v: v.tensor_scalar(out=oh[:], in0=m2[:], scalar1=gm1[:, 2:3], scalar2=None, op0=ALU.is_ge), R=[m2, gm1], W=[oh])
                    kb.op('dve', lambda v: v.tensor_tensor(out=sel[:], in0=sel[:], in1=oh[:], op=ALU.add), R=[sel, oh], W=[sel])
                    kb.op('dve', lambda v: v.tensor_tensor(out=w_[:], in0=sel[:], in1=aff[:], op=ALU.mult), R=[sel, aff], W=[w_])
                    kb.op('dve', lambda v: v.tensor_reduce(out=gm1[:, 3:4], in_=w_[:], axis=AX.X, op=ALU.add), R=[w_], W=[gm1])
                    kb.op('dve', lambda v: v.reciprocal(out=gm1[:, 3:4], in_=gm1[:, 3:4]), R=[gm1], W=[gm1])
                    kb.op('dve', lambda v: v.tensor_scalar(out=comb[:, t, :], in0=w_[:], scalar1=gm1[:, 3:4], scalar2=None, op0=ALU.mult), R=[w_, gm1], W=[comb])

    comb = psb([128, TT, NE], F32, "comb")

    def proj_phase(l):
        last = (l == DEPTH - 1)
        NTB = 512
        tm_groups = [("aq", aq_d, BF16), ("ak", ak_d, BF16), ("av", av_d, BF16), ("mv", mv_d, BF16), ("mo", mo_d, BF16), ("mg", mg_d, F32)]
        fm_groups = [("mqk", mqk_d, 0), ("ga", sga_d, 1), ("gm", sgm_d, 1)]
        with Ph() as ph:
            hTb = [ph.sb([128, KC, NTB], BF16, "hTb") for _ in range(1)]
            wtm = [ph.sb([128, KC, 512], BF16, "wtm") for _ in range(2)]
            brep = [ph.sb([128, 512], F32, "brep") for _ in range(2)]
            bcol = [ph.sb([128, 1], F32, "bcol") for _ in range(2)]
            pp = [ph.ps([128, 512], F32, "pp") for _ in range(4)]
            ot = [ph.sb([128, 512], BF16, "ot") for _ in range(3)]
            otf = [ph.sb([128, 512], F32, "otf") for _ in range(2)]
            wi = 0; pi = 0; oi = 0
            for tb0 in range(0, Tn, NTB):
                nt = min(NTB, Tn - tb0)
                hT_ = hTb[0]
                kb.dma('sp', hT_[:, :, 0:nt], hT_d[:, :, tb0:tb0 + nt], W=[hT_])
                for (gn, dst, dt) in tm_groups:
                    wsrc, c0, cw, BW = WS[l % NWS]['win_tm'][gn]
                    for n0 in range(0, cw, BW):
                        nw = BW
                        w_ = wtm[wi % 2]; br_ = brep[wi % 2]; wi += 1
                        kb.dma('sp', w_[:, :, 0:nw], wsrc[:, n0 // BW, :, :], W=[w_])
                        kb.dma('sp', br_[:, 0:nw], b_in[l:l + 1, c0 + n0:c0 + n0 + nw].to_broadcast([128, nw]), W=[br_])
                        cv_tick()
                        for tt in range(nt // 128):
                            p_ = pp[pi % 4]; pi += 1
                            kb.wait('pe', R=[hT_, w_], W=[p_])
                            for k in range(KC):
                                ins = nc.tensor.matmul(p_[:, 0:nw], lhsT=hT_[:, k, tt * 128:(tt + 1) * 128], rhs=w_[:, k, 0:nw], start=(k == 0), stop=(k == KC - 1))
                            kb.done('pe', ins, R=[hT_, w_], W=[p_])
                            if dt == BF16:
                                o_ = ot[oi % 3]; oi += 1
                            else:
                                o_ = otf[oi % 2]; oi += 1
                            kb.op('dve', lambda v: v.tensor_tensor(out=o_[:, 0:nw], in0=p_[:, 0:nw], in1=br_[:, 0:nw], op=ALU.add), R=[p_, br_], W=[o_])
                            kb.dma('pool', dst[tb0 + tt * 128:tb0 + (tt + 1) * 128, n0:n0 + nw], o_[:, 0:nw], R=[o_])
                for (gn, dst, sg) in fm_groups:
                    wsrc, c0, cw, BW = WS[l % NWS]['win_fm'][gn]
                    for n0 in range(0, cw, 128):
                        w_ = wtm[wi % 2]; bc_ = bcol[wi % 2]; wi += 1
                        kb.dma('sp', w_[:, :, 0:128], wsrc[:, n0 // 128, :, :], W=[w_])
                        kb.dma('sp', bc_[:], b_in[l, c0 + n0:c0 + n0 + 128].rearrange("(p o) -> p o", o=1), W=[bc_])
                        cv_tick()
                        p_ = pp[pi % 4]; pi += 1
                        kb.wait('pe', R=[hT_, w_], W=[p_])
                        for k in range(KC):
                            ins = nc.tensor.matmul(p_[:, 0:nt], lhsT=w_[:, k, 0:128], rhs=hT_[:, k, 0:nt], start=(k == 0), stop=(k == KC - 1))
                        kb.done('pe', ins, R=[hT_, w_], W=[p_])
                        o_ = ot[oi % 3]; oi += 1
                        kb.op('act', lambda a: a.activation(out=o_[:, 0:nt], in_=p_[:, 0:nt], func=(AF.Sigmoid if sg else AF.Identity), bias=bc_[:, 0:1], scale=1.0), R=[p_, bc_], W=[o_])
                        kb.dma('pool', dst[n0:n0 + 128, tb0:tb0 + nt], o_[:, 0:nt], R=[o_])

    def conv_phase(l):
        with Ph() as ph:
            xi = [ph.sb([128, Tn], BF16, "cxi") for _ in range(2)]
            ya = [ph.sb([128, Tn], F32, "cya") for _ in range(2)]
            yo = [ph.sb([128, Tn], BF16, "cyo") for _ in range(2)]
            cw = [ph.sb([128, 4], F32, "ccw") for _ in range(2)]
            for i, c0 in enumerate(range(0, 2 * QKW, 128)):
                x_ = xi[i % 2]; y_ = ya[i % 2]; o_ = yo[i % 2]; w_ = cw[i % 2]
                kb.dma('sp', x_[:], mqk_d[c0:c0 + 128, :], W=[x_])
                kb.dma('sp', w_[:, 0:3], conv_w[l, :, c0:c0 + 128].rearrange("j p -> p j"), W=[w_])
                kb.dma('sp', w_[:, 3:4], conv_b[l, c0:c0 + 128].rearrange("(p o) -> p o", o=1), W=[w_])
                kb.op('dve', lambda v: v.tensor_scalar(out=y_[:], in0=x_[:], scalar1=w_[:, 1:2], scalar2=w_[:, 3:4], op0=ALU.mult, op1=ALU.add), R=[x_, w_], W=[y_])
                for (s0, s1) in [(0, CTX), (CTX, Tn)]:
                    kb.op('dve', lambda v: v.scalar_tensor_tensor(out=y_[:, s0 + 1:s1], in0=x_[:, s0:s1 - 1], scalar=w_[:, 0:1], in1=y_[:, s0 + 1:s1], op0=ALU.mult, op1=ALU.add), R=[x_, w_, y_], W=[y_])
                    kb.op('dve', lambda v: v.scalar_tensor_tensor(out=y_[:, s0:s1 - 1], in0=x_[:, s0 + 1:s1], scalar=w_[:, 2:3], in1=y_[:, s0:s1 - 1], op0=ALU.mult, op1=ALU.add), R=[x_, w_, y_], W=[y_])
                sc = 1.0 if c0 < QKW else DK ** -0.5
                kb.op('act', lambda a: a.activation(out=o_[:], in_=y_[:], func=AF.Sigmoid), R=[y_], W=[o_])
                kb.op('dve', lambda v: v.scalar_tensor_tensor(out=o_[:], in0=y_[:], scalar=sc, in1=o_[:], op0=ALU.mult, op1=ALU.mult), R=[y_, o_], W=[o_])
                kb.dma('pool', mqkc_d[c0:c0 + 128, :], o_[:], R=[o_])

    def qk_phase(l):
        last = (l == DEPTH - 1)
        with Ph() as ph:
            gq = ph.sb([128, HD], F32, "gq"); gk = ph.sb([128, HD], F32, "gk")
            load_rep(gq, g_q[l:l + 1, :], HD); load_rep(gk, g_k[l:l + 1, :], HD)
            NQ = NH + NKV
            qi = [ph.sb([128, NQ, HD], BF16, "qi") for _ in range(2)]
            sq = ph.sb([128, NQ, HD], F32, "sq")
            xn = ph.sb([128, NQ, HD], F32, "xn")
            tmp = ph.sb([128, NQ, 2, 32], F32, "tmp"); tmp2 = ph.sb([128, NQ, 2, 32], F32, "tmp2")
            qo = [ph.sb([128, NQ, HD], BF16, "qo") for _ in range(2)]
            st = [ph.sb([128, 4, NQ], F32, "st") for _ in range(2)]
            cs = [ph.sb([128, 128], F32, "cs") for _ in range(2)]
            ptr = [ph.ps([128, 8, 128], BF16, "ptr") for _ in range(2)]
            oT = [ph.sb([128, NQ, 128], BF16, "oT") for _ in range(2)]
            for t in range(TT):
                q_ = qi[t % 2]; s_ = st[t % 2]; c_ = cs[t % 2]; o_ = qo[t % 2]; oT_ = oT[t % 2]
                kb.dma('sp', q_[:, 0:NH, :], aq_d[t * 128:(t + 1) * 128, :].rearrange("p (h d) -> p h d", d=HD), W=[q_])
                kb.dma('sp', q_[:, NH:NQ, :], ak_d[t * 128:(t + 1) * 128, :].rearrange("p (h d) -> p h d", d=HD), W=[q_])
                kb.op('pool', lambda v: v.tensor_tensor(out=sq[:], in0=q_[:], in1=q_[:], op=ALU.mult), R=[q_], W=[sq])
                kb.op('dve', lambda v: v.tensor_reduce(out=s_[:, 0, :], in_=sq[:], axis=AX.X, op=ALU.add), R=[sq], W=[s_])
                kb.op('dve', lambda v: v.tensor_scalar(out=s_[:, 1, :], in0=s_[:, 0, :], scalar1=1.0 / HD, scalar2=EPS, op0=ALU.mult, op1=ALU.add), R=[s_], W=[s_])
                kb.op('act', lambda a: a.activation(out=s_[:, 2, :], in_=s_[:, 1, :], func=AF.Sqrt), R=[s_], W=[s_])
                kb.op('dve', lambda v: v.reciprocal(out=s_[:, 3, :], in_=s_[:, 2, :]), R=[s_], W=[s_])
                kb.op('dve', lambda v: v.tensor_tensor(out=xn[:], in0=q_[:], in1=s_[:, 3, :].unsqueeze(2).to_broadcast([128, NQ, HD]), op=ALU.mult), R=[q_, s_], W=[xn])
                lat = t >= CT
                dstq = o_ if not lat else xn
                kb.op('dve', lambda v: v.tensor_tensor(out=dstq[:, 0:NH, :], in0=xn[:, 0:NH, :], in1=gq[:].unsqueeze(1).to_broadcast([128, NH, HD]), op=ALU.mult), R=[xn, gq], W=[dstq])
                kb.op('dve', lambda v: v.tensor_tensor(out=dstq[:, NH:NQ, :], in0=xn[:, NH:NQ, :], in1=gk[:].unsqueeze(1).to_broadcast([128, NKV, HD]), op=ALU.mult), R=[xn, gk], W=[dstq])
                if lat:
                    kb.dma('sp', c_[:], rope_cs[(t - CT) * 128:(t - CT + 1) * 128, :], W=[c_])
                    xv = xn[:].rearrange("p h (a b c) -> p h a b c", a=2, b=2)
                    ov = o_[:].rearrange("p h (a b c) -> p h a b c", a=2, b=2)
                    x1 = xv[:, :, :, 0, :]; x2 = xv[:, :, :, 1, :]
                    COS = c_[:, 0:64].rearrange("p (a c) -> p a c", a=2).unsqueeze(1).to_broadcast([128, NQ, 2, 32])
                    SIN = c_[:, 64:128].rearrange("p (a c) -> p a c", a=2).unsqueeze(1).to_broadcast([128, NQ, 2, 32])
                    kb.op('dve', lambda v: v.tensor_tensor(out=tmp[:], in0=x1, in1=COS, op=ALU.mult), R=[xn, c_], W=[tmp])
                    kb.op('pool', lambda v: v.tensor_tensor(out=tmp2[:], in0=x2, in1=SIN, op=ALU.mult), R=[xn, c_], W=[tmp2])
                    kb.op('dve', lambda v: v.tensor_tensor(out=ov[:, :, :, 0, :], in0=tmp[:], in1=tmp2[:], op=ALU.subtract), R=[tmp, tmp2], W=[o_])
                    kb.op('dve', lambda v: v.tensor_tensor(out=tmp[:], in0=x1, in1=SIN, op=ALU.mult), R=[xn, c_], W=[tmp])
                    kb.op('pool', lambda v: v.tensor_tensor(out=tmp2[:], in0=x2, in1=COS, op=ALU.mult), R=[xn, c_], W=[tmp2])
                    kb.op('dve', lambda v: v.tensor_tensor(out=ov[:, :, :, 1, :], in0=tmp[:], in1=tmp2[:], op=ALU.add), R=[tmp, tmp2], W=[o_])
                for k0 in range(0, NQ, 8):
                    p_ = ptr[(k0 // 8) % 2]
                    n = min(NQ, k0 + 8) - k0
                    kb.wait('pe', R=[o_, ident_b], W=[p_])
                    for k in range(k0, k0 + n):
                        ins = nc.tensor.transpose(out=p_[:, k - k0, :], in_=o_[:, k, :], identity=ident_b[:])
                    kb.done('pe', ins, R=[o_, ident_b], W=[p_])
                    kb.op('act', lambda a: a.copy(out=oT_[:, k0:k0 + n, :], in_=p_[:, 0:n, :]), R=[p_], W=[oT_])
                cv_tick(2)
                kb.dma('pool', qT_d[:, :, t * 128:(t + 1) * 128], oT_[:, 0:NH, :], R=[oT_])
                kb.dma('pool', kT_d[:, :, t * 128:(t + 1) * 128], oT_[:, NH:NQ, :], R=[oT_])

    def attn_phase(l):
        last = (l == DEPTH - 1)
        scale = HD ** -0.5
        with Ph() as ph:
            ES = ph.sb([128, NH], F32, "ES")
            load_rep(ES, sink[l:l + 1, :], NH)
            kb.op('act', lambda a: a.activation(out=ES[:], in_=ES[:], func=AF.Exp), R=[ES], W=[ES])
            kc = ph.sb([128, NKV, CTX], BF16, "kc")
            kb.dma('sp', kc[:], kT_d[:, :, 0:CTX], W=[kc])
            vc = ph.sb([128, CT, KW], BF16, "vc")
            kb.dma('sp', vc[:], av_d[0:CTX, :].rearrange("(c p) n -> p c n", p=128), W=[vc])
            qb = [ph.sb([128, NH, 128], BF16, "qb") for _ in range(2)]
            kw = [ph.sb([128, NKV, 384], BF16, "kw") for _ in range(2)]
            vw = [ph.sb([128, 3, KW], BF16, "vw") for _ in range(2)]
            pS = [ph.ps([128, 512], F32, "pS") for _ in range(2)]
            pN = [ph.ps([128, 512], F32, "pN") for _ in range(2)]
            pD = [ph.ps([128, 512], F32, "pD") for _ in range(2)]
            sm = [ph.sb([128, 512], F32, "sm") for _ in range(2)]
            pT = [ph.sb([128, 512], BF16, "pT") for _ in range(3)]
            dn = [ph.sb([128, 512], F32, "dn") for _ in range(2)]
            ob = [ph.sb([128, NH, 128], BF16, "ob") for _ in range(2)]
            blocks = ([] if last else [('c', c) for c in range(CT)]) + [('l', n) for n in range(NB)]
            si = 0; pi = 0
            for bi_, (kind, n) in enumerate(blocks):
                q_ = qb[bi_ % 2]; k_ = kw[bi_ % 2]; v_ = vw[bi_ % 2]; o_ = ob[bi_ % 2]
                tok0 = n * 128 if kind == 'c' else CTX + n * 128
                kb.dma('sp', q_[:], qT_d[:, :, tok0:tok0 + 128], W=[q_])
                chunks = []
                if kind == 'l':
                    lo = max(0, n - 1); hi = min(NB - 1, n + 1)
                    kb.dma('sp', k_[:, :, (lo - n + 1) * 128:(hi - n + 2) * 128], kT_d[:, :, CTX + lo * 128:CTX + (hi + 1) * 128], W=[k_])
                    kb.dma('sp', v_[:, lo - n + 1:hi - n + 2, :], av_d[CTX + lo * 128:CTX + (hi + 1) * 128, :].rearrange("(c p) n -> p c n", p=128), W=[v_])
                    for j in range(lo - n + 1, hi - n + 2):
                        chunks.append((k_, j, v_, j, j))
                for c in range(CT):
                    chunks.append((kc, c, vc, c, None))
                for g in range(NKV):
                    pN_ = pN[g % 2]; pD_ = pD[g % 2]
                    qg = q_[:, g * GRP:(g + 1) * GRP, :].rearrange("p h q -> p (h q)")
                    NQC = GRP * 128
                    for ci, (kt, kj, vt, vj, mj) in enumerate(chunks):
                        pS_ = pS[si % 2]; si += 1
                        kb.wait('pe', R=[kt, q_], W=[pS_])
                        ins = nc.tensor.matmul(pS_[:, 0:NQC], lhsT=kt[:, g, kj * 128:(kj + 1) * 128], rhs=qg, start=True, stop=True)
                        kb.done('pe', ins, R=[kt, q_], W=[pS_])
                        p_ = pT[pi % 3]; pi += 1
                        if mj is not None:
                            s_ = sm[ci % 2]
                            kb.op('dve', lambda v: v.tensor_tensor(out=s_[:, 0:NQC].rearrange("p (h q) -> p h q", h=GRP), in0=pS_[:, 0:NQC].rearrange("p (h q) -> p h q", h=GRP), in1=maskT[:, mj * 128:(mj + 1) * 128].unsqueeze(1).to_broadcast([128, GRP, 128]), op=ALU.add), R=[pS_, maskT], W=[s_])
                            kb.op('act', lambda a: a.activation(out=p_[:, 0:NQC], in_=s_[:, 0:NQC], func=AF.Exp, scale=scale), R=[s_], W=[p_])
                        else:
                            kb.op('act', lambda a: a.activation(out=p_[:, 0:NQC], in_=pS_[:, 0:NQC], func=AF.Exp, scale=scale), R=[pS_], W=[p_])
                        first = (ci == 0); lastc = (ci == len(chunks) - 1)
                        kb.wait('pe', R=[p_, vt, ones_b], W=[pN_, pD_] if first else [])
                        nc.tensor.matmul(pN_[:, 0:NQC], lhsT=vt[:, vj, g * HD:(g + 1) * HD], rhs=p_[:, 0:NQC], start=first, stop=lastc)
                        ins = nc.tensor.matmul(pD_[:, 0:NQC], lhsT=ones_b[:, :], rhs=p_[:, 0:NQC], start=first, stop=lastc)
                        kb.done('pe', ins, R=[p_, vt, ones_b], W=[pN_, pD_] if lastc else [])
                    d_ = dn[g % 2]
                    kb.op('dve', lambda v: v.tensor_tensor(out=d_[:, 0:NQC].rearrange("p (h q) -> p h q", h=GRP), in0=pD_[:, 0:NQC].rearrange("p (h q) -> p h q", h=GRP), in1=ES[:, g * GRP:(g + 1) * GRP].unsqueeze(2).to_broadcast([128, GRP, 128]), op=ALU.add), R=[pD_, ES], W=[d_])
                    kb.op('dve', lambda v: v.reciprocal(out=d_[:, 0:NQC], in_=d_[:, 0:NQC]), R=[d_], W=[d_])
                    kb.op('dve', lambda v: v.tensor_tensor(out=o_[:, g * GRP:(g + 1) * GRP, :].rearrange("p h q -> p (h q)"), in0=pN_[:, 0:NQC], in1=d_[:, 0:NQC], op=ALU.mult), R=[pN_, d_], W=[o_])
                cv_tick(2)
                kb.dma('pool', atT_d[:, :, tok0:tok0 + 128], o_[:], R=[o_])

    def mlstm_phase(l):
        last = (l == DEPTH - 1)
        NCH = TT
        M2 = 2 * MH
        order_f = list(range(NCH))
        order_b = list(range(CT - 1, -1, -1)) + list(range(NCH - 1, CT - 1, -1))
        with Ph() as ph:
            CS = ph.sb([128, NCH, M2], F32, "CS"); IA = ph.sb([128, NCH, M2], F32, "IA")
            EA = ph.sb([128, NCH, M2], F32, "EA"); WA = ph.sb([128, NCH, M2], F32, "WA")
            Um = [ph.sb([MH, NCH], F32, "Um") for _ in range(2)]; Be = [ph.sb([MH, NCH], F32, "Be") for _ in range(2)]
            Mm = [ph.sb([MH, NCH], F32, "Mm") for _ in range(2)]; Mp = [ph.sb([MH, NCH + 1], F32, "Mp") for _ in range(2)]
            Aa = [ph.sb([MH, NCH], F32, "Aa") for _ in range(2)]
            Mb = ph.sb([128, M2, NCH], F32, "Mb"); Ab = ph.sb([128, M2, NCH], F32, "Ab")
            with ExitStack() as es2:
                def sb2(shape, dt=F32, nm="g"):
                    return T(es2.enter_context(nc.sbuf_tensor(kb.name(nm), list(shape), dt)))
                def ps2(shape, dt=F32, nm="gp"):
                    return TP(es2.enter_context(nc.psum_tensor(kb.name(nm), [128, 512], F32)), list(shape), dt)
                gt = [sb2([128, NGC], F32, "gt") for _ in range(2)]
                spl = [sb2([128, M2], F32, "spl") for _ in range(2)]
                uu = [sb2([128, M2], F32, "uu") for _ in range(2)]
                pc = [ps2([128, M2], F32, "pc") for _ in range(2)]
                pu = [ps2([MH, 2, 128], F32, "pu") for _ in range(2)]
                pb = [ps2([MH, 2], F32, "pb") for _ in range(2)]
                for t in range(NCH):
                    g_ = gt[t % 2]; s_ = spl[t % 2]; u_ = uu[t % 2]; pc_ = pc[t % 2]; pu_ = pu[t % 2]; pb_ = pb[t % 2]
                    kb.dma('sp', g_[:], mg_d[t * 128:(t + 1) * 128, :], W=[g_])
                    kb.op('act', lambda a: a.activation(out=s_[:], in_=g_[:, M2:2 * M2], func=AF.Exp, scale=-1.0), R=[g_], W=[s_])
                    kb.op('act', lambda a: a.activation(out=s_[:], in_=s_[:], func=AF.Ln, bias=1.0, scale=1.0), R=[s_], W=[s_])
                    kb.wait('pe', R=[s_, tri_f, tri_b], W=[pc_])
                    nc.tensor.matmul(pc_[:, 0:MH], lhsT=tri_f[:], rhs=s_[:, 0:MH], start=True, stop=True)
                    ins = nc.tensor.matmul(pc_[:, MH:M2], lhsT=tri_b[:], rhs=s_[:, MH:M2], start=True, stop=True)
                    kb.done('pe', ins, R=[s_, tri_f, tri_b], W=[pc_])
                    kb.op('dve', lambda v: v.tensor_copy(out=CS[:, t, :], in_=pc_[:]), R=[pc_], W=[CS])
                    kb.op('dve', lambda v: v.tensor_copy(out=IA[:, t, :], in_=g_[:, 0:M2]), R=[g_], W=[IA])
                    kb.op('dve', lambda v: v.tensor_tensor(out=u_[:], in0=g_[:, 0:M2], in1=pc_[:], op=ALU.add), R=[g_, pc_], W=[u_])
                    kb.wait('pe', R=[u_, s_, cst_f, ones_f], W=[pu_, pb_])
                    for d in range(2):
                        nc.tensor.matmul(pu_[:, d, :], lhsT=u_[:, d * MH:(d + 1) * MH], rhs=ident_f[:, 0:128], start=True, stop=True)
                    for d in range(2):
                        ins = nc.tensor.matmul(pb_[:, d:d + 1], lhsT=s_[:, d * MH:(d + 1) * MH], rhs=ones_f[:, 0:1], start=True, stop=True)
                    kb.done('pe', ins, R=[u_, s_, cst_f, ones_f], W=[pu_, pb_])
                    for d in range(2):
                        kb.op('dve', lambda v: v.tensor_reduce(out=Um[d][:, t:t + 1], in_=pu_[:, d, :], axis=AX.X, op=ALU.max), R=[pu_], W=[Um[d]])
                        kb.op('dve', lambda v: v.tensor_scalar(out=Be[d][:, t:t + 1], in0=pb_[:, d:d + 1], scalar1=-1.0, scalar2=None, op0=ALU.mult), R=[pb_], W=[Be[d]])
                for d, order in enumerate([order_f, order_b]):
                    kb.op('dve', lambda v: v.memset(Mp[d][:], 0.0), W=[Mp[d]])
                    for k, c in enumerate(order):
                        kb.op('dve', lambda v: v.tensor_tensor(out=Mm[d][:, c:c + 1], in0=Mp[d][:, k:k + 1], in1=Um[d][:, c:c + 1], op=ALU.max), R=[Mp[d], Um[d]], W=[Mm[d]])
                        kb.op('dve', lambda v: v.tensor_tensor(out=Aa[d][:, c:c + 1], in0=Mp[d][:, k:k + 1], in1=Mm[d][:, c:c + 1], op=ALU.subtract), R=[Mp[d], Mm[d]], W=[Aa[d]])
                        kb.op('dve', lambda v: v.tensor_tensor(out=Mp[d][:, k + 1:k + 2], in0=Be[d][:, c:c + 1], in1=Mm[d][:, c:c + 1], op=ALU.add), R=[Be[d], Mm[d]], W=[Mp[d]])
                    kb.op('act', lambda a: a.activation(out=Aa[d][:], in_=Aa[d][:], func=AF.Exp), R=[Aa[d]], W=[Aa[d]])
                X = sb2([MH, MH, NCH], F32, "X")
                pbc = ps2([128, 512], F32, "pbc")
                HPB = max(1, 512 // NCH)
                for d in range(2):
                    for (src, dstb) in [(Mm[d], Mb), (Aa[d], Ab)]:
                        kb.op('dve', lambda v: v.tensor_tensor(out=X[:], in0=ident_f[0:MH, 0:MH].unsqueeze(2).to_broadcast([MH, MH, NCH]), in1=src[:].unsqueeze(1).to_broadcast([MH, MH, NCH]), op=ALU.mult), R=[cst_f, src], W=[X])
                        for r0 in range(0, MH, HPB):
                            nr = min(HPB, MH - r0)
                            kb.wait('pe', R=[X, ones_f], W=[pbc])
                            ins = nc.tensor.matmul(pbc[:, 0:nr * NCH], lhsT=ones_f[0:MH, :], rhs=X[:, r0:r0 + nr, :].rearrange("q r c -> q (r c)"), start=True, stop=True)
                            kb.done('pe', ins, R=[X, ones_f], W=[pbc])
                            kb.op('dve', lambda v: v.tensor_copy(out=dstb[:, d * MH + r0:d * MH + r0 + nr, :].rearrange("p r c -> p (r c)"), in_=pbc[:, 0:nr * NCH]), R=[pbc], W=[dstb])
                kb.op('dve', lambda v: v.tensor_tensor(out=EA[:], in0=CS[:], in1=Mb[:].rearrange("p r c -> p c r"), op=ALU.subtract), R=[CS, Mb], W=[EA])
                kb.op('act', lambda a: a.activation(out=EA[:], in_=EA[:], func=AF.Exp), R=[EA], W=[EA])
                kb.op('act', lambda a: a.activation(out=WA[:], in_=IA[:], func=AF.Exp), R=[IA], W=[WA])
                kb.op('dve', lambda v: v.tensor_tensor(out=WA[:], in0=WA[:], in1=EA[:], op=ALU.mult), R=[WA, EA], W=[WA])
                kb.barrier()
            qT = [ph.sb([128, MH, 128], BF16, "mq") for _ in range(2)]
            kT = [ph.sb([128, MH, 128], BF16, "mk") for _ in range(2)]
            va = [ph.sb([128, MH, DV + 1], BF16, "va") for _ in range(2)]
            for v_ in va:
                kb.op('dve', lambda v: v.memset(v_[:], 1.0), W=[v_])
            Cn = [ph.sb([128, DV + 1], F32, "Cn") for _ in range(MH)]
            Cb = [ph.sb([128, DV + 1], BF16, "Cb") for _ in range(MH)]
            pk = [ph.ps([128, 128], BF16, "pk") for _ in range(2)]
            pst = [ph.ps([128, 128], F32, "pst") for _ in range(2)]
            pnum = [ph.ps([128, DV + 1], F32, "pnum") for _ in range(2)]
            pup = [ph.ps([128, DV + 1], F32, "pup") for _ in range(2)]
            ksc = [ph.sb([128, 128], BF16, "ksc") for _ in range(2)]
            Sp = [ph.sb([128, 128], BF16, "Sp") for _ in range(2)]
            dd = [ph.sb([128, 2], F32, "dd") for _ in range(2)]
            Ho = [ph.sb([128, MH, DV], BF16, "Ho") for _ in range(2)]
            it = 0
            for d, order in enumerate([order_f, order_b]):
                tri = tri_fb if d == 0 else tri_bb
                hdst = hf_d if d == 0 else hb_d
                for h in range(MH):
                    kb.op('dve', lambda v: v.memset(Cn[h][:], 0.0), W=[Cn[h]])
                for k, c in enumerate(order):
                    q_ = qT[k % 2]; k_ = kT[k % 2]; v_ = va[k % 2]; H_ = Ho[k % 2]
                    kb.dma('sp', q_[:], mqkc_d[0:QKW, c * 128:(c + 1) * 128].rearrange("(h d) t -> d h t", d=DK), W=[q_])
                    kb.dma('sp', k_[:], mqkc_d[QKW:2 * QKW, c * 128:(c + 1) * 128].rearrange("(h d) t -> d h t", d=DK), W=[k_])
                    kb.dma('sp', v_[:, :, 0:DV], mv_d[c * 128:(c + 1) * 128, :].rearrange("p (h v) -> p h v", v=DV), W=[v_])
                    skip_out = last and c < CT
                    for h in range(MH):
                        r = d * MH + h
                        i2 = it % 2; it += 1
                        kb.op('dve', lambda v: v.tensor_scalar(out=Cn[h][:], in0=Cn[h][:], scalar1=Ab[:, r, c:c + 1], scalar2=None, op0=ALU.mult), R=[Cn[h], Ab], W=[Cn[h]])
                        kb.op('pool', lambda v: v.tensor_copy(out=Cb[h][:], in_=Cn[h][:]), R=[Cn[h]], W=[Cb[h]])
                        kb.wait('pe', R=[k_, ident_b], W=[pk[i2]])
                        ins = nc.tensor.transpose(out=pk[i2][:], in_=k_[:, h, :], identity=ident_b[:])
                        kb.done('pe', ins, R=[k_, ident_b], W=[pk[i2]])
                        kb.op('dve', lambda v: v.tensor_scalar(out=ksc[i2][:], in0=pk[i2][:], scalar1=WA[:, c, r:r + 1], scalar2=None, op0=ALU.mult), R=[pk[i2], WA], W=[ksc[i2]])
                        if not skip_out:
                            kb.wait('pe', R=[k_, q_], W=[pst[i2]])
                            ins = nc.tensor.matmul(pst[i2][:], lhsT=k_[:, h, :], rhs=q_[:, h, :], start=True, stop=True)
                            kb.done('pe', ins, R=[k_, q_], W=[pst[i2]])
                            kb.op('dve', lambda v: v.scalar_tensor_tensor(out=Sp[i2][:], in0=pst[i2][:], scalar=WA[:, c, r:r + 1], in1=tri[:], op0=ALU.mult, op1=ALU.mult), R=[pst[i2], WA, tri], W=[Sp[i2]])
                            kb.wait('pe', R=[Sp[i2], v_, q_, Cb[h]], W=[pnum[i2]])
                            nc.tensor.matmul(pnum[i2][:], lhsT=Sp[i2][:], rhs=v_[:, h, :], start=True, stop=False)
                            ins = nc.tensor.matmul(pnum[i2][:], lhsT=q_[:, h, :], rhs=Cb[h][:], start=False, stop=True)
                            kb.done('pe', ins, R=[Sp[i2], v_, q_, Cb[h]], W=[pnum[i2]])
                            kb.op('dve', lambda v: v.tensor_scalar(out=dd[i2][:, 0:1], in0=pnum[i2][:, DV:DV + 1], scalar1=-1.0, scalar2=None, op0=ALU.mult), R=[pnum[i2]], W=[dd[i2]])
                            kb.op('dve', lambda v: v.tensor_tensor(out=dd[i2][:, 0:1], in0=dd[i2][:, 0:1], in1=pnum[i2][:, DV:DV + 1], op=ALU.max), R=[pnum[i2], dd[i2]], W=[dd[i2]])
                            kb.op('dve', lambda v: v.tensor_tensor(out=dd[i2][:, 0:1], in0=dd[i2][:, 0:1], in1=EA[:, c, r:r + 1], op=ALU.max), R=[dd[i2], EA], W=[dd[i2]])
                            kb.op('dve', lambda v: v.reciprocal(out=dd[i2][:, 1:2], in_=dd[i2][:, 0:1]), R=[dd[i2]], W=[dd[i2]])
                            kb.op('act', lambda a: a.activation(out=H_[:, h, :], in_=pnum[i2][:, 0:DV], func=AF.Copy, scale=dd[i2][:, 1:2]), R=[pnum[i2], dd[i2]], W=[H_])
                        kb.wait('pe', R=[ksc[i2], v_], W=[pup[i2]])
                        ins = nc.tensor.matmul(pup[i2][:], lhsT=ksc[i2][:], rhs=v_[:, h, :], start=True, stop=True)
                        kb.done('pe', ins, R=[ksc[i2], v_], W=[pup[i2]])
                        kb.op('dve', lambda v: v.tensor_tensor(out=Cn[h][:], in0=Cn[h][:], in1=pup[i2][:], op=ALU.add), R=[Cn[h], pup[i2]], W=[Cn[h]])
                    if not skip_out:
                        kb.dma('pool', hdst[c * 128:(c + 1) * 128, :].rearrange("p (h v) -> p h v", v=DV), H_[:], R=[H_])

    def hm_phase(l):
        last = (l == DEPTH - 1)
        MC = MW // 128
        with Ph() as ph:
            gm = ph.sb([128, MW], F32, "gmh")
            load_rep(gm, g_mh[l:l + 1, :], MW)
            hf = [ph.sb([128, MW], BF16, "hf") for _ in range(2)]; hb = [ph.sb([128, MW], BF16, "hb") for _ in range(2)]
            mo = [ph.sb([128, MW], BF16, "mo") for _ in range(2)]
            sg = ph.sb([128, MW], F32, "sg"); hs = ph.sb([128, MW], F32, "hs"); sq = ph.sb([128, MW], F32, "sq")
            ho = [ph.sb([128, MW], BF16, "ho") for _ in range(2)]
            st = [ph.sb([128, 4, MH], F32, "st") for _ in range(2)]
            ptr = [ph.ps([128, 8, 128], BF16, "ptr") for _ in range(2)]
            oT = [ph.sb([128, MC, 128], BF16, "oT") for _ in range(2)]
            for t in range(CT if last else 0, TT):
                f_ = hf[t % 2]; b_ = hb[t % 2]; m_ = mo[t % 2]; o_ = ho[t % 2]; s_ = st[t % 2]; oT_ = oT[t % 2]
                kb.dma('sp', f_[:], hf_d[t * 128:(t + 1) * 128, :], W=[f_])
                kb.dma('sp', b_[:], hb_d[t * 128:(t + 1) * 128, :], W=[b_])
                kb.dma('sp', m_[:], mo_d[t * 128:(t + 1) * 128, :], W=[m_])
                kb.op('act', lambda a: a.activation(out=sg[:], in_=m_[:], func=AF.Sigmoid), R=[m_], W=[sg])
                kb.op('pool', lambda v: v.tensor_tensor(out=hs[:], in0=f_[:], in1=b_[:], op=ALU.add), R=[f_, b_], W=[hs])
                kb.op('dve', lambda v: v.tensor_tensor(out=hs[:], in0=hs[:], in1=sg[:], op=ALU.mult), R=[hs, sg], W=[hs])
                kb.op('pool', lambda v: v.tensor_tensor(out=sq[:], in0=hs[:], in1=hs[:], op=ALU.mult), R=[hs], W=[sq])
                kb.op('dve', lambda v: v.tensor_reduce(out=s_[:, 0, :], in_=sq[:].rearrange("p (h v) -> p h v", v=DV), axis=AX.X, op=ALU.add), R=[sq], W=[s_])
                kb.op('dve', lambda v: v.tensor_scalar(out=s_[:, 1, :], in0=s_[:, 0, :], scalar1=1.0 / DV, scalar2=EPS, op0=ALU.mult, op1=ALU.add), R=[s_], W=[s_])
                kb.op('act', lambda a: a.activation(out=s_[:, 2, :], in_=s_[:, 1, :], func=AF.Sqrt), R=[s_], W=[s_])
                kb.op('dve', lambda v: v.reciprocal(out=s_[:, 3, :], in_=s_[:, 2, :]), R=[s_], W=[s_])
                kb.op('dve', lambda v: v.tensor_tensor(out=hs[:].rearrange("p (h v) -> p h v", v=DV), in0=hs[:].rearrange("p (h v) -> p h v", v=DV), in1=s_[:, 3, :].unsqueeze(2).to_broadcast([128, MH, DV]), op=ALU.mult), R=[hs, s_], W=[hs])
                kb.op('dve', lambda v: v.tensor_tensor(out=o_[:], in0=hs[:], in1=gm[:], op=ALU.mult), R=[hs, gm], W=[o_])
                for k0 in range(0, MC, 8):
                    p_ = ptr[(k0 // 8) % 2]
                    n = min(MC, k0 + 8) - k0
                    kb.wait('pe', R=[o_, ident_b], W=[p_])
                    for k in range(k0, k0 + n):
                        ins = nc.tensor.transpose(out=p_[:, k - k0, :], in_=o_[:, k * 128:(k + 1) * 128], identity=ident_b[:])
                    kb.done('pe', ins, R=[o_, ident_b], W=[p_])
                    kb.op('act', lambda a: a.copy(out=oT_[:, k0:k0 + n, :], in_=p_[:, 0:n, :]), R=[p_], W=[oT_])
                cv_tick(1)
                kb.dma('pool', hmT_d[:, :, t * 128:(t + 1) * 128], oT_[:], R=[oT_])

    def merge_phase(l):
        last = (l == DEPTH - 1)
        NTB = 512
        AC = AW // 128; MC = MW // 128
        with Ph() as ph:
            gts = [ph.sb([128, 512], F32, "gts") for _ in range(3)]
            aT = ph.sb([128, AC, NTB], BF16, "aT"); mT = ph.sb([128, MC, NTB], BF16, "mT")
            wa = [ph.sb([128, AC, 128], BF16, "wa") for _ in range(2)]; wm = [ph.sb([128, MC, 128], BF16, "wm") for _ in range(2)]
            ga = [ph.sb([128, NTB], BF16, "ga") for _ in range(2)]; gmm = [ph.sb([128, NTB], BF16, "gmm") for _ in range(2)]
            pa = [ph.ps([128, NTB], F32, "pa") for _ in range(2)]; pm = [ph.ps([128, NTB], F32, "pm") for _ in range(2)]
            t1 = [ph.sb([128, NTB], F32, "t1") for _ in range(2)]
            mg = ph.sb([128, KC, NTB], BF16, "mg")
            wo = [ph.sb([128, KC, 512], BF16, "wo") for _ in range(2)]
            po = [ph.ps([128, 512], F32, "po") for _ in range(2)]
            xt = [ph.sb([128, 512], F32, "xt") for _ in range(3)]
            wi = 0; xi = 0; pi = 0
            tbs = list(range(0, Tn, NTB))
            for tb0 in tbs:
                nt = min(NTB, Tn - tb0)
                kb.dma('sp', aT[:, :, 0:nt], atT_d[:, :, tb0:tb0 + nt], W=[aT])
                kb.dma('sp', mT[:, :, 0:nt], hmT_d[:, :, tb0:tb0 + nt], W=[mT])
                for j in range(KC):
                    a_ = wa[j % 2]; m_ = wm[j % 2]; ga_ = ga[j % 2]; gm_ = gmm[j % 2]; pa_ = pa[j % 2]; pm_ = pm[j % 2]; t_ = t1[j % 2]
                    kb.dma('sp', a_[:], WS[l % NWS]['wba_b'][:, j, :, :], W=[a_])
                    kb.dma('sp', m_[:], WS[l % NWS]['wbm_b'][:, j, :, :], W=[m_])
                    kb.dma('sp', ga_[:, 0:nt], sga_d[j * 128:(j + 1) * 128, tb0:tb0 + nt], W=[ga_])
                    kb.dma('sp', gm_[:, 0:nt], sgm_d[j * 128:(j + 1) * 128, tb0:tb0 + nt], W=[gm_])
                    kb.wait('pe', R=[a_, aT], W=[pa_])
                    for k in range(AC):
                        ins = nc.tensor.matmul(pa_[:, 0:nt], lhsT=a_[:, k, :], rhs=aT[:, k, 0:nt], start=(k == 0), stop=(k == AC - 1))
                    kb.done('pe', ins, R=[a_, aT], W=[pa_])
                    kb.wait('pe', R=[m_, mT], W=[pm_])
                    for k in range(MC):
                        ins = nc.tensor.matmul(pm_[:, 0:nt], lhsT=m_[:, k, :], rhs=mT[:, k, 0:nt], start=(k == 0), stop=(k == MC - 1))
                    kb.done('pe', ins, R=[m_, mT], W=[pm_])
                    kb.op('dve', lambda v: v.tensor_tensor(out=t_[:, 0:nt], in0=pa_[:, 0:nt], in1=ga_[:, 0:nt], op=ALU.mult), R=[pa_, ga_], W=[t_])
                    kb.op('dve', lambda v: v.tensor_tensor(out=mg[:, j, 0:nt], in0=pm_[:, 0:nt], in1=gm_[:, 0:nt], op=ALU.mult), R=[pm_, gm_], W=[mg])
                    kb.op('pool', lambda v: v.tensor_tensor(out=mg[:, j, 0:nt], in0=mg[:, j, 0:nt], in1=t_[:, 0:nt], op=ALU.add), R=[mg, t_], W=[mg])
                for n0 in range(0, D, 512):
                    w_ = wo[wi % 2]; wi += 1
                    kb.dma('sp', w_[:], WS[l % NWS]['wo_b'][:, n0 // 512, :, :], W=[w_])
                    for tt in range(nt // 128):
                        tg = (tb0 // 128) + tt
                        if last and tg < CT:
                            continue
                        r = 1 if tg < CT else 0
                        p_ = po[pi % 2]; pi += 1
                        x_ = xt[xi % 3]; xi += 1
                        kb.dma('sp', x_[:], xs[tg * 128:(tg + 1) * 128, n0:n0 + 512], W=[x_])
                        g_ = gts[xi % 3]
                        kb.dma('sp', g_[:], mod_d[l, r:r + 1, 2 * D + n0:2 * D + n0 + 512].to_broadcast([128, 512]), W=[g_])
                        kb.wait('pe', R=[mg, w_], W=[p_])
                        for k in range(KC):
                            ins = nc.tensor.matmul(p_[:], lhsT=mg[:, k, tt * 128:(tt + 1) * 128], rhs=w_[:, k, :], start=(k == 0), stop=(k == KC - 1))
                        kb.done('pe', ins, R=[mg, w_], W=[p_])
                        t_ = t1[pi % 2]
                        kb.op('dve', lambda v: v.tensor_tensor(out=t_[:, 0:512], in0=p_[:], in1=g_[:], op=ALU.mult), R=[p_, g_], W=[t_])
                        kb.op('pool', lambda v: v.tensor_tensor(out=x_[:], in0=x_[:], in1=t_[:, 0:512], op=ALU.add), R=[x_, t_], W=[x_])
                        kb.dma('pool', xs[tg * 128:(tg + 1) * 128, n0:n0 + 512], x_[:], R=[x_])

    def moe_phase(l):
        last = (l == DEPTH - 1)
        NTB = 512
        FC = FF // 128
        with Ph() as ph:
            gts = [ph.sb([128, 512], F32, "gts") for _ in range(2)]
            hTb = ph.sb([128, KC, NTB], BF16, "hTb")
            yacc = ph.sb([128, NTB // 128, D], F32, "yacc")
            wg = [ph.sb([128, KC, 128], BF16, "wg") for _ in range(2)]; wu = [ph.sb([128, KC, 128], BF16, "wu") for _ in range(2)]
            wd = [ph.sb([128, FC, 512], BF16, "wd") for _ in range(2)]
            pg = [ph.ps([128, NTB], F32, "pg") for _ in range(2)]; pu = [ph.ps([128, NTB], F32, "pu") for _ in range(2)]
            pdn = [ph.ps([128, 512], F32, "pdn") for _ in range(4)]
            sg = [ph.sb([128, NTB], F32, "sg") for _ in range(2)]
            aT = [ph.sb([128, FC, NTB], BF16, "aT") for _ in range(2)]
            xt = [ph.sb([128, 512], F32, "xt") for _ in range(2)]
            gi = 0; di = 0; xi = 0
            t_start = CTX if last else 0
            for tb0 in range(t_start, Tn, NTB):
                nt = min(NTB, Tn - tb0)
                ntl = nt // 128
                kb.dma('sp', hTb[:, :, 0:nt], hT_d[:, :, tb0:tb0 + nt], W=[hTb])
                kb.op('pool', lambda v: v.memset(yacc[:], 0.0), W=[yacc])
                for e in range(NE):
                    a_ = aT[e % 2]
                    for j in range(FC):
                        g_ = wg[gi % 2]; u_ = wu[gi % 2]; pg_ = pg[gi % 2]; pu_ = pu[gi % 2]; s_ = sg[gi % 2]; gi += 1
                        kb.dma('sp', g_[:], WS[l % NWS]['wg_b'][e, :, j, :, :], W=[g_])
                        cv_tick()
                        kb.dma('sp', u_[:], WS[l % NWS]['wu_b'][e, :, j, :, :], W=[u_])
                        kb.wait('pe', R=[g_, hTb], W=[pg_])
                        for k in range(KC):
                            ins = nc.tensor.matmul(pg_[:, 0:nt], lhsT=g_[:, k, :], rhs=hTb[:, k, 0:nt], start=(k == 0), stop=(k == KC - 1))
                        kb.done('pe', ins, R=[g_, hTb], W=[pg_])
                        kb.wait('pe', R=[u_, hTb], W=[pu_])
                        for k in range(KC):
                            ins = nc.tensor.matmul(pu_[:, 0:nt], lhsT=u_[:, k, :], rhs=hTb[:, k, 0:nt], start=(k == 0), stop=(k == KC - 1))
                        kb.done('pe', ins, R=[u_, hTb], W=[pu_])
                        kb.op('act', lambda a: a.activation(out=s_[:, 0:nt], in_=pg_[:, 0:nt], func=AF.Sigmoid), R=[pg_], W=[s_])
                        kb.op('dve', lambda v: v.tensor_tensor(out=s_[:, 0:nt], in0=s_[:, 0:nt], in1=pg_[:, 0:nt], op=ALU.mult), R=[s_, pg_], W=[s_])
                        kb.op('dve', lambda v: v.tensor_tensor(out=a_[:, j, 0:nt], in0=s_[:, 0:nt], in1=pu_[:, 0:nt], op=ALU.mult), R=[s_, pu_], W=[a_])
                    for n0 in range(0, D, 512):
                        d_ = wd[(di // max(1, ntl)) % 2]
                        kb.dma('sp', d_[:], WS[l % NWS]['wd_b'][e, :, n0 // 512, :, :], W=[d_])
                        cv_tick()
                        for tt in range(ntl):
                            tg = tb0 // 128 + tt
                            p_ = pdn[di % 4]; di += 1
                            kb.wait('pe', R=[a_, d_], W=[p_])
                            for k in range(FC):
                                ins = nc.tensor.matmul(p_[:], lhsT=a_[:, k, tt * 128:(tt + 1) * 128], rhs=d_[:, k, :], start=(k == 0), stop=(k == FC - 1))
                            kb.done('pe', ins, R=[a_, d_], W=[p_])
                            kb.op('dve', lambda v: v.scalar_tensor_tensor(out=yacc[:, tt, n0:n0 + 512], in0=p_[:], scalar=comb[:, tg, e:e + 1], in1=yacc[:, tt, n0:n0 + 512], op0=ALU.mult, op1=ALU.add), R=[p_, comb, yacc], W=[yacc])
                for tt in range(ntl):
                    tg = tb0 // 128 + tt
                    r = 1 if tg < CT else 0
                    for n0 in range(0, D, 512):
                        x_ = xt[xi % 2]; g_ = gts[xi % 2]; xi += 1
                        kb.dma('sp', x_[:], xs[tg * 128:(tg + 1) * 128, n0:n0 + 512], W=[x_])
                        kb.dma('sp', g_[:], mod_d[l, r:r + 1, 5 * D + n0:5 * D + n0 + 512].to_broadcast([128, 512]), W=[g_])
                        kb.op('dve', lambda v: v.tensor_tensor(out=yacc[:, tt, n0:n0 + 512], in0=yacc[:, tt, n0:n0 + 512], in1=g_[:], op=ALU.mult), R=[yacc, g_], W=[yacc])
                        kb.op('pool', lambda v: v.tensor_tensor(out=x_[:], in0=x_[:], in1=yacc[:, tt, n0:n0 + 512], op=ALU.add), R=[x_, yacc], W=[x_])
                        if last:
                            kb.dma('pool', y_out[(tg - CT) * 128:(tg - CT + 1) * 128, n0:n0 + 512], x_[:], R=[x_])
                        else:
                            kb.dma('pool', xs[tg * 128:(tg + 1) * 128, n0:n0 + 512], x_[:], R=[x_])

    import os
    stop = int(os.environ.get("KSTOP", "999"))
    phs = []
    OVL = os.environ.get("KOVL", "1") == "1"

    def convert_layer(l):
        if l == 0:
            pending.append(convert_gen(0, None, "A" if OVL else "AB"))
            cv_flush()
            if OVL:
                pending.append(convert_gen(0, 'pool', "B"))
        else:
            if not OVL:
                pending.append(convert_gen(l, None, "AB"))
            cv_flush()

    def pre_merge(l):
        if OVL:
            while pending and not ready.get((l, 'm')):
                cv_tick()
            kb.barrier()

    def pre_moe(l):
        if OVL:
            cv_flush()
            if l + 1 < DEPTH:
                pending.append(convert_gen(l + 1, 'pool', "AB"))

    for l in range(DEPTH):
        phs += [lambda l=l: convert_layer(l), lambda l=l: adaln(l), lambda l=l: norm_phase(l, 0), lambda l=l: proj_phase(l),
                lambda l=l: conv_phase(l), lambda l=l: qk_phase(l), lambda l=l: attn_phase(l), lambda l=l: mlstm_phase(l),
                lambda l=l: hm_phase(l), lambda l=l: (pre_merge(l), merge_phase(l)), lambda l=l: norm_phase(l, 1), lambda l=l: (pre_moe(l), moe_phase(l))]
    for i, p in enumerate(phs):
        if i < stop:
            p()
    kb.barrier()
    dbg = [d for d in os.environ.get("KDBG", "").split(",") if d]
    scr = dict(xs=xs, hT_d=hT_d, mod_d=mod_d, aq_d=aq_d, ak_d=ak_d, av_d=av_d, mv_d=mv_d, mo_d=mo_d, mg_d=mg_d, mqk_d=mqk_d,
               mqkc_d=mqkc_d, sga_d=sga_d, sgm_d=sgm_d, qT_d=qT_d, kT_d=kT_d, atT_d=atT_d, hf_d=hf_d, hb_d=hb_d, hmT_d=hmT_d)
    for d in dbg:
        src = scr[d]
        o = nc.dram_tensor("dbg_" + d, list(src.shape), src.dtype, kind="ExternalOutput").ap()
        if len(src.shape) == 3:
            for i in range(src.shape[0]):
                kb.dma('sp', o[i], src[i])
        else:
            kb.dma('sp', o[:, :], src[:, :])
    if "comb" in os.environ.get("KDBG2", ""):
        o = nc.dram_tensor("dbg_comb", [128, TT * NE], F32, kind="ExternalOutput").ap()
        kb.dma('sp', o[:, :], comb[:].rearrange("p t e -> p (t e)"), R=[comb])
    kb.barrier()
    pes.__exit__(None, None, None)
    return nc


def host_consts(cfg):
    NLAT = cfg['NLAT']; GW = cfg['GRID_W']
    rp = 32
    inv = (10000.0 ** (-np.arange(rp, dtype=np.float32) / rp)).astype(np.float32)
    t = np.arange(NLAT)
    ar = (t // GW).astype(np.float32)[:, None] * inv
    ac = (t % GW).astype(np.float32)[:, None] * inv
    rope = np.concatenate([np.cos(ar), np.cos(ac), np.sin(ar), np.sin(ac)], axis=1).astype(np.float32)
    ident = np.eye(128, dtype=np.float32)
    i = np.arange(128)[None, :]; j = np.arange(128)[:, None]
    m = []
    for c in (-1, 0, 1):
        rel = (c * 128 + j) - i
        m.append(np.where(np.abs(rel) <= 128, 0.0, NEG).astype(np.float32))
    trif = (j <= i).astype(np.float32)
    cst = np.concatenate([ident] + m + [trif], axis=1).astype(np.float32)
    return rope, cst


def make_in_maps(cfg, inp):
    D = cfg['D']; MH = cfg['MH']; NH = cfg['NH']; NKV = cfg['NKV']
    AW = NH * 128; KW = NKV * 128; QKW = MH * cfg['DK']; MW = MH * cfg['DV']
    o_mg = AW + 2 * KW + 2 * QKW + 2 * MW
    perm = np.arange(inp['w_in'].shape[-1])
    g = np.arange(4 * MH).reshape(4, MH)
    perm[o_mg:o_mg + 4 * MH] = o_mg + np.concatenate([g[0], g[2], g[1], g[3]])
    w_in = np.ascontiguousarray(inp['w_in'][:, :, perm]); b_in = np.ascontiguousarray(inp['b_in'][:, perm])
    rope, cst = host_consts(cfg)
    f = lambda a: np.ascontiguousarray(np.asarray(a, dtype=np.float32))
    maps = []
    for b in range(inp['x'].shape[0]):
        maps.append({
            'x': f(inp['x'][b]), 'ctx': f(inp['ctx'][b]), 'cc': f(np.stack([inp['c'][b], inp['c_ctx']], 0)),
            'w_ada': f(inp['w_ada']), 'b_ada': f(inp['b_ada']), 'g_mix': f(inp['g_mix']), 'g_ffn': f(inp['g_ffn']),
            'w_in': f(w_in), 'b_in': f(b_in), 'g_q': f(inp['g_q']), 'g_k': f(inp['g_k']), 'sink': f(inp['sink']),
            'conv_w': f(inp['conv_w']), 'conv_b': f(inp['conv_b']), 'g_mh': f(inp['g_mh']),
            'w_br_attn': f(inp['w_br_attn']), 'w_br_mlstm': f(inp['w_br_mlstm']), 'w_out': f(inp['w_out']),
            'w_router': f(inp['w_router']), 'b_router': f(np.asarray(inp['b_router']).reshape(1, -1)),
            'w_gate': f(inp['w_gate']), 'w_up': f(inp['w_up']), 'w_down': f(inp['w_down']),
            'rope_cs': rope, 'cst': cst,
        })
    return maps


def run(cfg, inp, trace=False):
    nc = build(cfg)
    maps = make_in_maps(cfg, inp)
    res = run_bass_kernel_spmd(nc, maps, core_ids=list(range(len(maps))), trace=trace)
    out = np.stack([res.results[b]['y'] for b in range(len(maps))], 0).astype(np.float32)
    return out, res


def kernel(**inputs):
    inp = {k: np.asarray(v) for k, v in inputs.items()}
    out, _ = run(CFG_FULL, inp)
    return out
```

```python
from contextlib import ExitStack
import os
import numpy as np
import concourse.bass as bass
import concourse.mybir as mybir
from concourse.bass_utils import run_bass_kernel_spmd

F32 = mybir.dt.float32
BF16 = mybir.dt.bfloat16
ALU = mybir.AluOpType
AF = mybir.ActivationFunctionType
AX = mybir.AxisListType
EPS = 1e-6
NEG = -1e30

CFG_FULL = dict(D=4096, NLAT=8192, CTX=256, DEPTH=2, NH=16, NKV=4, MH=8, DK=128, DV=256, NE=16, NG=4, FF=1024, GRID_W=64)


class T:
    psum = False
    def __init__(s, h):
        s.h = h; s.lw = None; s.rd = {}
    def __getitem__(s, i):
        return s.h[i]


class TP(T):
    psum = True
    def __init__(s, h, shape, dt):
        T.__init__(s, h)
        v = h[:]
        if dt != F32:
            v = v.bitcast(dt)
        n = 1
        for d in shape[1:]:
            n *= d
        v = v[0:shape[0], 0:n]
        if len(shape) == 3:
            v = v.rearrange("p (a b) -> p a b", a=shape[1])
        s.view = v
    def __getitem__(s, i):
        return s.view[i]


class KB:
    NDS = 12
    def __init__(s, nc):
        s.nc = nc
        s.eng = {'pe': nc.tensor, 'act': nc.scalar, 'dve': nc.vector, 'pool': nc.gpsimd, 'sp': nc.sync}
        s.sem = {e: nc.alloc_semaphore('s_' + e) for e in ['pe', 'act', 'dve', 'pool']}
        s.cnt = {e: 0 for e in s.sem}
        s.seen = {e: {} for e in s.eng}
        s.dq = ['sp', 'pool']
        s.dsem = {q: [nc.alloc_semaphore(f'd_{q}{i}') for i in range(s.NDS)] for q in s.dq}
        s.dcnt = {q: [0] * s.NDS for q in s.dq}
        s.dnext = {q: 0 for q in s.dq}
        s.uid = 0

    def semobj(s, key):
        return s.sem[key[1]] if key[0] == 'c' else s.dsem[key[1]][key[2]]

    def need(s, e, ev):
        if ev is None:
            return
        key, val = ev
        if val <= 0 or s.seen[e].get(key, 0) >= val:
            return
        if key == ('c', 'pe') and e == 'pe':
            return
        s.eng[e].wait_ge(s.semobj(key), val)
        s.seen[e][key] = val

    def wait(s, e, R=(), W=()):
        for t in R:
            s.need(e, t.lw)
            if t.psum:
                for k, v in t.rd.items():
                    if k != ('c', e):
                        s.need(e, (k, v))
        for t in W:
            s.need(e, t.lw)
            for k, v in t.rd.items():
                s.need(e, (k, v))

    def done(s, e, ins, R=(), W=()):
        s.cnt[e] += 1
        ins.then_inc(s.sem[e], 1)
        key = ('c', e); val = s.cnt[e]
        for t in W:
            t.lw = (key, val); t.rd = {}
        for t in R:
            t.rd[key] = val

    def op(s, e, f, R=(), W=()):
        s.wait(e, R, W)
        ins = f(s.eng[e])
        s.done(e, ins, R, W)
        return ins

    def dma(s, q, out, in_, R=(), W=()):
        s.wait(q, R, W)
        k = s.dnext[q]; s.dnext[q] = (k + 1) % s.NDS
        key = ('d', q, k)
        s.need(q, (key, s.dcnt[q][k]))
        ins = s.eng[q].dma_start(out=out, in_=in_)
        s.dcnt[q][k] += 16
        ins.then_inc(s.dsem[q][k], 16)
        val = s.dcnt[q][k]
        for t in W:
            t.lw = (key, val); t.rd = {}
        for t in R:
            t.rd[key] = val

    def barrier(s):
        for e in s.eng:
            for c in s.sem:
                s.need(e, (('c', c), s.cnt[c]))
            for q in s.dq:
                for k in range(s.NDS):
                    s.need(e, (('d', q, k), s.dcnt[q][k]))

    def name(s, p):
        s.uid += 1
        return f"{p}_{s.uid}"


def build(cfg, need_out_ctx=False):
    D = cfg['D']; NLAT = cfg['NLAT']; CTX = cfg['CTX']; DEPTH = cfg['DEPTH']
    NH = cfg['NH']; NKV = cfg['NKV']; MH = cfg['MH']; DK = cfg['DK']; DV = cfg['DV']
    NE = cfg['NE']; NG = cfg['NG']; FF = cfg['FF']
    HD = 128
    GRP = NH // NKV
    AW = NH * HD; KW = NKV * HD; QKW = MH * DK; MW = MH * DV; NGC = 4 * MH
    DIN = AW + 2 * KW + 2 * QKW + 2 * MW + NGC + 2 * D
    Tn = CTX + NLAT; TT = Tn // 128; CT = CTX // 128; KC = D // 128
    NB = NLAT // 128
    EPG = NE // NG
    o_aq = 0; o_ak = AW; o_av = AW + KW; o_mq = AW + 2 * KW; o_mk = o_mq + QKW; o_mv = o_mk + QKW
    o_mo = o_mv + MW; o_mg = o_mo + MW; o_ga = o_mg + NGC; o_gm = o_ga + D

    nc = bass.Bass("TRN2", target_bir_lowering=False)
    kb = KB(nc)

    def din(name, shape, dt=F32):
        return nc.dram_tensor(name, list(shape), dt, kind="ExternalInput").ap()

    def dsc(name, shape, dt):
        return nc.dram_tensor(name, list(shape), dt, kind="Internal").ap()

    x_in = din("x", [NLAT, D]); ctx_in = din("ctx", [CTX, D]); cc_in = din("cc", [2, D])
    w_ada = din("w_ada", [DEPTH, D, 6 * D]); b_ada = din("b_ada", [DEPTH, 6 * D])
    g_mix = din("g_mix", [DEPTH, D]); g_ffn = din("g_ffn", [DEPTH, D])
    w_in = din("w_in", [DEPTH, D, DIN]); b_in = din("b_in", [DEPTH, DIN])
    g_q = din("g_q", [DEPTH, HD]); g_k = din("g_k", [DEPTH, HD]); sink = din("sink", [DEPTH, NH])
    conv_w = din("conv_w", [DEPTH, 3, 2 * QKW]); conv_b = din("conv_b", [DEPTH, 2 * QKW])
    g_mh = din("g_mh", [DEPTH, MW])
    w_ba = din("w_br_attn", [DEPTH, AW, D]); w_bm = din("w_br_mlstm", [DEPTH, MW, D]); w_o = din("w_out", [DEPTH, D, D])
    w_r = din("w_router", [D, NE]); b_r = din("b_router", [1, NE])
    w_g = din("w_gate", [DEPTH, NE, D, FF]); w_u = din("w_up", [DEPTH, NE, D, FF]); w_d = din("w_down", [DEPTH, NE, FF, D])
    rope_cs = din("rope_cs", [NLAT, 128])
    cst = din("cst", [128, 5 * 128])
    y_out = nc.dram_tensor("y", [NLAT, D], F32, kind="ExternalOutput").ap()

    xs = dsc("xs", [Tn, D], F32)
    hT_d = dsc("hT_d", [128, KC, Tn], BF16)
    mod_d = dsc("mod_d", [DEPTH, 2, 6 * D], F32)
    def wblk(name, R_, C_, BW, lead=()):
        return dsc(name, list(lead) + [128, C_ // BW, R_ // 128, BW], BF16)
    tm_cols = [("aq", o_aq, AW), ("ak", o_ak, KW), ("av", o_av, KW), ("mv", o_mv, MW), ("mo", o_mo, MW), ("mg", o_mg, NGC)]
    fm_cols = [("mqk", o_mq, 2 * QKW), ("ga", o_ga, D), ("gm", o_gm, D)]
    NWS = min(2, DEPTH)
    WS = []
    for wl in range(NWS):
        sfx = f"_{wl}"
        WS.append(dict(
            win_tm={n: (wblk("wtm_" + n + sfx, D, cw, min(512, cw)), c0, cw, min(512, cw)) for (n, c0, cw) in tm_cols},
            win_fm={n: (wblk("wfm_" + n + sfx, D, cw, 128), c0, cw, 128) for (n, c0, cw) in fm_cols},
            wba_b=wblk("wba_b" + sfx, AW, D, 128), wbm_b=wblk("wbm_b" + sfx, MW, D, 128), wo_b=wblk("wo_b" + sfx, D, D, 512),
            wg_b=wblk("wg_b" + sfx, D, FF, 128, [NE]), wu_b=wblk("wu_b" + sfx, D, FF, 128, [NE]), wd_b=wblk("wd_b" + sfx, FF, D, 512, [NE])))
    aq_d = dsc("aq_d", [Tn, AW], BF16); ak_d = dsc("ak_d", [Tn, KW], BF16); av_d = dsc("av_d", [Tn, KW], BF16)
    mv_d = dsc("mv_d", [Tn, MW], BF16); mo_d = dsc("mo_d", [Tn, MW], BF16); mg_d = dsc("mg_d", [Tn, NGC], F32)
    mqk_d = dsc("mqk_d", [2 * QKW, Tn], BF16); mqkc_d = dsc("mqkc_d", [2 * QKW, Tn], BF16)
    sga_d = dsc("sga_d", [D, Tn], BF16); sgm_d = dsc("sgm_d", [D, Tn], BF16)
    qT_d = dsc("qT_d", [128, NH, Tn], BF16); kT_d = dsc("kT_d", [128, NKV, Tn], BF16)
    atT_d = dsc("atT_d", [128, NH, Tn], BF16)
    hf_d = dsc("hf_d", [Tn, MW], BF16); hb_d = dsc("hb_d", [Tn, MW], BF16)
    hmT_d = dsc("hmT_d", [128, MW // 128, Tn], BF16)

    def phase():
        kb.barrier()

    class Ph:
        def __enter__(s):
            s.es = ExitStack(); s.es.__enter__(); return s
        def __exit__(s, *a):
            kb.barrier(); return s.es.__exit__(*a)
        def sb(s, shape, dt=F32, nm="t"):
            return T(s.es.enter_context(nc.sbuf_tensor(kb.name(nm), list(shape), dt)))
        def ps(s, shape, dt=F32, nm="p"):
            return TP(s.es.enter_context(nc.psum_tensor(kb.name(nm), [128, 512], F32)), list(shape), dt)

    pes = ExitStack(); pes.__enter__()
    pes.enter_context(nc.allow_non_contiguous_dma(reason="strided layout transforms"))
    def psb(shape, dt=F32, nm="c"):
        return T(pes.enter_context(nc.sbuf_tensor(kb.name(nm), list(shape), dt)))
    cst_f = psb([128, 5 * 128], F32, "cstf")
    ident_f = None
    ident_b = psb([128, 128], BF16, "idb")
    maskT = psb([128, 3 * 128], F32, "maskT")
    tri_f = psb([128, 128], F32, "trif")
    tri_b = psb([128, 128], F32, "trib")
    tri_fb = psb([128, 128], BF16, "trifb")
    tri_bb = psb([128, 128], BF16, "tribb")
    ones_f = psb([128, 128], F32, "onesf")
    ones_b = psb([128, 128], BF16, "onesb")
    kb.dma('sp', cst_f[:], cst[:, :], W=[cst_f])
    kb.op('dve', lambda v: v.tensor_copy(out=ident_b[:], in_=cst_f[:, 0:128]), R=[cst_f], W=[ident_b])
    kb.op('dve', lambda v: v.tensor_copy(out=maskT[:], in_=cst_f[:, 128:512]), R=[cst_f], W=[maskT])
    kb.op('dve', lambda v: v.tensor_copy(out=tri_f[:], in_=cst_f[:, 512:640]), R=[cst_f], W=[tri_f])
    kb.op('dve', lambda v: v.memset(ones_f[:], 1.0), W=[ones_f])
    kb.op('dve', lambda v: v.memset(ones_b[:], 1.0), W=[ones_b])
    kb.op('dve', lambda v: v.tensor_tensor(out=tri_b[:], in0=ones_f[:], in1=tri_f[:], op=ALU.subtract), R=[ones_f, tri_f], W=[tri_b])
    kb.op('dve', lambda v: v.tensor_tensor(out=tri_b[:], in0=tri_b[:], in1=cst_f[:, 0:128], op=ALU.add), R=[tri_b, cst_f], W=[tri_b])
    kb.op('dve', lambda v: v.tensor_copy(out=tri_fb[:], in_=tri_f[:]), R=[tri_f], W=[tri_fb])
    kb.op('dve', lambda v: v.tensor_copy(out=tri_bb[:], in_=tri_b[:]), R=[tri_b], W=[tri_bb])
    ident_f = cst_f

    rr = [0]
    def cast_eng():
        rr[0] += 1
        return ['dve', 'act', 'pool'][rr[0] % 3]

    def copy_on(e, out, in_, R, W):
        if e == 'act':
            kb.op('act', lambda a: a.copy(out=out, in_=in_), R=R, W=W)
        else:
            kb.op(e, lambda v: v.tensor_copy(out=out, in_=in_), R=R, W=W)

    CVW = 1024
    cvbuf = [(psb([128, CVW], F32, "cf"), psb([128, CVW], BF16, "cb")) for _ in range(3)]
    cvi = [0]

    def cvt(src, dst, R_, C_, BW, eng):
        CC = min(C_, CVW)
        for r0 in range(0, R_, 128):
            k = r0 // 128
            for c0 in range(0, C_, CC):
                cw = min(CC, C_ - c0)
                f, b = cvbuf[cvi[0] % 3]; cvi[0] += 1
                kb.dma('sp', f[:, 0:cw], src[r0:r0 + 128, c0:c0 + cw], W=[f])
                copy_on(eng if eng else cast_eng(), b[:, 0:cw], f[:, 0:cw], [f], [b])
                kb.dma('pool', dst[:, c0 // BW:(c0 + cw) // BW, k, :], b[:, 0:cw].rearrange("p (a b) -> p a b", b=BW), R=[b])
                yield

    ready = {}

    def convert_gen(l, eng=None, parts="AB"):
        ws = WS[l % NWS]
        if "A" in parts:
            for n, (dst, c0, cw, BW) in list(ws['win_tm'].items()) + list(ws['win_fm'].items()):
                yield from cvt(w_in[l][:, c0:c0 + cw], dst, D, cw, BW, eng)
        if "B" in parts:
            yield from cvt(w_ba[l], ws['wba_b'], AW, D, 128, eng)
            yield from cvt(w_bm[l], ws['wbm_b'], MW, D, 128, eng)
            yield from cvt(w_o[l], ws['wo_b'], D, D, 512, eng)
            ready[(l, 'm')] = True
            for e in range(NE):
                yield from cvt(w_g[l, e], ws['wg_b'][e], D, FF, 128, eng)
                yield from cvt(w_u[l, e], ws['wu_b'][e], D, FF, 128, eng)
                yield from cvt(w_d[l, e], ws['wd_b'][e], FF, D, 512, eng)

    pending = []

    def cv_tick(n=1):
        for _ in range(n):
            if not pending:
                return
            try:
                next(pending[0])
            except StopIteration:
                pending.pop(0)

    def cv_flush():
        while pending:
            cv_tick()
        kb.barrier()

    kb.dma('sp', xs[0:CTX, :], ctx_in[:, :])
    for r0 in range(0, NLAT, 1024):
        r1 = min(NLAT, r0 + 1024)
        kb.dma('sp', xs[CTX + r0:CTX + r1, :], x_in[r0:r1, :])
    kb.barrier()

    def adaln(l):
        with Ph() as ph:
            ct = ph.sb([128, 2, KC], F32, "ct")
            for r in range(2):
                kb.dma('sp', ct[:, r, :], cc_in[r, :].rearrange("(k p) -> p k", p=128), W=[ct])
            sg = ph.sb([128, 2, KC], F32, "sg")
            kb.op('act', lambda a: a.activation(out=sg[:], in_=ct[:], func=AF.Sigmoid), R=[ct], W=[sg])
            kb.op('dve', lambda v: v.tensor_tensor(out=ct[:], in0=ct[:], in1=sg[:], op=ALU.mult), R=[ct, sg], W=[ct])
            BW = 2048 if (6 * D) % 2048 == 0 else 1024
            NJ = BW // 512
            wt = [ph.sb([128, BW], F32, "wt") for _ in range(3)]
            pss = [ph.ps([2, 512], F32, "pa") for _ in range(NJ)]
            ob = ph.sb([2, BW], F32, "ob"); bb = ph.sb([2, BW], F32, "bb")
            NBLK = (6 * D) // BW
            i = 0
            for nb in range(NBLK):
                kb.dma('sp', bb[:], b_ada[l:l + 1, nb * BW:(nb + 1) * BW].to_broadcast([2, BW]), W=[bb])
                for kc in range(KC):
                    w = wt[i % 3]; i += 1
                    kb.dma('sp', w[:], w_ada[l, kc * 128:(kc + 1) * 128, nb * BW:(nb + 1) * BW], W=[w])
                    kb.wait('pe', R=[w, ct], W=pss if kc == 0 else [])
                    for j in range(NJ):
                        ins = nc.tensor.matmul(pss[j][:], lhsT=ct[:, :, kc], rhs=w[:, j * 512:(j + 1) * 512], start=(kc == 0), stop=(kc == KC - 1))
                    kb.done('pe', ins, R=[w, ct], W=pss if kc == KC - 1 else [])
                for j in range(NJ):
                    kb.op('dve', lambda v: v.tensor_tensor(out=ob[:, j * 512:(j + 1) * 512], in0=pss[j][:], in1=bb[:, j * 512:(j + 1) * 512], op=ALU.add), R=[pss[j], bb], W=[ob])
                kb.dma('pool', mod_d[l, :, nb * BW:(nb + 1) * BW], ob[:], R=[ob])

    def load_rep(ph_t, src_row_ap, n):
        kb.dma('sp', ph_t[:, 0:n], src_row_ap.to_broadcast([128, n]), W=[ph_t])

    def norm_phase(l, which):
        last = (l == DEPTH - 1)
        gsrc = g_mix if which == 0 else g_ffn
        so = 0 if which == 0 else 3
        with Ph() as ph:
            G = [ph.sb([128, D], F32, "G") for _ in range(2)]
            S = [ph.sb([128, D], F32, "S") for _ in range(2)]
            gr = ph.sb([128, D], F32, "gr")
            load_rep(gr, gsrc[l:l + 1, :], D)
            for r in range(2):
                load_rep(S[r], mod_d[l, r:r + 1, so * D:(so + 1) * D], D)
                load_rep(G[r], mod_d[l, r:r + 1, (so + 1) * D:(so + 2) * D], D)
                kb.op('dve', lambda v: v.scalar_tensor_tensor(out=G[r][:], in0=G[r][:], scalar=1.0, in1=gr[:], op0=ALU.add, op1=ALU.mult), R=[G[r], gr], W=[G[r]])
            xt = [ph.sb([128, D], F32, "xt") for _ in range(2)]
            junk = ph.sb([128, D], BF16, "junk")
            hb = [ph.sb([128, D], BF16 if which == 0 else F32, "hb") for _ in range(2 if which == 0 else 1)]
            hTt = [ph.sb([128, KC, 128], BF16, "hTt") for _ in range(2)]
            st = [ph.sb([128, 4], F32, "st") for _ in range(2)]
            if which == 0:
                ptr = [ph.ps([128, 8, 128], BF16, "ptr") for _ in range(2)]
                NPT = 8
            else:
                ptr = [ph.ps([128, 4, 128], F32, "ptr") for _ in range(2)]
                NPT = 4
                hTf = [ph.sb([128, KC, 128], F32, "hTf") for _ in range(1)]
                wr = ph.sb([128, KC, NE], F32, "wr")
                if 'a' not in os.environ.get("KX", ""):
                    kb.dma('sp', wr[:], w_r.rearrange("(k p) e -> p k e", p=128), W=[wr])
                brr = ph.sb([128, NE], F32, "brr")
                if 'a' not in os.environ.get("KX", ""):
                    load_rep(brr, b_r[0:1, :], NE)
                pl = [ph.ps([128, NE], F32, "pl") for _ in range(2)]
                rt = {k: ph.sb([128, NE], F32, "r" + k) for k in ['aff', 'bi', 'sel', 'm2', 'oh', 'w']}
                ps6 = ph.sb([128, NG, 6], F32, "ps6"); gs = ph.sb([128, NG], F32, "gs"); gm1 = ph.sb([128, 4], F32, "gm1")
                goh = ph.sb([128, NG], F32, "goh")
            t0 = CT if (last and which == 1) else 0
            for t in range(t0, TT):
                r = 1 if t < CT else 0
                x_ = xt[t % 2]; h_ = hb[t % len(hb)]; s_ = st[t % 2]; hT_ = hTt[t % 2]
                kb.dma('sp', x_[:], xs[t * 128:(t + 1) * 128, :], W=[x_])
                kb.op('act', lambda a: a.activation(out=junk[:], in_=x_[:], func=AF.Square, accum_out=s_[:, 0:1]), R=[x_], W=[junk, s_])
                kb.op('dve', lambda v: v.tensor_scalar(out=s_[:, 1:2], in0=s_[:, 0:1], scalar1=1.0 / D, scalar2=EPS, op0=ALU.mult, op1=ALU.add), R=[s_], W=[s_])
                kb.op('act', lambda a: a.activation(out=s_[:, 2:3], in_=s_[:, 1:2], func=AF.Sqrt), R=[s_], W=[s_])
                kb.op('dve', lambda v: v.reciprocal(out=s_[:, 3:4], in_=s_[:, 2:3]), R=[s_], W=[s_])
                kb.op('dve', lambda v: v.scalar_tensor_tensor(out=x_[:], in0=x_[:], scalar=s_[:, 3:4], in1=G[r][:], op0=ALU.mult, op1=ALU.mult), R=[x_, s_, G[r]], W=[x_])
                kb.op('pool', lambda v: v.tensor_tensor(out=h_[:], in0=x_[:], in1=S[r][:], op=ALU.add), R=[x_, S[r]], W=[h_])
                idn = ident_b[:] if which == 0 else ident_f[:, 0:128]
                idt = ident_b if which == 0 else cst_f
                for k0 in range(0, KC, NPT):
                    p_ = ptr[(k0 // NPT) % 2]
                    kb.wait('pe', R=[h_, idt], W=[p_])
                    for k in range(k0, min(KC, k0 + NPT)):
                        if which == 0:
                            ins = nc.tensor.transpose(out=p_[:, k - k0, :], in_=h_[:, k * 128:(k + 1) * 128], identity=idn)
                        else:
                            ins = nc.tensor.matmul(p_[:, k - k0, :], lhsT=h_[:, k * 128:(k + 1) * 128], rhs=idn, start=True, stop=True)
                    kb.done('pe', ins, R=[h_, idt], W=[p_])
                    n = min(KC, k0 + NPT) - k0
                    kb.op('act', lambda a: a.copy(out=hT_[:, k0:k0 + n, :], in_=p_[:, 0:n, :]), R=[p_], W=[hT_])
                    if which == 1 and 'b' not in os.environ.get("KX", ""):
                        hf_ = hTf[0]
                        kb.op('dve', lambda v: v.tensor_copy(out=hf_[:, k0:k0 + n, :], in_=p_[:, 0:n, :]), R=[p_], W=[hf_])
                kb.dma('pool', hT_d[:, :, t * 128:(t + 1) * 128], hT_[:], R=[hT_])
                if which == 1 and os.environ.get("KNOROUTE") != "2":
                    hf_ = hTf[0]; pl_ = pl[t % 2]
                    kb.wait('pe', R=[hf_, wr], W=[pl_])
                    for k in range(KC):
                        ins = nc.tensor.matmul(pl_[:], lhsT=hf_[:, k, :], rhs=wr[:, k, :], start=(k == 0), stop=(k == KC - 1))
                    kb.done('pe', ins, R=[hf_, wr], W=[pl_])
                    if os.environ.get("KNOROUTE"):
                        continue
                    aff = rt['aff']; bi = rt['bi']; sel = rt['sel']; m2 = rt['m2']; oh = rt['oh']; w_ = rt['w']
                    kb.op('act', lambda a: a.activation(out=aff[:], in_=pl_[:], func=AF.Sigmoid), R=[pl_], W=[aff])
                    kb.op('dve', lambda v: v.tensor_tensor(out=bi[:], in0=aff[:], in1=brr[:], op=ALU.add), R=[aff, brr], W=[bi])
                    big = bi[:].rearrange("p (g e) -> p g e", g=NG)
                    pi = 0
                    for a_ in range(EPG):
                        for b_ in range(a_ + 1, EPG):
                            kb.op('dve', lambda v: v.tensor_tensor(out=ps6[:, :, pi], in0=big[:, :, a_], in1=big[:, :, b_], op=ALU.add), R=[bi], W=[ps6])
                            pi += 1
                    kb.op('dve', lambda v: v.tensor_reduce(out=gs[:], in_=ps6[:], axis=AX.X, op=ALU.max), R=[ps6], W=[gs])
                    kb.op('dve', lambda v: v.tensor_reduce(out=gm1[:, 0:1], in_=gs[:], axis=AX.X, op=ALU.max), R=[gs], W=[gm1])
                    kb.op('dve', lambda v: v.tensor_scalar(out=goh[:], in0=gs[:], scalar1=gm1[:, 0:1], scalar2=None, op0=ALU.is_ge), R=[gs, gm1], W=[goh])
                    kb.op('dve', lambda v: v.tensor_tensor(out=m2[:].rearrange("p (g e) -> p g e", g=NG), in0=big, in1=goh[:].unsqueeze(2).to_broadcast([128, NG, EPG]), op=ALU.mult), R=[bi, goh], W=[m2])
                    kb.op('dve', lambda v: v.tensor_scalar(out=goh[:], in0=goh[:], scalar1=-1.0, scalar2=1e30, op0=ALU.add, op1=ALU.mult), R=[goh], W=[goh])
                    kb.op('dve', lambda v: v.tensor_tensor(out=m2[:].rearrange("p (g e) -> p g e", g=NG), in0=m2[:].rearrange("p (g e) -> p g e", g=NG), in1=goh[:].unsqueeze(2).to_broadcast([128, NG, EPG]), op=ALU.add), R=[m2, goh], W=[m2])
                    kb.op('dve', lambda v: v.tensor_reduce(out=gm1[:, 1:2], in_=m2[:], axis=AX.X, op=ALU.max), R=[m2], W=[gm1])
                    kb.op('dve', lambda v: v.tensor_scalar(out=sel[:], in0=m2[:], scalar1=gm1[:, 1:2], scalar2=None, op0=ALU.is_ge), R=[m2, gm1], W=[sel])
                    kb.op('dve', lambda v: v.scalar_tensor_tensor(out=m2[:], in0=sel[:], scalar=-1e30, in1=m2[:], op0=ALU.mult, op1=ALU.add), R=[sel, m2], W=[m2])
                    kb.op('dve', lambda v: v.tensor_reduce(out=gm1[:, 2:3], in_=m2[:], axis=AX.X, op=ALU.max), R=[m2], W=[gm1])
                    kb.op('dve', lambda v: v.tensor_scalar(out=oh[:], in0=m2[:], scalar1=gm1[:, 2:3], scalar2=None, op0=ALU.is_ge), R=[m2, gm1], W=[oh])
                    kb.op('dve', lambda v: v.tensor_tensor(out=sel[:], in0=sel[:], in1=oh[:], op=ALU.add), R=[sel, oh], W=[sel])
                    kb.op('dve', lambda v: v.tensor_tensor(out=w_[:], in0=sel[:], in1=aff[:], op=ALU.mult), R=[sel, aff], W=[w_])
                    kb.op('dve', lambda v: v.tensor_reduce(out=gm1[:, 3:4], in_=w_[:], axis=AX.X, op=ALU.add), R=[w_], W=[gm1])
                    kb.op('dve', lambda v: v.reciprocal(out=gm1[:, 3:4], in_=gm1[:, 3:4]), R=[gm1], W=[gm1])
                    kb.op('dve', lambda v: v.tensor_scalar(out=comb[:, t, :], in0=w_[:], scalar1=gm1[:, 3:4], scalar2=None, op0=ALU.mult), R=[w_, gm1], W=[comb])

    comb = psb([128, TT, NE], F32, "comb")

    def proj_phase(l):
        last = (l == DEPTH - 1)
        NTB = 512
        tm_groups = [("aq", aq_d, BF16), ("ak", ak_d, BF16), ("av", av_d, BF16), ("mv", mv_d, BF16), ("mo", mo_d, BF16), ("mg", mg_d, F32)]
        fm_groups = [("mqk", mqk_d, 0), ("ga", sga_d, 1), ("gm", sgm_d, 1)]
        with Ph() as ph:
            hTb = [ph.sb([128, KC, NTB], BF16, "hTb") for _ in range(1)]
            wtm = [ph.sb([128, KC, 512], BF16, "wtm") for _ in range(2)]
            brep = [ph.sb([128, 512], F32, "brep") for _ in range(2)]
            bcol = [ph.sb([128, 1], F32, "bcol") for _ in range(2)]
            pp = [ph.ps([128, 512], F32, "pp") for _ in range(4)]
            ot = [ph.sb([128, 512], BF16, "ot") for _ in range(3)]
            otf = [ph.sb([128, 512], F32, "otf") for _ in range(2)]
            wi = 0; pi = 0; oi = 0
            for tb0 in range(0, Tn, NTB):
                nt = min(NTB, Tn - tb0)
                hT_ = hTb[0]
                kb.dma('sp', hT_[:, :, 0:nt], hT_d[:, :, tb0:tb0 + nt], W=[hT_])
                for (gn, dst, dt) in tm_groups:
                    wsrc, c0, cw, BW = WS[l % NWS]['win_tm'][gn]
                    for n0 in range(0, cw, BW):
                        nw = BW
                        w_ = wtm[wi % 2]; br_ = brep[wi % 2]; wi += 1
                        kb.dma('sp', w_[:, :, 0:nw], wsrc[:, n0 // BW, :, :], W=[w_])
                        kb.dma('sp', br_[:, 0:nw], b_in[l:l + 1, c0 + n0:c0 + n0 + nw].to_broadcast([128, nw]), W=[br_])
                        cv_tick()
                        for tt in range(nt // 128):
                            p_ = pp[pi % 4]; pi += 1
                            kb.wait('pe', R=[hT_, w_], W=[p_])
                            for k in range(KC):
                                ins = nc.tensor.matmul(p_[:, 0:nw], lhsT=hT_[:, k, tt * 128:(tt + 1) * 128], rhs=w_[:, k, 0:nw], start=(k == 0), stop=(k == KC - 1))
                            kb.done('pe', ins, R=[hT_, w_], W=[p_])
                            if dt == BF16:
                                o_ = ot[oi % 3]; oi += 1
                            else:
                                o_ = otf[oi % 2]; oi += 1
                            kb.op('dve', lambda v: v.tensor_tensor(out=o_[:, 0:nw], in0=p_[:, 0:nw], in1=br_[:, 0:nw], op=ALU.add), R=[p_, br_], W=[o_])
                            kb.dma('pool', dst[tb0 + tt * 128:tb0 + (tt + 1) * 128, n0:n0 + nw], o_[:, 0:nw], R=[o_])
                for (gn, dst, sg) in fm_groups:
                    wsrc, c0, cw, BW = WS[l % NWS]['win_fm'][gn]
                    for n0 in range(0, cw, 128):
                        w_ = wtm[wi % 2]; bc_ = bcol[wi % 2]; wi += 1
                        kb.dma('sp', w_[:, :, 0:128], wsrc[:, n0 // 128, :, :], W=[w_])
                        kb.dma('sp', bc_[:], b_in[l, c0 + n0:c0 + n0 + 128].rearrange("(p o) -> p o", o=1), W=[bc_])
                        cv_tick()
                        p_ = pp[pi % 4]; pi += 1
                        kb.wait('pe', R=[hT_, w_], W=[p_])
                        for k in range(KC):
                            ins = nc.tensor.matmul(p_[:, 0:nt], lhsT=w_[:, k, 0:128], rhs=hT_[:, k, 0:nt], start=(k == 0), stop=(k == KC - 1))
                        kb.done('pe', ins, R=[hT_, w_], W=[p_])
                        o_ = ot[oi % 3]; oi += 1
                        kb.op('act', lambda a: a.activation(out=o_[:, 0:nt], in_=p_[:, 0:nt], func=(AF.Sigmoid if sg else AF.Identity), bias=bc_[:, 0:1], scale=1.0), R=[p_, bc_], W=[o_])
                        kb.dma('pool', dst[n0:n0 + 128, tb0:tb0 + nt], o_[:, 0:nt], R=[o_])

    def conv_phase(l):
        with Ph() as ph:
            xi = [ph.sb([128, Tn], BF16, "cxi") for _ in range(2)]
            ya = [ph.sb([128, Tn], F32, "cya") for _ in range(2)]
            yo = [ph.sb([128, Tn], BF16, "cyo") for _ in range(2)]
            cw = [ph.sb([128, 4], F32, "ccw") for _ in range(2)]
            for i, c0 in enumerate(range(0, 2 * QKW, 128)):
                x_ = xi[i % 2]; y_ = ya[i % 2]; o_ = yo[i % 2]; w_ = cw[i % 2]
                kb.dma('sp', x_[:], mqk_d[c0:c0 + 128, :], W=[x_])
                kb.dma('sp', w_[:, 0:3], conv_w[l, :, c0:c0 + 128].rearrange("j p -> p j"), W=[w_])
                kb.dma('sp', w_[:, 3:4], conv_b[l, c0:c0 + 128].rearrange("(p o) -> p o", o=1), W=[w_])
                kb.op('dve', lambda v: v.tensor_scalar(out=y_[:], in0=x_[:], scalar1=w_[:, 1:2], scalar2=w_[:, 3:4], op0=ALU.mult, op1=ALU.add), R=[x_, w_], W=[y_])
                for (s0, s1) in [(0, CTX), (CTX, Tn)]:
                    kb.op('dve', lambda v: v.scalar_tensor_tensor(out=y_[:, s0 + 1:s1], in0=x_[:, s0:s1 - 1], scalar=w_[:, 0:1], in1=y_[:, s0 + 1:s1], op0=ALU.mult, op1=ALU.add), R=[x_, w_, y_], W=[y_])
                    kb.op('dve', lambda v: v.scalar_tensor_tensor(out=y_[:, s0:s1 - 1], in0=x_[:, s0 + 1:s1], scalar=w_[:, 2:3], in1=y_[:, s0:s1 - 1], op0=ALU.mult, op1=ALU.add), R=[x_, w_, y_], W=[y_])
                sc = 1.0 if c0 < QKW else DK ** -0.5
                kb.op('act', lambda a: a.activation(out=o_[:], in_=y_[:], func=AF.Sigmoid), R=[y_], W=[o_])
                kb.op('dve', lambda v: v.scalar_tensor_tensor(out=o_[:], in0=y_[:], scalar=sc, in1=o_[:], op0=ALU.mult, op1=ALU.mult), R=[y_, o_], W=[o_])
                kb.dma('pool', mqkc_d[c0:c0 + 128, :], o_[:], R=[o_])

    def qk_phase(l):
        last = (l == DEPTH - 1)
        with Ph() as ph:
            gq = ph.sb([128, HD], F32, "gq"); gk = ph.sb([128, HD], F32, "gk")
            load_rep(gq, g_q[l:l + 1, :], HD); load_rep(gk, g_k[l:l + 1, :], HD)
            NQ = NH + NKV
            qi = [ph.sb([128, NQ, HD], BF16, "qi") for _ in range(2)]
            sq = ph.sb([128, NQ, HD], F32, "sq")
            xn = ph.sb([128, NQ, HD], F32, "xn")
            tmp = ph.sb([128, NQ, 2, 32], F32, "tmp"); tmp2 = ph.sb([128, NQ, 2, 32], F32, "tmp2")
            qo = [ph.sb([128, NQ, HD], BF16, "qo") for _ in range(2)]
            st = [ph.sb([128, 4, NQ], F32, "st") for _ in range(2)]
            cs = [ph.sb([128, 128], F32, "cs") for _ in range(2)]
            ptr = [ph.ps([128, 8, 128], BF16, "ptr") for _ in range(2)]
            oT = [ph.sb([128, NQ, 128], BF16, "oT") for _ in range(2)]
            for t in range(TT):
                q_ = qi[t % 2]; s_ = st[t % 2]; c_ = cs[t % 2]; o_ = qo[t % 2]; oT_ = oT[t % 2]
                kb.dma('sp', q_[:, 0:NH, :], aq_d[t * 128:(t + 1) * 128, :].rearrange("p (h d) -> p h d", d=HD), W=[q_])
                kb.dma('sp', q_[:, NH:NQ, :], ak_d[t * 128:(t + 1) * 128, :].rearrange("p (h d) -> p h d", d=HD), W=[q_])
                kb.op('pool', lambda v: v.tensor_tensor(out=sq[:], in0=q_[:], in1=q_[:], op=ALU.mult), R=[q_], W=[sq])
                kb.op('dve', lambda v: v.tensor_reduce(out=s_[:, 0, :], in_=sq[:], axis=AX.X, op=ALU.add), R=[sq], W=[s_])
                kb.op('dve', lambda v: v.tensor_scalar(out=s_[:, 1, :], in0=s_[:, 0, :], scalar1=1.0 / HD, scalar2=EPS, op0=ALU.mult, op1=ALU.add), R=[s_], W=[s_])
                kb.op('act', lambda a: a.activation(out=s_[:, 2, :], in_=s_[:, 1, :], func=AF.Sqrt), R=[s_], W=[s_])
                kb.op('dve', lambda v: v.reciprocal(out=s_[:, 3, :], in_=s_[:, 2, :]), R=[s_], W=[s_])
                kb.op('dve', lambda v: v.tensor_tensor(out=xn[:], in0=q_[:], in1=s_[:, 3, :].unsqueeze(2).to_broadcast([128, NQ, HD]), op=ALU.mult), R=[q_, s_], W=[xn])
                lat = t >= CT
                dstq = o_ if not lat else xn
                kb.op('dve', lambda v: v.tensor_tensor(out=dstq[:, 0:NH, :], in0=xn[:, 0:NH, :], in1=gq[:].unsqueeze(1).to_broadcast([128, NH, HD]), op=ALU.mult), R=[xn, gq], W=[dstq])
                kb.op('dve', lambda v: v.tensor_tensor(out=dstq[:, NH:NQ, :], in0=xn[:, NH:NQ, :], in1=gk[:].unsqueeze(1).to_broadcast([128, NKV, HD]), op=ALU.mult), R=[xn, gk], W=[dstq])
                if lat:
                    kb.dma('sp', c_[:], rope_cs[(t - CT) * 128:(t - CT + 1) * 128, :], W=[c_])
                    xv = xn[:].rearrange("p h (a b c) -> p h a b c", a=2, b=2)
                    ov = o_[:].rearrange("p h (a b c) -> p h a b c", a=2, b=2)
                    x1 = xv[:, :, :, 0, :]; x2 = xv[:, :, :, 1, :]
                    COS = c_[:, 0:64].rearrange("p (a c) -> p a c", a=2).unsqueeze(1).to_broadcast([128, NQ, 2, 32])
                    SIN = c_[:, 64:128].rearrange("p (a c) -> p a c", a=2).unsqueeze(1).to_broadcast([128, NQ, 2, 32])
                    kb.op('dve', lambda v: v.tensor_tensor(out=tmp[:], in0=x1, in1=COS, op=ALU.mult), R=[xn, c_], W=[tmp])
                    kb.op('pool', lambda v: v.tensor_tensor(out=tmp2[:], in0=x2, in1=SIN, op=ALU.mult), R=[xn, c_], W=[tmp2])
                    kb.op('dve', lambda v: v.tensor_tensor(out=ov[:, :, :, 0, :], in0=tmp[:], in1=tmp2[:], op=ALU.subtract), R=[tmp, tmp2], W=[o_])
                    kb.op('dve', lambda v: v.tensor_tensor(out=tmp[:], in0=x1, in1=SIN, op=ALU.mult), R=[xn, c_], W=[tmp])
                    kb.op('pool', lambda v: v.tensor_tensor(out=tmp2[:], in0=x2, in1=COS, op=ALU.mult), R=[xn, c_], W=[tmp2])
                    kb.op('dve', lambda v: v.tensor_tensor(out=ov[:, :, :, 1, :], in0=tmp[:], in1=tmp2[:], op=ALU.add), R=[tmp, tmp2], W=[o_])
                for k0 in range(0, NQ, 8):
                    p_ = ptr[(k0 // 8) % 2]
                    n = min(NQ, k0 + 8) - k0
                    kb.wait('pe', R=[o_, ident_b], W=[p_])
                    for k in range(k0, k0 + n):
                        ins = nc.tensor.transpose(out=p_[:, k - k0, :], in_=o_[:, k, :], identity=ident_b[:])
                    kb.done('pe', ins, R=[o_, ident_b], W=[p_])
                    kb.op('act', lambda a: a.copy(out=oT_[:, k0:k0 + n, :], in_=p_[:, 0:n, :]), R=[p_], W=[oT_])
                cv_tick(2)
                kb.dma('pool', qT_d[:, :, t * 128:(t + 1) * 128], oT_[:, 0:NH, :], R=[oT_])
                kb.dma('pool', kT_d[:, :, t * 128:(t + 1) * 128], oT_[:, NH:NQ, :], R=[oT_])

    def attn_phase(l):
        last = (l == DEPTH - 1)
        scale = HD ** -0.5
        with Ph() as ph:
            ES = ph.sb([128, NH], F32, "ES")
            load_rep(ES, sink[l:l + 1, :], NH)
            kb.op('act', lambda a: a.activation(out=ES[:], in_=ES[:], func=AF.Exp), R=[ES], W=[ES])
            kc = ph.sb([128, NKV, CTX], BF16, "kc")
            kb.dma('sp', kc[:], kT_d[:, :, 0:CTX], W=[kc])
            vc = ph.sb([128, CT, KW], BF16, "vc")
            kb.dma('sp', vc[:], av_d[0:CTX, :].rearrange("(c p) n -> p c n", p=128), W=[vc])
            qb = [ph.sb([128, NH, 128], BF16, "qb") for _ in range(2)]
            kw = [ph.sb([128, NKV, 384], BF16, "kw") for _ in range(2)]
            vw = [ph.sb([128, 3, KW], BF16, "vw") for _ in range(2)]
            pS = [ph.ps([128, 512], F32, "pS") for _ in range(4)]
            pN = [ph.ps([128, 512], F32, "pN") for _ in range(2)]
            pD = [ph.ps([128, 512], F32, "pD") for _ in range(2)]
            sm = [ph.sb([128, 512], F32, "sm") for _ in range(2)]
            pT = [ph.sb([128, 512], BF16, "pT") for _ in range(3)]
            dn = [ph.sb([128, 512], F32, "dn") for _ in range(2)]
            ob = [ph.sb([128, NH, 128], BF16, "ob") for _ in range(2)]
            blocks = ([] if last else [('c', c) for c in range(CT)]) + [('l', n) for n in range(NB)]
            si = 0; pi = 0
            for bi_, (kind, n) in enumerate(blocks):
                q_ = qb[bi_ % 2]; k_ = kw[bi_ % 2]; v_ = vw[bi_ % 2]; o_ = ob[bi_ % 2]
                tok0 = n * 128 if kind == 'c' else CTX + n * 128
                kb.dma('sp', q_[:], qT_d[:, :, tok0:tok0 + 128], W=[q_])
                chunks = []
                if kind == 'l':
                    lo = max(0, n - 1); hi = min(NB - 1, n + 1)
                    kb.dma('sp', k_[:, :, (lo - n + 1) * 128:(hi - n + 2) * 128], kT_d[:, :, CTX + lo * 128:CTX + (hi + 1) * 128], W=[k_])
                    kb.dma('sp', v_[:, lo - n + 1:hi - n + 2, :], av_d[CTX + lo * 128:CTX + (hi + 1) * 128, :].rearrange("(c p) n -> p c n", p=128), W=[v_])
                    for j in range(lo - n + 1, hi - n + 2):
                        chunks.append((k_, j, v_, j, j))
                for c in range(CT):
                    chunks.append((kc, c, vc, c, None))
                for g in range(NKV):
                    pN_ = pN[g % 2]; pD_ = pD[g % 2]
                    qg = q_[:, g * GRP:(g + 1) * GRP, :].rearrange("p h q -> p (h q)")
                    NQC = GRP * 128
                    nch = len(chunks)
                    psl = {}
                    def score(ci):
                        kt, kj, vt, vj, mj = chunks[ci]
                        pS_ = pS[(si0 + ci) % 4]
                        kb.wait('pe', R=[kt, q_], W=[pS_])
                        ins = nc.tensor.matmul(pS_[:, 0:NQC], lhsT=kt[:, g, kj * 128:(kj + 1) * 128], rhs=qg, start=True, stop=True)
                        kb.done('pe', ins, R=[kt, q_], W=[pS_])
                        psl[ci] = pS_
                    si0 = si; si += nch
                    for ci in range(min(3, nch)):
                        score(ci)
                    for ci, (kt, kj, vt, vj, mj) in enumerate(chunks):
                        if ci + 3 < nch:
                            score(ci + 3)
                        pS_ = psl[ci]
                        p_ = pT[pi % 3]; pi += 1
                        if mj is not None:
                            s_ = sm[ci % 2]
                            kb.op('dve', lambda v: v.tensor_tensor(out=s_[:, 0:NQC].rearrange("p (h q) -> p h q", h=GRP), in0=pS_[:, 0:NQC].rearrange("p (h q) -> p h q", h=GRP), in1=maskT[:, mj * 128:(mj + 1) * 128].unsqueeze(1).to_broadcast([128, GRP, 128]), op=ALU.add), R=[pS_, maskT], W=[s_])
                            kb.op('act', lambda a: a.activation(out=p_[:, 0:NQC], in_=s_[:, 0:NQC], func=AF.Exp, scale=scale), R=[s_], W=[p_])
                        else:
                            kb.op('act', lambda a: a.activation(out=p_[:, 0:NQC], in_=pS_[:, 0:NQC], func=AF.Exp, scale=scale), R=[pS_], W=[p_])
                        first = (ci == 0); lastc = (ci == nch - 1)
                        kb.wait('pe', R=[p_, vt, ones_b], W=[pN_, pD_] if first else [])
                        nc.tensor.matmul(pN_[:, 0:NQC], lhsT=vt[:, vj, g * HD:(g + 1) * HD], rhs=p_[:, 0:NQC], start=first, stop=lastc)
                        ins = nc.tensor.matmul(pD_[:, 0:NQC], lhsT=ones_b[:, :], rhs=p_[:, 0:NQC], start=first, stop=lastc)
                        kb.done('pe', ins, R=[p_, vt, ones_b], W=[pN_, pD_] if lastc else [])
                    d_ = dn[g % 2]
                    kb.op('dve', lambda v: v.tensor_tensor(out=d_[:, 0:NQC].rearrange("p (h q) -> p h q", h=GRP), in0=pD_[:, 0:NQC].rearrange("p (h q) -> p h q", h=GRP), in1=ES[:, g * GRP:(g + 1) * GRP].unsqueeze(2).to_broadcast([128, GRP, 128]), op=ALU.add), R=[pD_, ES], W=[d_])
                    kb.op('dve', lambda v: v.reciprocal(out=d_[:, 0:NQC], in_=d_[:, 0:NQC]), R=[d_], W=[d_])
                    kb.op('dve', lambda v: v.tensor_tensor(out=o_[:, g * GRP:(g + 1) * GRP, :].rearrange("p h q -> p (h q)"), in0=pN_[:, 0:NQC], in1=d_[:, 0:NQC], op=ALU.mult), R=[pN_, d_], W=[o_])
                cv_tick(2)
                kb.dma('pool', atT_d[:, :, tok0:tok0 + 128], o_[:], R=[o_])

    def mlstm_phase(l):
        last = (l == DEPTH - 1)
        NCH = TT
        M2 = 2 * MH
        order_f = list(range(NCH))
        order_b = list(range(CT - 1, -1, -1)) + list(range(NCH - 1, CT - 1, -1))
        with Ph() as ph:
            CS = ph.sb([128, NCH, M2], F32, "CS"); IA = ph.sb([128, NCH, M2], F32, "IA")
            EA = ph.sb([128, NCH, M2], F32, "EA"); WA = ph.sb([128, NCH, M2], F32, "WA")
            Um = [ph.sb([MH, NCH], F32, "Um") for _ in range(2)]; Be = [ph.sb([MH, NCH], F32, "Be") for _ in range(2)]
            Mm = [ph.sb([MH, NCH], F32, "Mm") for _ in range(2)]; Mp = [ph.sb([MH, NCH + 1], F32, "Mp") for _ in range(2)]
            Aa = [ph.sb([MH, NCH], F32, "Aa") for _ in range(2)]
            Mb = ph.sb([128, M2, NCH], F32, "Mb"); Ab = ph.sb([128, M2, NCH], F32, "Ab")
            with ExitStack() as es2:
                def sb2(shape, dt=F32, nm="g"):
                    return T(es2.enter_context(nc.sbuf_tensor(kb.name(nm), list(shape), dt)))
                def ps2(shape, dt=F32, nm="gp"):
                    return TP(es2.enter_context(nc.psum_tensor(kb.name(nm), [128, 512], F32)), list(shape), dt)
                gt = [sb2([128, NGC], F32, "gt") for _ in range(2)]
                spl = [sb2([128, M2], F32, "spl") for _ in range(2)]
                uu = [sb2([128, M2], F32, "uu") for _ in range(2)]
                pc = [ps2([128, M2], F32, "pc") for _ in range(2)]
                pu = [ps2([MH, 2, 128], F32, "pu") for _ in range(2)]
                pb = [ps2([MH, 2], F32, "pb") for _ in range(2)]
                for t in range(NCH):
                    g_ = gt[t % 2]; s_ = spl[t % 2]; u_ = uu[t % 2]; pc_ = pc[t % 2]; pu_ = pu[t % 2]; pb_ = pb[t % 2]
                    kb.dma('sp', g_[:], mg_d[t * 128:(t + 1) * 128, :], W=[g_])
                    kb.op('act', lambda a: a.activation(out=s_[:], in_=g_[:, M2:2 * M2], func=AF.Exp, scale=-1.0), R=[g_], W=[s_])
                    kb.op('act', lambda a: a.activation(out=s_[:], in_=s_[:], func=AF.Ln, bias=1.0, scale=1.0), R=[s_], W=[s_])
                    kb.wait('pe', R=[s_, tri_f, tri_b], W=[pc_])
                    nc.tensor.matmul(pc_[:, 0:MH], lhsT=tri_f[:], rhs=s_[:, 0:MH], start=True, stop=True)
                    ins = nc.tensor.matmul(pc_[:, MH:M2], lhsT=tri_b[:], rhs=s_[:, MH:M2], start=True, stop=True)
                    kb.done('pe', ins, R=[s_, tri_f, tri_b], W=[pc_])
                    kb.op('dve', lambda v: v.tensor_copy(out=CS[:, t, :], in_=pc_[:]), R=[pc_], W=[CS])
                    kb.op('dve', lambda v: v.tensor_copy(out=IA[:, t, :], in_=g_[:, 0:M2]), R=[g_], W=[IA])
                    kb.op('dve', lambda v: v.tensor_tensor(out=u_[:], in0=g_[:, 0:M2], in1=pc_[:], op=ALU.add), R=[g_, pc_], W=[u_])
                    kb.wait('pe', R=[u_, s_, cst_f, ones_f], W=[pu_, pb_])
                    for d in range(2):
                        nc.tensor.matmul(pu_[:, d, :], lhsT=u_[:, d * MH:(d + 1) * MH], rhs=ident_f[:, 0:128], start=True, stop=True)
                    for d in range(2):
                        ins = nc.tensor.matmul(pb_[:, d:d + 1], lhsT=s_[:, d * MH:(d + 1) * MH], rhs=ones_f[:, 0:1], start=True, stop=True)
                    kb.done('pe', ins, R=[u_, s_, cst_f, ones_f], W=[pu_, pb_])
                    for d in range(2):
                        kb.op('dve', lambda v: v.tensor_reduce(out=Um[d][:, t:t + 1], in_=pu_[:, d, :], axis=AX.X, op=ALU.max), R=[pu_], W=[Um[d]])
                        kb.op('dve', lambda v: v.tensor_scalar(out=Be[d][:, t:t + 1], in0=pb_[:, d:d + 1], scalar1=-1.0, scalar2=None, op0=ALU.mult), R=[pb_], W=[Be[d]])
                for d, order in enumerate([order_f, order_b]):
                    kb.op('dve', lambda v: v.memset(Mp[d][:], 0.0), W=[Mp[d]])
                    for k, c in enumerate(order):
                        kb.op('dve', lambda v: v.tensor_tensor(out=Mm[d][:, c:c + 1], in0=Mp[d][:, k:k + 1], in1=Um[d][:, c:c + 1], op=ALU.max), R=[Mp[d], Um[d]], W=[Mm[d]])
                        kb.op('dve', lambda v: v.tensor_tensor(out=Aa[d][:, c:c + 1], in0=Mp[d][:, k:k + 1], in1=Mm[d][:, c:c + 1], op=ALU.subtract), R=[Mp[d], Mm[d]], W=[Aa[d]])
                        kb.op('dve', lambda v: v.tensor_tensor(out=Mp[d][:, k + 1:k + 2], in0=Be[d][:, c:c + 1], in1=Mm[d][:, c:c + 1], op=ALU.add), R=[Be[d], Mm[d]], W=[Mp[d]])
                    kb.op('act', lambda a: a.activation(out=Aa[d][:], in_=Aa[d][:], func=AF.Exp), R=[Aa[d]], W=[Aa[d]])
                X = sb2([MH, MH, NCH], F32, "X")
                pbc = ps2([128, 512], F32, "pbc")
                HPB = max(1, 512 // NCH)
                for d in range(2):
                    for (src, dstb) in [(Mm[d], Mb), (Aa[d], Ab)]:
                        kb.op('dve', lambda v: v.tensor_tensor(out=X[:], in0=ident_f[0:MH, 0:MH].unsqueeze(2).to_broadcast([MH, MH, NCH]), in1=src[:].unsqueeze(1).to_broadcast([MH, MH, NCH]), op=ALU.mult), R=[cst_f, src], W=[X])
                        for r0 in range(0, MH, HPB):
                            nr = min(HPB, MH - r0)
                            kb.wait('pe', R=[X, ones_f], W=[pbc])
                            ins = nc.tensor.matmul(pbc[:, 0:nr * NCH], lhsT=ones_f[0:MH, :], rhs=X[:, r0:r0 + nr, :].rearrange("q r c -> q (r c)"), start=True, stop=True)
                            kb.done('pe', ins, R=[X, ones_f], W=[pbc])
                            kb.op('dve', lambda v: v.tensor_copy(out=dstb[:, d * MH + r0:d * MH + r0 + nr, :].rearrange("p r c -> p (r c)"), in_=pbc[:, 0:nr * NCH]), R=[pbc], W=[dstb])
                kb.op('dve', lambda v: v.tensor_tensor(out=EA[:], in0=CS[:], in1=Mb[:].rearrange("p r c -> p c r"), op=ALU.subtract), R=[CS, Mb], W=[EA])
                kb.op('act', lambda a: a.activation(out=EA[:], in_=EA[:], func=AF.Exp), R=[EA], W=[EA])
                kb.op('act', lambda a: a.activation(out=WA[:], in_=IA[:], func=AF.Exp), R=[IA], W=[WA])
                kb.op('dve', lambda v: v.tensor_tensor(out=WA[:], in0=WA[:], in1=EA[:], op=ALU.mult), R=[WA, EA], W=[WA])
                kb.barrier()
            qT = [ph.sb([128, MH, 128], BF16, "mq") for _ in range(2)]
            kT = [ph.sb([128, MH, 128], BF16, "mk") for _ in range(2)]
            va = [ph.sb([128, MH, DV + 1], BF16, "va") for _ in range(2)]
            for v_ in va:
                kb.op('dve', lambda v: v.memset(v_[:], 1.0), W=[v_])
            Cn = [ph.sb([128, DV + 1], F32, "Cn") for _ in range(MH)]
            Cb = [ph.sb([128, DV + 1], BF16, "Cb") for _ in range(MH)]
            pk = [ph.ps([128, 128], BF16, "pk") for _ in range(2)]
            pst = [ph.ps([128, 128], F32, "pst") for _ in range(2)]
            pnum = [ph.ps([128, DV + 1], F32, "pnum") for _ in range(2)]
            pup = [ph.ps([128, DV + 1], F32, "pup") for _ in range(2)]
            ksc = [ph.sb([128, 128], BF16, "ksc") for _ in range(2)]
            Sp = [ph.sb([128, 128], BF16, "Sp") for _ in range(2)]
            dd = [ph.sb([128, 2], F32, "dd") for _ in range(2)]
            Ho = [ph.sb([128, MH, DV], BF16, "Ho") for _ in range(2)]
            it = 0
            for d, order in enumerate([order_f, order_b]):
                tri = tri_fb if d == 0 else tri_bb
                hdst = hf_d if d == 0 else hb_d
                for h in range(MH):
                    kb.op('dve', lambda v: v.memset(Cn[h][:], 0.0), W=[Cn[h]])
                for k, c in enumerate(order):
                    q_ = qT[k % 2]; k_ = kT[k % 2]; v_ = va[k % 2]; H_ = Ho[k % 2]
                    kb.dma('sp', q_[:], mqkc_d[0:QKW, c * 128:(c + 1) * 128].rearrange("(h d) t -> d h t", d=DK), W=[q_])
                    kb.dma('sp', k_[:], mqkc_d[QKW:2 * QKW, c * 128:(c + 1) * 128].rearrange("(h d) t -> d h t", d=DK), W=[k_])
                    kb.dma('sp', v_[:, :, 0:DV], mv_d[c * 128:(c + 1) * 128, :].rearrange("p (h v) -> p h v", v=DV), W=[v_])
                    skip_out = last and c < CT
                    for h in range(MH):
                        r = d * MH + h
                        i2 = it % 2; it += 1
                        kb.op('dve', lambda v: v.tensor_scalar(out=Cn[h][:], in0=Cn[h][:], scalar1=Ab[:, r, c:c + 1], scalar2=None, op0=ALU.mult), R=[Cn[h], Ab], W=[Cn[h]])
                        kb.op('pool', lambda v: v.tensor_copy(out=Cb[h][:], in_=Cn[h][:]), R=[Cn[h]], W=[Cb[h]])
                        kb.wait('pe', R=[k_, ident_b], W=[pk[i2]])
                        ins = nc.tensor.transpose(out=pk[i2][:], in_=k_[:, h, :], identity=ident_b[:])
                        kb.done('pe', ins, R=[k_, ident_b], W=[pk[i2]])
                        kb.op('dve', lambda v: v.tensor_scalar(out=ksc[i2][:], in0=pk[i2][:], scalar1=WA[:, c, r:r + 1], scalar2=None, op0=ALU.mult), R=[pk[i2], WA], W=[ksc[i2]])
                        if not skip_out:
                            kb.wait('pe', R=[k_, q_], W=[pst[i2]])
                            ins = nc.tensor.matmul(pst[i2][:], lhsT=k_[:, h, :], rhs=q_[:, h, :], start=True, stop=True)
                            kb.done('pe', ins, R=[k_, q_], W=[pst[i2]])
                            kb.op('dve', lambda v: v.scalar_tensor_tensor(out=Sp[i2][:], in0=pst[i2][:], scalar=WA[:, c, r:r + 1], in1=tri[:], op0=ALU.mult, op1=ALU.mult), R=[pst[i2], WA, tri], W=[Sp[i2]])
                            kb.wait('pe', R=[Sp[i2], v_, q_, Cb[h]], W=[pnum[i2]])
                            nc.tensor.matmul(pnum[i2][:], lhsT=Sp[i2][:], rhs=v_[:, h, :], start=True, stop=False)
                            ins = nc.tensor.matmul(pnum[i2][:], lhsT=q_[:, h, :], rhs=Cb[h][:], start=False, stop=True)
                            kb.done('pe', ins, R=[Sp[i2], v_, q_, Cb[h]], W=[pnum[i2]])
                            kb.op('dve', lambda v: v.tensor_scalar(out=dd[i2][:, 0:1], in0=pnum[i2][:, DV:DV + 1], scalar1=-1.0, scalar2=None, op0=ALU.mult), R=[pnum[i2]], W=[dd[i2]])
                            kb.op('dve', lambda v: v.tensor_tensor(out=dd[i2][:, 0:1], in0=dd[i2][:, 0:1], in1=pnum[i2][:, DV:DV + 1], op=ALU.max), R=[pnum[i2], dd[i2]], W=[dd[i2]])
                            kb.op('dve', lambda v: v.tensor_tensor(out=dd[i2][:, 0:1], in0=dd[i2][:, 0:1], in1=EA[:, c, r:r + 1], op=ALU.max), R=[dd[i2], EA], W=[dd[i2]])
                            kb.op('dve', lambda v: v.reciprocal(out=dd[i2][:, 1:2], in_=dd[i2][:, 0:1]), R=[dd[i2]], W=[dd[i2]])
                            kb.op('act', lambda a: a.activation(out=H_[:, h, :], in_=pnum[i2][:, 0:DV], func=AF.Copy, scale=dd[i2][:, 1:2]), R=[pnum[i2], dd[i2]], W=[H_])
                        kb.wait('pe', R=[ksc[i2], v_], W=[pup[i2]])
                        ins = nc.tensor.matmul(pup[i2][:], lhsT=ksc[i2][:], rhs=v_[:, h, :], start=True, stop=True)
                        kb.done('pe', ins, R=[ksc[i2], v_], W=[pup[i2]])
                        kb.op('dve', lambda v: v.tensor_tensor(out=Cn[h][:], in0=Cn[h][:], in1=pup[i2][:], op=ALU.add), R=[Cn[h], pup[i2]], W=[Cn[h]])
                    if not skip_out:
                        kb.dma('pool', hdst[c * 128:(c + 1) * 128, :].rearrange("p (h v) -> p h v", v=DV), H_[:], R=[H_])

    def hm_phase(l):
        last = (l == DEPTH - 1)
        MC = MW // 128
        with Ph() as ph:
            gm = ph.sb([128, MW], F32, "gmh")
            load_rep(gm, g_mh[l:l + 1, :], MW)
            hf = [ph.sb([128, MW], BF16, "hf") for _ in range(2)]; hb = [ph.sb([128, MW], BF16, "hb") for _ in range(2)]
            mo = [ph.sb([128, MW], BF16, "mo") for _ in range(2)]
            sg = ph.sb([128, MW], F32, "sg"); hs = ph.sb([128, MW], F32, "hs"); sq = ph.sb([128, MW], F32, "sq")
            ho = [ph.sb([128, MW], BF16, "ho") for _ in range(2)]
            st = [ph.sb([128, 4, MH], F32, "st") for _ in range(2)]
            ptr = [ph.ps([128, 8, 128], BF16, "ptr") for _ in range(2)]
            oT = [ph.sb([128, MC, 128], BF16, "oT") for _ in range(2)]
            for t in range(CT if last else 0, TT):
                f_ = hf[t % 2]; b_ = hb[t % 2]; m_ = mo[t % 2]; o_ = ho[t % 2]; s_ = st[t % 2]; oT_ = oT[t % 2]
                kb.dma('sp', f_[:], hf_d[t * 128:(t + 1) * 128, :], W=[f_])
                kb.dma('sp', b_[:], hb_d[t * 128:(t + 1) * 128, :], W=[b_])
                kb.dma('sp', m_[:], mo_d[t * 128:(t + 1) * 128, :], W=[m_])
                kb.op('act', lambda a: a.activation(out=sg[:], in_=m_[:], func=AF.Sigmoid), R=[m_], W=[sg])
                kb.op('pool', lambda v: v.tensor_tensor(out=hs[:], in0=f_[:], in1=b_[:], op=ALU.add), R=[f_, b_], W=[hs])
                kb.op('dve', lambda v: v.tensor_tensor(out=hs[:], in0=hs[:], in1=sg[:], op=ALU.mult), R=[hs, sg], W=[hs])
                kb.op('pool', lambda v: v.tensor_tensor(out=sq[:], in0=hs[:], in1=hs[:], op=ALU.mult), R=[hs], W=[sq])
                kb.op('dve', lambda v: v.tensor_reduce(out=s_[:, 0, :], in_=sq[:].rearrange("p (h v) -> p h v", v=DV), axis=AX.X, op=ALU.add), R=[sq], W=[s_])
                kb.op('dve', lambda v: v.tensor_scalar(out=s_[:, 1, :], in0=s_[:, 0, :], scalar1=1.0 / DV, scalar2=EPS, op0=ALU.mult, op1=ALU.add), R=[s_], W=[s_])
                kb.op('act', lambda a: a.activation(out=s_[:, 2, :], in_=s_[:, 1, :], func=AF.Sqrt), R=[s_], W=[s_])
                kb.op('dve', lambda v: v.reciprocal(out=s_[:, 3, :], in_=s_[:, 2, :]), R=[s_], W=[s_])
                kb.op('dve', lambda v: v.tensor_tensor(out=hs[:].rearrange("p (h v) -> p h v", v=DV), in0=hs[:].rearrange("p (h v) -> p h v", v=DV), in1=s_[:, 3, :].unsqueeze(2).to_broadcast([128, MH, DV]), op=ALU.mult), R=[hs, s_], W=[hs])
                kb.op('dve', lambda v: v.tensor_tensor(out=o_[:], in0=hs[:], in1=gm[:], op=ALU.mult), R=[hs, gm], W=[o_])
                for k0 in range(0, MC, 8):
                    p_ = ptr[(k0 // 8) % 2]
                    n = min(MC, k0 + 8) - k0
                    kb.wait('pe', R=[o_, ident_b], W=[p_])
                    for k in range(k0, k0 + n):
                        ins = nc.tensor.transpose(out=p_[:, k - k0, :], in_=o_[:, k * 128:(k + 1) * 128], identity=ident_b[:])
                    kb.done('pe', ins, R=[o_, ident_b], W=[p_])
                    kb.op('act', lambda a: a.copy(out=oT_[:, k0:k0 + n, :], in_=p_[:, 0:n, :]), R=[p_], W=[oT_])
                cv_tick(1)
                kb.dma('pool', hmT_d[:, :, t * 128:(t + 1) * 128], oT_[:], R=[oT_])

    def merge_phase(l):
        last = (l == DEPTH - 1)
        NTB = 512
        AC = AW // 128; MC = MW // 128
        with Ph() as ph:
            gts = [ph.sb([128, 512], F32, "gts") for _ in range(3)]
            aT = ph.sb([128, AC, NTB], BF16, "aT"); mT = ph.sb([128, MC, NTB], BF16, "mT")
            wa = [ph.sb([128, AC, 128], BF16, "wa") for _ in range(2)]; wm = [ph.sb([128, MC, 128], BF16, "wm") for _ in range(2)]
            ga = [ph.sb([128, NTB], BF16, "ga") for _ in range(2)]; gmm = [ph.sb([128, NTB], BF16, "gmm") for _ in range(2)]
            pa = [ph.ps([128, NTB], F32, "pa") for _ in range(2)]; pm = [ph.ps([128, NTB], F32, "pm") for _ in range(2)]
            t1 = [ph.sb([128, NTB], F32, "t1") for _ in range(2)]
            mg = ph.sb([128, KC, NTB], BF16, "mg")
            wo = [ph.sb([128, KC, 512], BF16, "wo") for _ in range(2)]
            po = [ph.ps([128, 512], F32, "po") for _ in range(2)]
            xt = [ph.sb([128, 512], F32, "xt") for _ in range(3)]
            wi = 0; xi = 0; pi = 0
            tbs = list(range(0, Tn, NTB))
            for tb0 in tbs:
                nt = min(NTB, Tn - tb0)
                kb.dma('sp', aT[:, :, 0:nt], atT_d[:, :, tb0:tb0 + nt], W=[aT])
                kb.dma('sp', mT[:, :, 0:nt], hmT_d[:, :, tb0:tb0 + nt], W=[mT])
                for j in range(KC):
                    a_ = wa[j % 2]; m_ = wm[j % 2]; ga_ = ga[j % 2]; gm_ = gmm[j % 2]; pa_ = pa[j % 2]; pm_ = pm[j % 2]; t_ = t1[j % 2]
                    kb.dma('sp', a_[:], WS[l % NWS]['wba_b'][:, j, :, :], W=[a_])
                    kb.dma('sp', m_[:], WS[l % NWS]['wbm_b'][:, j, :, :], W=[m_])
                    kb.dma('sp', ga_[:, 0:nt], sga_d[j * 128:(j + 1) * 128, tb0:tb0 + nt], W=[ga_])
                    kb.dma('sp', gm_[:, 0:nt], sgm_d[j * 128:(j + 1) * 128, tb0:tb0 + nt], W=[gm_])
                    kb.wait('pe', R=[a_, aT], W=[pa_])
                    for k in range(AC):
                        ins = nc.tensor.matmul(pa_[:, 0:nt], lhsT=a_[:, k, :], rhs=aT[:, k, 0:nt], start=(k == 0), stop=(k == AC - 1))
                    kb.done('pe', ins, R=[a_, aT], W=[pa_])
                    kb.wait('pe', R=[m_, mT], W=[pm_])
                    for k in range(MC):
                        ins = nc.tensor.matmul(pm_[:, 0:nt], lhsT=m_[:, k, :], rhs=mT[:, k, 0:nt], start=(k == 0), stop=(k == MC - 1))
                    kb.done('pe', ins, R=[m_, mT], W=[pm_])
                    kb.op('dve', lambda v: v.tensor_tensor(out=t_[:, 0:nt], in0=pa_[:, 0:nt], in1=ga_[:, 0:nt], op=ALU.mult), R=[pa_, ga_], W=[t_])
                    kb.op('dve', lambda v: v.tensor_tensor(out=mg[:, j, 0:nt], in0=pm_[:, 0:nt], in1=gm_[:, 0:nt], op=ALU.mult), R=[pm_, gm_], W=[mg])
                    kb.op('pool', lambda v: v.tensor_tensor(out=mg[:, j, 0:nt], in0=mg[:, j, 0:nt], in1=t_[:, 0:nt], op=ALU.add), R=[mg, t_], W=[mg])
                for n0 in range(0, D, 512):
                    w_ = wo[wi % 2]; wi += 1
                    kb.dma('sp', w_[:], WS[l % NWS]['wo_b'][:, n0 // 512, :, :], W=[w_])
                    for tt in range(nt // 128):
                        tg = (tb0 // 128) + tt
                        if last and tg < CT:
                            continue
                        r = 1 if tg < CT else 0
                        p_ = po[pi % 2]; pi += 1
                        x_ = xt[xi % 3]; xi += 1
                        kb.dma('sp', x_[:], xs[tg * 128:(tg + 1) * 128, n0:n0 + 512], W=[x_])
                        g_ = gts[xi % 3]
                        kb.dma('sp', g_[:], mod_d[l, r:r + 1, 2 * D + n0:2 * D + n0 + 512].to_broadcast([128, 512]), W=[g_])
                        kb.wait('pe', R=[mg, w_], W=[p_])
                        for k in range(KC):
                            ins = nc.tensor.matmul(p_[:], lhsT=mg[:, k, tt * 128:(tt + 1) * 128], rhs=w_[:, k, :], start=(k == 0), stop=(k == KC - 1))
                        kb.done('pe', ins, R=[mg, w_], W=[p_])
                        t_ = t1[pi % 2]
                        kb.op('dve', lambda v: v.tensor_tensor(out=t_[:, 0:512], in0=p_[:], in1=g_[:], op=ALU.mult), R=[p_, g_], W=[t_])
                        kb.op('pool', lambda v: v.tensor_tensor(out=x_[:], in0=x_[:], in1=t_[:, 0:512], op=ALU.add), R=[x_, t_], W=[x_])
                        kb.dma('pool', xs[tg * 128:(tg + 1) * 128, n0:n0 + 512], x_[:], R=[x_])

    def moe_phase(l):
        last = (l == DEPTH - 1)
        NTB = 512
        FC = FF // 128
        with Ph() as ph:
            gts = [ph.sb([128, 512], F32, "gts") for _ in range(2)]
            hTb = ph.sb([128, KC, NTB], BF16, "hTb")
            yacc = ph.sb([128, NTB // 128, D], F32, "yacc")
            wg = [ph.sb([128, KC, 128], BF16, "wg") for _ in range(2)]; wu = [ph.sb([128, KC, 128], BF16, "wu") for _ in range(2)]
            wd = [ph.sb([128, FC, 512], BF16, "wd") for _ in range(2)]
            pg = [ph.ps([128, NTB], F32, "pg") for _ in range(2)]; pu = [ph.ps([128, NTB], F32, "pu") for _ in range(2)]
            pdn = [ph.ps([128, 512], F32, "pdn") for _ in range(4)]
            sg = [ph.sb([128, NTB], F32, "sg") for _ in range(2)]
            aT = [ph.sb([128, FC, NTB], BF16, "aT") for _ in range(2)]
            xt = [ph.sb([128, 512], F32, "xt") for _ in range(2)]
            gi = 0; di = 0; xi = 0
            t_start = CTX if last else 0
            for tb0 in range(t_start, Tn, NTB):
                nt = min(NTB, Tn - tb0)
                ntl = nt // 128
                kb.dma('sp', hTb[:, :, 0:nt], hT_d[:, :, tb0:tb0 + nt], W=[hTb])
                kb.op('pool', lambda v: v.memset(yacc[:], 0.0), W=[yacc])
                for e in range(NE):
                    a_ = aT[e % 2]
                    for j in range(FC):
                        g_ = wg[gi % 2]; u_ = wu[gi % 2]; pg_ = pg[gi % 2]; pu_ = pu[gi % 2]; s_ = sg[gi % 2]; gi += 1
                        kb.dma('sp', g_[:], WS[l % NWS]['wg_b'][e, :, j, :, :], W=[g_])
                        cv_tick()
                        kb.dma('sp', u_[:], WS[l % NWS]['wu_b'][e, :, j, :, :], W=[u_])
                        kb.wait('pe', R=[g_, hTb], W=[pg_])
                        for k in range(KC):
                            ins = nc.tensor.matmul(pg_[:, 0:nt], lhsT=g_[:, k, :], rhs=hTb[:, k, 0:nt], start=(k == 0), stop=(k == KC - 1))
                        kb.done('pe', ins, R=[g_, hTb], W=[pg_])
                        kb.wait('pe', R=[u_, hTb], W=[pu_])
                        for k in range(KC):
                            ins = nc.tensor.matmul(pu_[:, 0:nt], lhsT=u_[:, k, :], rhs=hTb[:, k, 0:nt], start=(k == 0), stop=(k == KC - 1))
                        kb.done('pe', ins, R=[u_, hTb], W=[pu_])
                        kb.op('act', lambda a: a.activation(out=s_[:, 0:nt], in_=pg_[:, 0:nt], func=AF.Sigmoid), R=[pg_], W=[s_])
                        kb.op('dve', lambda v: v.tensor_tensor(out=s_[:, 0:nt], in0=s_[:, 0:nt], in1=pg_[:, 0:nt], op=ALU.mult), R=[s_, pg_], W=[s_])
                        kb.op('dve', lambda v: v.tensor_tensor(out=a_[:, j, 0:nt], in0=s_[:, 0:nt], in1=pu_[:, 0:nt], op=ALU.mult), R=[s_, pu_], W=[a_])
                    for n0 in range(0, D, 512):
                        d_ = wd[(di // max(1, ntl)) % 2]
                        kb.dma('sp', d_[:], WS[l % NWS]['wd_b'][e, :, n0 // 512, :, :], W=[d_])
                        cv_tick()
                        for tt in range(ntl):
                            tg = tb0 // 128 + tt
                            p_ = pdn[di % 4]; di += 1
                            kb.wait('pe', R=[a_, d_], W=[p_])
                            for k in range(FC):
                                ins = nc.tensor.matmul(p_[:], lhsT=a_[:, k, tt * 128:(tt + 1) * 128], rhs=d_[:, k, :], start=(k == 0), stop=(k == FC - 1))
                            kb.done('pe', ins, R=[a_, d_], W=[p_])
                            kb.op('dve', lambda v: v.scalar_tensor_tensor(out=yacc[:, tt, n0:n0 + 512], in0=p_[:], scalar=comb[:, tg, e:e + 1], in1=yacc[:, tt, n0:n0 + 512], op0=ALU.mult, op1=ALU.add), R=[p_, comb, yacc], W=[yacc])
                for tt in range(ntl):
                    tg = tb0 // 128 + tt
                    r = 1 if tg < CT else 0
                    for n0 in range(0, D, 512):
                        x_ = xt[xi % 2]; g_ = gts[xi % 2]; xi += 1
                        kb.dma('sp', x_[:], xs[tg * 128:(tg + 1) * 128, n0:n0 + 512], W=[x_])
                        kb.dma('sp', g_[:], mod_d[l, r:r + 1, 5 * D + n0:5 * D + n0 + 512].to_broadcast([128, 512]), W=[g_])
                        kb.op('dve', lambda v: v.tensor_tensor(out=yacc[:, tt, n0:n0 + 512], in0=yacc[:, tt, n0:n0 + 512], in1=g_[:], op=ALU.mult), R=[yacc, g_], W=[yacc])
                        kb.op('pool', lambda v: v.tensor_tensor(out=x_[:], in0=x_[:], in1=yacc[:, tt, n0:n0 + 512], op=ALU.add), R=[x_, yacc], W=[x_])
                        if last:
                            kb.dma('pool', y_out[(tg - CT) * 128:(tg - CT + 1) * 128, n0:n0 + 512], x_[:], R=[x_])
                        else:
                            kb.dma('pool', xs[tg * 128:(tg + 1) * 128, n0:n0 + 512], x_[:], R=[x_])

    import os
    stop = int(os.environ.get("KSTOP", "999"))
    phs = []
    OVL = os.environ.get("KOVL", "1") == "1"

    def convert_layer(l):
        if l == 0:
            pending.append(convert_gen(0, None, "A" if OVL else "AB"))
            cv_flush()
            if OVL:
                pending.append(convert_gen(0, 'pool', "B"))
        else:
            if not OVL:
                pending.append(convert_gen(l, None, "AB"))
            cv_flush()

    def pre_merge(l):
        if OVL:
            while pending and not ready.get((l, 'm')):
                cv_tick()
            kb.barrier()

    def pre_moe(l):
        if OVL:
            cv_flush()
            if l + 1 < DEPTH:
                pending.append(convert_gen(l + 1, 'pool', "AB"))

    for l in range(DEPTH):
        phs += [lambda l=l: convert_layer(l), lambda l=l: adaln(l), lambda l=l: norm_phase(l, 0), lambda l=l: proj_phase(l),
                lambda l=l: conv_phase(l), lambda l=l: qk_phase(l), lambda l=l: attn_phase(l), lambda l=l: mlstm_phase(l),
                lambda l=l: hm_phase(l), lambda l=l: (pre_merge(l), merge_phase(l)), lambda l=l: norm_phase(l, 1), lambda l=l: (pre_moe(l), moe_phase(l))]
    for i, p in enumerate(phs):
        if i < stop:
            p()
    kb.barrier()
    dbg = [d for d in os.environ.get("KDBG", "").split(",") if d]
    scr = dict(xs=xs, hT_d=hT_d, mod_d=mod_d, aq_d=aq_d, ak_d=ak_d, av_d=av_d, mv_d=mv_d, mo_d=mo_d, mg_d=mg_d, mqk_d=mqk_d,
               mqkc_d=mqkc_d, sga_d=sga_d, sgm_d=sgm_d, qT_d=qT_d, kT_d=kT_d, atT_d=atT_d, hf_d=hf_d, hb_d=hb_d, hmT_d=hmT_d)
    for d in dbg:
        src = scr[d]
        o = nc.dram_tensor("dbg_" + d, list(src.shape), src.dtype, kind="ExternalOutput").ap()
        if len(src.shape) == 3:
            for i in range(src.shape[0]):
                kb.dma('sp', o[i], src[i])
        else:
            kb.dma('sp', o[:, :], src[:, :])
    if "comb" in os.environ.get("KDBG2", ""):
        o = nc.dram_tensor("dbg_comb", [128, TT * NE], F32, kind="ExternalOutput").ap()
        kb.dma('sp', o[:, :], comb[:].rearrange("p t e -> p (t e)"), R=[comb])
    kb.barrier()
    pes.__exit__(None, None, None)
    return nc


def host_consts(cfg):
    NLAT = cfg['NLAT']; GW = cfg['GRID_W']
    rp = 32
    inv = (10000.0 ** (-np.arange(rp, dtype=np.float32) / rp)).astype(np.float32)
    t = np.arange(NLAT)
    ar = (t // GW).astype(np.float32)[:, None] * inv
    ac = (t % GW).astype(np.float32)[:, None] * inv
    rope = np.concatenate([np.cos(ar), np.cos(ac), np.sin(ar), np.sin(ac)], axis=1).astype(np.float32)
    ident = np.eye(128, dtype=np.float32)
    i = np.arange(128)[None, :]; j = np.arange(128)[:, None]
    m = []
    for c in (-1, 0, 1):
        rel = (c * 128 + j) - i
        m.append(np.where(np.abs(rel) <= 128, 0.0, NEG).astype(np.float32))
    trif = (j <= i).astype(np.float32)
    cst = np.concatenate([ident] + m + [trif], axis=1).astype(np.float32)
    return rope, cst


def make_in_maps(cfg, inp):
    D = cfg['D']; MH = cfg['MH']; NH = cfg['NH']; NKV = cfg['NKV']
    AW = NH * 128; KW = NKV * 128; QKW = MH * cfg['DK']; MW = MH * cfg['DV']
    o_mg = AW + 2 * KW + 2 * QKW + 2 * MW
    perm = np.arange(inp['w_in'].shape[-1])
    g = np.arange(4 * MH).reshape(4, MH)
    perm[o_mg:o_mg + 4 * MH] = o_mg + np.concatenate([g[0], g[2], g[1], g[3]])
    w_in = np.ascontiguousarray(inp['w_in'][:, :, perm]); b_in = np.ascontiguousarray(inp['b_in'][:, perm])
    rope, cst = host_consts(cfg)
    f = lambda a: np.ascontiguousarray(np.asarray(a, dtype=np.float32))
    maps = []
    for b in range(inp['x'].shape[0]):
        maps.append({
            'x': f(inp['x'][b]), 'ctx': f(inp['ctx'][b]), 'cc': f(np.stack([inp['c'][b], inp['c_ctx']], 0)),
            'w_ada': f(inp['w_ada']), 'b_ada': f(inp['b_ada']), 'g_mix': f(inp['g_mix']), 'g_ffn': f(inp['g_ffn']),
            'w_in': f(w_in), 'b_in': f(b_in), 'g_q': f(inp['g_q']), 'g_k': f(inp['g_k']), 'sink': f(inp['sink']),
            'conv_w': f(inp['conv_w']), 'conv_b': f(inp['conv_b']), 'g_mh': f(inp['g_mh']),
            'w_br_attn': f(inp['w_br_attn']), 'w_br_mlstm': f(inp['w_br_mlstm']), 'w_out': f(inp['w_out']),
            'w_router': f(inp['w_router']), 'b_router': f(np.asarray(inp['b_router']).reshape(1, -1)),
            'w_gate': f(inp['w_gate']), 'w_up': f(inp['w_up']), 'w_down': f(inp['w_down']),
            'rope_cs': rope, 'cst': cst,
        })
    return maps


def run(cfg, inp, trace=False):
    nc = build(cfg)
    maps = make_in_maps(cfg, inp)
    res = run_bass_kernel_spmd(nc, maps, core_ids=list(range(len(maps))), trace=trace)
    out = np.stack([res.results[b]['y'] for b in range(len(maps))], 0).astype(np.float32)
    return out, res


def kernel(**inputs):
    inp = {k: np.asarray(v) for k, v in inputs.items()}
    out, _ = run(CFG_FULL, inp)
    return out
```

```python
from contextlib import ExitStack
import os
import numpy as np
import concourse.bass as bass
import concourse.mybir as mybir
from concourse.bass_utils import run_bass_kernel_spmd

F32 = mybir.dt.float32
BF16 = mybir.dt.bfloat16
ALU = mybir.AluOpType
AF = mybir.ActivationFunctionType
AX = mybir.AxisListType
EPS = 1e-6
NEG = -1e30

CFG_FULL = dict(D=4096, NLAT=8192, CTX=256, DEPTH=2, NH=16, NKV=4, MH=8, DK=128, DV=256, NE=16, NG=4, FF=1024, GRID_W=64)


class T:
    psum = False
    def __init__(s, h):
        s.h = h; s.lw = None; s.rd = {}
    def __getitem__(s, i):
        return s.h[i]


class TP(T):
    psum = True
    def __init__(s, h, shape, dt):
        T.__init__(s, h)
        v = h[:]
        if dt != F32:
            v = v.bitcast(dt)
        n = 1
        for d in shape[1:]:
            n *= d
        v = v[0:shape[0], 0:n]
        if len(shape) == 3:
            v = v.rearrange("p (a b) -> p a b", a=shape[1])
        s.view = v
    def __getitem__(s, i):
        return s.view[i]


class KB:
    NDS = 12
    def __init__(s, nc):
        s.nc = nc
        s.eng = {'pe': nc.tensor, 'act': nc.scalar, 'dve': nc.vector, 'pool': nc.gpsimd, 'sp': nc.sync}
        s.sem = {e: nc.alloc_semaphore('s_' + e) for e in ['pe', 'act', 'dve', 'pool']}
        s.cnt = {e: 0 for e in s.sem}
        s.seen = {e: {} for e in s.eng}
        s.dq = ['sp', 'pool']
        s.dsem = {q: [nc.alloc_semaphore(f'd_{q}{i}') for i in range(s.NDS)] for q in s.dq}
        s.dcnt = {q: [0] * s.NDS for q in s.dq}
        s.dnext = {q: 0 for q in s.dq}
        s.uid = 0

    def semobj(s, key):
        return s.sem[key[1]] if key[0] == 'c' else s.dsem[key[1]][key[2]]

    def need(s, e, ev):
        if ev is None:
            return
        key, val = ev
        if val <= 0 or s.seen[e].get(key, 0) >= val:
            return
        if key == ('c', 'pe') and e == 'pe':
            return
        s.eng[e].wait_ge(s.semobj(key), val)
        s.seen[e][key] = val

    def wait(s, e, R=(), W=()):
        for t in R:
            s.need(e, t.lw)
            if t.psum:
                for k, v in t.rd.items():
                    if k != ('c', e):
                        s.need(e, (k, v))
        for t in W:
            s.need(e, t.lw)
            for k, v in t.rd.items():
                s.need(e, (k, v))

    def done(s, e, ins, R=(), W=()):
        s.cnt[e] += 1
        ins.then_inc(s.sem[e], 1)
        key = ('c', e); val = s.cnt[e]
        for t in W:
            t.lw = (key, val); t.rd = {}
        for t in R:
            t.rd[key] = val

    def op(s, e, f, R=(), W=()):
        s.wait(e, R, W)
        ins = f(s.eng[e])
        s.done(e, ins, R, W)
        return ins

    def dma(s, q, out, in_, R=(), W=()):
        s.wait(q, R, W)
        k = s.dnext[q]; s.dnext[q] = (k + 1) % s.NDS
        key = ('d', q, k)
        s.need(q, (key, s.dcnt[q][k]))
        ins = s.eng[q].dma_start(out=out, in_=in_)
        s.dcnt[q][k] += 16
        ins.then_inc(s.dsem[q][k], 16)
        val = s.dcnt[q][k]
        for t in W:
            t.lw = (key, val); t.rd = {}
        for t in R:
            t.rd[key] = val

    def barrier(s):
        for e in s.eng:
            for c in s.sem:
                s.need(e, (('c', c), s.cnt[c]))
            for q in s.dq:
                for k in range(s.NDS):
                    s.need(e, (('d', q, k), s.dcnt[q][k]))

    def name(s, p):
        s.uid += 1
        return f"{p}_{s.uid}"


def build(cfg, need_out_ctx=False):
    D = cfg['D']; NLAT = cfg['NLAT']; CTX = cfg['CTX']; DEPTH = cfg['DEPTH']
    NH = cfg['NH']; NKV = cfg['NKV']; MH = cfg['MH']; DK = cfg['DK']; DV = cfg['DV']
    NE = cfg['NE']; NG = cfg['NG']; FF = cfg['FF']
    HD = 128
    GRP = NH // NKV
    AW = NH * HD; KW = NKV * HD; QKW = MH * DK; MW = MH * DV; NGC = 4 * MH
    DIN = AW + 2 * KW + 2 * QKW + 2 * MW + NGC + 2 * D
    Tn = CTX + NLAT; TT = Tn // 128; CT = CTX // 128; KC = D // 128
    NB = NLAT // 128
    EPG = NE // NG
    o_aq = 0; o_ak = AW; o_av = AW + KW; o_mq = AW + 2 * KW; o_mk = o_mq + QKW; o_mv = o_mk + QKW
    o_mo = o_mv + MW; o_mg = o_mo + MW; o_ga = o_mg + NGC; o_gm = o_ga + D

    nc = bass.Bass("TRN2", target_bir_lowering=False)
    kb = KB(nc)

    def din(name, shape, dt=F32):
        return nc.dram_tensor(name, list(shape), dt, kind="ExternalInput").ap()

    def dsc(name, shape, dt):
        return nc.dram_tensor(name, list(shape), dt, kind="Internal").ap()

    x_in = din("x", [NLAT, D]); ctx_in = din("ctx", [CTX, D]); cc_in = din("cc", [2, D])
    w_ada = din("w_ada", [DEPTH, D, 6 * D]); b_ada = din("b_ada", [DEPTH, 6 * D])
    g_mix = din("g_mix", [DEPTH, D]); g_ffn = din("g_ffn", [DEPTH, D])
    w_in = din("w_in", [DEPTH, D, DIN]); b_in = din("b_in", [DEPTH, DIN])
    g_q = din("g_q", [DEPTH, HD]); g_k = din("g_k", [DEPTH, HD]); sink = din("sink", [DEPTH, NH])
    conv_w = din("conv_w", [DEPTH, 3, 2 * QKW]); conv_b = din("conv_b", [DEPTH, 2 * QKW])
    g_mh = din("g_mh", [DEPTH, MW])
    w_ba = din("w_br_attn", [DEPTH, AW, D]); w_bm = din("w_br_mlstm", [DEPTH, MW, D]); w_o = din("w_out", [DEPTH, D, D])
    w_r = din("w_router", [D, NE]); b_r = din("b_router", [1, NE])
    w_g = din("w_gate", [DEPTH, NE, D, FF]); w_u = din("w_up", [DEPTH, NE, D, FF]); w_d = din("w_down", [DEPTH, NE, FF, D])
    rope_cs = din("rope_cs", [NLAT, 128])
    cst = din("cst", [128, 5 * 128])
    y_out = nc.dram_tensor("y", [NLAT, D], F32, kind="ExternalOutput").ap()

    xs = dsc("xs", [Tn, D], F32)
    hT_d = dsc("hT_d", [128, KC, Tn], BF16)
    mod_d = dsc("mod_d", [DEPTH, 2, 6 * D], F32)
    def wblk(name, R_, C_, BW, lead=()):
        return dsc(name, list(lead) + [128, C_ // BW, R_ // 128, BW], BF16)
    tm_cols = [("aq", o_aq, AW), ("ak", o_ak, KW), ("av", o_av, KW), ("mv", o_mv, MW), ("mo", o_mo, MW), ("mg", o_mg, NGC)]
    fm_cols = [("mqk", o_mq, 2 * QKW), ("ga", o_ga, D), ("gm", o_gm, D)]
    NWS = min(2, DEPTH)
    WS = []
    for wl in range(NWS):
        sfx = f"_{wl}"
        WS.append(dict(
            win_tm={n: (wblk("wtm_" + n + sfx, D, cw, min(512, cw)), c0, cw, min(512, cw)) for (n, c0, cw) in tm_cols},
            win_fm={n: (wblk("wfm_" + n + sfx, D, cw, 128), c0, cw, 128) for (n, c0, cw) in fm_cols},
            wba_b=wblk("wba_b" + sfx, AW, D, 128), wbm_b=wblk("wbm_b" + sfx, MW, D, 128), wo_b=wblk("wo_b" + sfx, D, D, 512),
            wg_b=wblk("wg_b" + sfx, D, FF, 128, [NE]), wu_b=wblk("wu_b" + sfx, D, FF, 128, [NE]), wd_b=wblk("wd_b" + sfx, FF, D, 512, [NE])))
    aq_d = dsc("aq_d", [Tn, AW], BF16); ak_d = dsc("ak_d", [Tn, KW], BF16); av_d = dsc("av_d", [Tn, KW], BF16)
    mv_d = dsc("mv_d", [Tn, MW], BF16); mo_d = dsc("mo_d", [Tn, MW], BF16); mg_d = dsc("mg_d", [Tn, NGC], F32)
    mqk_d = dsc("mqk_d", [2 * QKW, Tn], BF16); mqkc_d = dsc("mqkc_d", [2 * QKW, Tn], BF16)
    sga_d = dsc("sga_d", [D, Tn], BF16); sgm_d = dsc("sgm_d", [D, Tn], BF16)
    qT_d = dsc("qT_d", [128, NH, Tn], BF16); kT_d = dsc("kT_d", [128, NKV, Tn], BF16)
    atT_d = dsc("atT_d", [128, NH, Tn], BF16)
    hf_d = dsc("hf_d", [Tn, MW], BF16); hb_d = dsc("hb_d", [Tn, MW], BF16)
    hmT_d = dsc("hmT_d", [128, MW // 128, Tn], BF16)

    def phase():
        kb.barrier()

    class Ph:
        def __enter__(s):
            s.es = ExitStack(); s.es.__enter__(); return s
        def __exit__(s, *a):
            kb.barrier(); return s.es.__exit__(*a)
        def sb(s, shape, dt=F32, nm="t"):
            return T(s.es.enter_context(nc.sbuf_tensor(kb.name(nm), list(shape), dt)))
        def ps(s, shape, dt=F32, nm="p"):
            return TP(s.es.enter_context(nc.psum_tensor(kb.name(nm), [128, 512], F32)), list(shape), dt)

    pes = ExitStack(); pes.__enter__()
    pes.enter_context(nc.allow_non_contiguous_dma(reason="strided layout transforms"))
    def psb(shape, dt=F32, nm="c"):
        return T(pes.enter_context(nc.sbuf_tensor(kb.name(nm), list(shape), dt)))
    cst_f = psb([128, 5 * 128], F32, "cstf")
    ident_f = None
    ident_b = psb([128, 128], BF16, "idb")
    maskT = psb([128, 3 * 128], F32, "maskT")
    tri_f = psb([128, 128], F32, "trif")
    tri_b = psb([128, 128], F32, "trib")
    tri_fb = psb([128, 128], BF16, "trifb")
    tri_bb = psb([128, 128], BF16, "tribb")
    ones_f = psb([128, 128], F32, "onesf")
    ones_b = psb([128, 128], BF16, "onesb")
    kb.dma('sp', cst_f[:], cst[:, :], W=[cst_f])
    kb.op('dve', lambda v: v.tensor_copy(out=ident_b[:], in_=cst_f[:, 0:128]), R=[cst_f], W=[ident_b])
    kb.op('dve', lambda v: v.tensor_copy(out=maskT[:], in_=cst_f[:, 128:512]), R=[cst_f], W=[maskT])
    kb.op('dve', lambda v: v.tensor_copy(out=tri_f[:], in_=cst_f[:, 512:640]), R=[cst_f], W=[tri_f])
    kb.op('dve', lambda v: v.memset(ones_f[:], 1.0), W=[ones_f])
    kb.op('dve', lambda v: v.memset(ones_b[:], 1.0), W=[ones_b])
    kb.op('dve', lambda v: v.tensor_tensor(out=tri_b[:], in0=ones_f[:], in1=tri_f[:], op=ALU.subtract), R=[ones_f, tri_f], W=[tri_b])
    kb.op('dve', lambda v: v.tensor_tensor(out=tri_b[:], in0=tri_b[:], in1=cst_f[:, 0:128], op=ALU.add), R=[tri_b, cst_f], W=[tri_b])
    kb.op('dve', lambda v: v.tensor_copy(out=tri_fb[:], in_=tri_f[:]), R=[tri_f], W=[tri_fb])
    kb.op('dve', lambda v: v.tensor_copy(out=tri_bb[:], in_=tri_b[:]), R=[tri_b], W=[tri_bb])
    ident_f = cst_f

    rr = [0]
    def cast_eng():
        rr[0] += 1
        return ['dve', 'act', 'pool'][rr[0] % 3]

    def copy_on(e, out, in_, R, W):
        if e == 'act':
            kb.op('act', lambda a: a.copy(out=out, in_=in_), R=R, W=W)
        else:
            kb.op(e, lambda v: v.tensor_copy(out=out, in_=in_), R=R, W=W)

    CVW = 1024
    cvbuf = [(psb([128, CVW], F32, "cf"), psb([128, CVW], BF16, "cb")) for _ in range(3)]
    cvi = [0]

    def cvt(src, dst, R_, C_, BW, eng):
        CC = min(C_, CVW)
        for r0 in range(0, R_, 128):
            k = r0 // 128
            for c0 in range(0, C_, CC):
                cw = min(CC, C_ - c0)
                f, b = cvbuf[cvi[0] % 3]; cvi[0] += 1
                kb.dma('sp', f[:, 0:cw], src[r0:r0 + 128, c0:c0 + cw], W=[f])
                copy_on(eng if eng else cast_eng(), b[:, 0:cw], f[:, 0:cw], [f], [b])
                kb.dma('pool', dst[:, c0 // BW:(c0 + cw) // BW, k, :], b[:, 0:cw].rearrange("p (a b) -> p a b", b=BW), R=[b])
                yield

    ready = {}

    def convert_gen(l, eng=None, parts="AB"):
        ws = WS[l % NWS]
        if "A" in parts:
            for n, (dst, c0, cw, BW) in list(ws['win_tm'].items()) + list(ws['win_fm'].items()):
                yield from cvt(w_in[l][:, c0:c0 + cw], dst, D, cw, BW, eng)
        if "B" in parts:
            yield from cvt(w_ba[l], ws['wba_b'], AW, D, 128, eng)
            yield from cvt(w_bm[l], ws['wbm_b'], MW, D, 128, eng)
            yield from cvt(w_o[l], ws['wo_b'], D, D, 512, eng)
            ready[(l, 'm')] = True
            for e in range(NE):
                yield from cvt(w_g[l, e], ws['wg_b'][e], D, FF, 128, eng)
                yield from cvt(w_u[l, e], ws['wu_b'][e], D, FF, 128, eng)
                yield from cvt(w_d[l, e], ws['wd_b'][e], FF, D, 512, eng)

    pending = []

    def cv_tick(n=1):
        for _ in range(n):
            if not pending:
                return
            try:
                next(pending[0])
            except StopIteration:
                pending.pop(0)

    def cv_flush():
        while pending:
            cv_tick()
        kb.barrier()

    kb.dma('sp', xs[0:CTX, :], ctx_in[:, :])
    for r0 in range(0, NLAT, 1024):
        r1 = min(NLAT, r0 + 1024)
        kb.dma('sp', xs[CTX + r0:CTX + r1, :], x_in[r0:r1, :])
    kb.barrier()

    def adaln(l):
        with Ph() as ph:
            ct = ph.sb([128, 2, KC], F32, "ct")
            for r in range(2):
                kb.dma('sp', ct[:, r, :], cc_in[r, :].rearrange("(k p) -> p k", p=128), W=[ct])
            sg = ph.sb([128, 2, KC], F32, "sg")
            kb.op('act', lambda a: a.activation(out=sg[:], in_=ct[:], func=AF.Sigmoid), R=[ct], W=[sg])
            kb.op('dve', lambda v: v.tensor_tensor(out=ct[:], in0=ct[:], in1=sg[:], op=ALU.mult), R=[ct, sg], W=[ct])
            BW = 2048 if (6 * D) % 2048 == 0 else 1024
            NJ = BW // 512
            wt = [ph.sb([128, BW], F32, "wt") for _ in range(3)]
            pss = [ph.ps([2, 512], F32, "pa") for _ in range(NJ)]
            ob = ph.sb([2, BW], F32, "ob"); bb = ph.sb([2, BW], F32, "bb")
            NBLK = (6 * D) // BW
            i = 0
            for nb in range(NBLK):
                kb.dma('sp', bb[:], b_ada[l:l + 1, nb * BW:(nb + 1) * BW].to_broadcast([2, BW]), W=[bb])
                for kc in range(KC):
                    w = wt[i % 3]; i += 1
                    kb.dma('sp', w[:], w_ada[l, kc * 128:(kc + 1) * 128, nb * BW:(nb + 1) * BW], W=[w])
                    kb.wait('pe', R=[w, ct], W=pss if kc == 0 else [])
                    for j in range(NJ):
                        ins = nc.tensor.matmul(pss[j][:], lhsT=ct[:, :, kc], rhs=w[:, j * 512:(j + 1) * 512], start=(kc == 0), stop=(kc == KC - 1))
                    kb.done('pe', ins, R=[w, ct], W=pss if kc == KC - 1 else [])
                for j in range(NJ):
                    kb.op('dve', lambda v: v.tensor_tensor(out=ob[:, j * 512:(j + 1) * 512], in0=pss[j][:], in1=bb[:, j * 512:(j + 1) * 512], op=ALU.add), R=[pss[j], bb], W=[ob])
                kb.dma('pool', mod_d[l, :, nb * BW:(nb + 1) * BW], ob[:], R=[ob])

    def load_rep(ph_t, src_row_ap, n):
        kb.dma('sp', ph_t[:, 0:n], src_row_ap.to_broadcast([128, n]), W=[ph_t])

    def norm_phase(l, which):
        last = (l == DEPTH - 1)
        gsrc = g_mix if which == 0 else g_ffn
        so = 0 if which == 0 else 3
        with Ph() as ph:
            G = [ph.sb([128, D], F32, "G") for _ in range(2)]
            S = [ph.sb([128, D], F32, "S") for _ in range(2)]
            gr = ph.sb([128, D], F32, "gr")
            load_rep(gr, gsrc[l:l + 1, :], D)
            for r in range(2):
                load_rep(S[r], mod_d[l, r:r + 1, so * D:(so + 1) * D], D)
                load_rep(G[r], mod_d[l, r:r + 1, (so + 1) * D:(so + 2) * D], D)
                kb.op('dve', lambda v: v.scalar_tensor_tensor(out=G[r][:], in0=G[r][:], scalar=1.0, in1=gr[:], op0=ALU.add, op1=ALU.mult), R=[G[r], gr], W=[G[r]])
            xt = [ph.sb([128, D], F32, "xt") for _ in range(2)]
            junk = ph.sb([128, D], BF16, "junk")
            hb = [ph.sb([128, D], BF16 if which == 0 else F32, "hb") for _ in range(2 if which == 0 else 1)]
            hTt = [ph.sb([128, KC, 128], BF16, "hTt") for _ in range(2)]
            st = [ph.sb([128, 4], F32, "st") for _ in range(2)]
            if which == 0:
                ptr = [ph.ps([128, 8, 128], BF16, "ptr") for _ in range(2)]
                NPT = 8
            else:
                ptr = [ph.ps([128, 4, 128], F32, "ptr") for _ in range(2)]
                NPT = 4
                hTf = [ph.sb([128, KC, 128], F32, "hTf") for _ in range(1)]
                wr = ph.sb([128, KC, NE], F32, "wr")
                if 'a' not in os.environ.get("KX", ""):
                    kb.dma('sp', wr[:], w_r.rearrange("(k p) e -> p k e", p=128), W=[wr])
                brr = ph.sb([128, NE], F32, "brr")
                if 'a' not in os.environ.get("KX", ""):
                    load_rep(brr, b_r[0:1, :], NE)
                pl = [ph.ps([128, NE], F32, "pl") for _ in range(2)]
                rt = {k: ph.sb([128, NE], F32, "r" + k) for k in ['aff', 'bi', 'sel', 'm2', 'oh', 'w']}
                ps6 = ph.sb([128, NG, 6], F32, "ps6"); gs = ph.sb([128, NG], F32, "gs"); gm1 = ph.sb([128, 4], F32, "gm1")
                goh = ph.sb([128, NG], F32, "goh")
            t0 = CT if (last and which == 1) else 0
            for t in range(t0, TT):
                r = 1 if t < CT else 0
                x_ = xt[t % 2]; h_ = hb[t % len(hb)]; s_ = st[t % 2]; hT_ = hTt[t % 2]
                kb.dma('sp', x_[:], xs[t * 128:(t + 1) * 128, :], W=[x_])
                kb.op('act', lambda a: a.activation(out=junk[:], in_=x_[:], func=AF.Square, accum_out=s_[:, 0:1]), R=[x_], W=[junk, s_])
                kb.op('dve', lambda v: v.tensor_scalar(out=s_[:, 1:2], in0=s_[:, 0:1], scalar1=1.0 / D, scalar2=EPS, op0=ALU.mult, op1=ALU.add), R=[s_], W=[s_])
                kb.op('act', lambda a: a.activation(out=s_[:, 2:3], in_=s_[:, 1:2], func=AF.Sqrt), R=[s_], W=[s_])
                kb.op('dve', lambda v: v.reciprocal(out=s_[:, 3:4], in_=s_[:, 2:3]), R=[s_], W=[s_])
                kb.op('dve', lambda v: v.scalar_tensor_tensor(out=x_[:], in0=x_[:], scalar=s_[:, 3:4], in1=G[r][:], op0=ALU.mult, op1=ALU.mult), R=[x_, s_, G[r]], W=[x_])
                kb.op('pool', lambda v: v.tensor_tensor(out=h_[:], in0=x_[:], in1=S[r][:], op=ALU.add), R=[x_, S[r]], W=[h_])
                idn = ident_b[:] if which == 0 else ident_f[:, 0:128]
                idt = ident_b if which == 0 else cst_f
                for k0 in range(0, KC, NPT):
                    p_ = ptr[(k0 // NPT) % 2]
                    kb.wait('pe', R=[h_, idt], W=[p_])
                    for k in range(k0, min(KC, k0 + NPT)):
                        if which == 0:
                            ins = nc.tensor.transpose(out=p_[:, k - k0, :], in_=h_[:, k * 128:(k + 1) * 128], identity=idn)
                        else:
                            ins = nc.tensor.matmul(p_[:, k - k0, :], lhsT=h_[:, k * 128:(k + 1) * 128], rhs=idn, start=True, stop=True)
                    kb.done('pe', ins, R=[h_, idt], W=[p_])
                    n = min(KC, k0 + NPT) - k0
                    kb.op('act', lambda a: a.copy(out=hT_[:, k0:k0 + n, :], in_=p_[:, 0:n, :]), R=[p_], W=[hT_])
                    if which == 1 and 'b' not in os.environ.get("KX", ""):
                        hf_ = hTf[0]
                        kb.op('dve', lambda v: v.tensor_copy(out=hf_[:, k0:k0 + n, :], in_=p_[:, 0:n, :]), R=[p_], W=[hf_])
                kb.dma('pool', hT_d[:, :, t * 128:(t + 1) * 128], hT_[:], R=[hT_])
                if which == 1 and os.environ.get("KNOROUTE") != "2":
                    hf_ = hTf[0]; pl_ = pl[t % 2]
                    kb.wait('pe', R=[hf_, wr], W=[pl_])
                    for k in range(KC):
                        ins = nc.tensor.matmul(pl_[:], lhsT=hf_[:, k, :], rhs=wr[:, k, :], start=(k == 0), stop=(k == KC - 1))
                    kb.done('pe', ins, R=[hf_, wr], W=[pl_])
                    if os.environ.get("KNOROUTE"):
                        continue
                    aff = rt['aff']; bi = rt['bi']; sel = rt['sel']; m2 = rt['m2']; oh = rt['oh']; w_ = rt['w']
                    kb.op('act', lambda a: a.activation(out=aff[:], in_=pl_[:], func=AF.Sigmoid), R=[pl_], W=[aff])
                    kb.op('dve', lambda v: v.tensor_tensor(out=bi[:], in0=aff[:], in1=brr[:], op=ALU.add), R=[aff, brr], W=[bi])
                    big = bi[:].rearrange("p (g e) -> p g e", g=NG)
                    pi = 0
                    for a_ in range(EPG):
                        for b_ in range(a_ + 1, EPG):
                            kb.op('dve', lambda v: v.tensor_tensor(out=ps6[:, :, pi], in0=big[:, :, a_], in1=big[:, :, b_], op=ALU.add), R=[bi], W=[ps6])
                            pi += 1
                    kb.op('dve', lambda v: v.tensor_reduce(out=gs[:], in_=ps6[:], axis=AX.X, op=ALU.max), R=[ps6], W=[gs])
                    kb.op('dve', lambda v: v.tensor_reduce(out=gm1[:, 0:1], in_=gs[:], axis=AX.X, op=ALU.max), R=[gs], W=[gm1])
                    kb.op('dve', lambda v: v.tensor_scalar(out=goh[:], in0=gs[:], scalar1=gm1[:, 0:1], scalar2=None, op0=ALU.is_ge), R=[gs, gm1], W=[goh])
                    kb.op('dve', lambda v: v.tensor_tensor(out=m2[:].rearrange("p (g e) -> p g e", g=NG), in0=big, in1=goh[:].unsqueeze(2).to_broadcast([128, NG, EPG]), op=ALU.mult), R=[bi, goh], W=[m2])
                    kb.op('dve', lambda v: v.tensor_scalar(out=goh[:], in0=goh[:], scalar1=-1.0, scalar2=1e30, op0=ALU.add, op1=ALU.mult), R=[goh], W=[goh])
                    kb.op('dve', lambda v: v.tensor_tensor(out=m2[:].rearrange("p (g e) -> p g e", g=NG), in0=m2[:].rearrange("p (g e) -> p g e", g=NG), in1=goh[:].unsqueeze(2).to_broadcast([128, NG, EPG]), op=ALU.add), R=[m2, goh], W=[m2])
                    kb.op('dve', lambda v: v.tensor_reduce(out=gm1[:, 1:2], in_=m2[:], axis=AX.X, op=ALU.max), R=[m2], W=[gm1])
                    kb.op('dve', lambda v: v.tensor_scalar(out=sel[:], in0=m2[:], scalar1=gm1[:, 1:2], scalar2=None, op0=ALU.is_ge), R=[m2, gm1], W=[sel])
                    kb.op('dve', lambda v: v.scalar_tensor_tensor(out=m2[:], in0=sel[:], scalar=-1e30, in1=m2[:], op0=ALU.mult, op1=ALU.add), R=[sel, m2], W=[m2])
                    kb.op('dve', lambda v: v.tensor_reduce(out=gm1[:, 2:3], in_=m2[:], axis=AX.X, op=ALU.max), R=[m2], W=[gm1])
                    kb.op('dve', lambda v: v.tensor_scalar(out=oh[:], in0=m2[:], scalar1=gm1[:, 2:3], scalar2=None, op0=ALU.is_ge), R=[m2, gm1], W=[oh])
                    kb.op('dve', lambda v: v.tensor_tensor(out=sel[:], in0=sel[:], in1=oh[:], op=ALU.add), R=[sel, oh], W=[sel])
                    kb.op('dve', lambda v: v.tensor_tensor(out=w_[:], in0=sel[:], in1=aff[:], op=ALU.mult), R=[sel, aff], W=[w_])
                    kb.op('dve', lambda v: v.tensor_reduce(out=gm1[:, 3:4], in_=w_[:], axis=AX.X, op=ALU.add), R=[w_], W=[gm1])
                    kb.op('dve', lambda v: v.reciprocal(out=gm1[:, 3:4], in_=gm1[:, 3:4]), R=[gm1], W=[gm1])
                    kb.op('dve', lambda v: v.tensor_scalar(out=comb[:, t, :], in0=w_[:], scalar1=gm1[:, 3:4], scalar2=None, op0=ALU.mult), R=[w_, gm1], W=[comb])

    comb = psb([128, TT, NE], F32, "comb")

    def proj_phase(l):
        last = (l == DEPTH - 1)
        NTB = 512
        tm_groups = [("aq", aq_d, BF16), ("ak", ak_d, BF16), ("av", av_d, BF16), ("mv", mv_d, BF16), ("mo", mo_d, BF16), ("mg", mg_d, F32)]
        fm_groups = [("mqk", mqk_d, 0), ("ga", sga_d, 1), ("gm", sgm_d, 1)]
        with Ph() as ph:
            hTb = [ph.sb([128, KC, NTB], BF16, "hTb") for _ in range(1)]
            wtm = [ph.sb([128, KC, 512], BF16, "wtm") for _ in range(2)]
            brep = [ph.sb([128, 512], F32, "brep") for _ in range(2)]
            bcol = [ph.sb([128, 1], F32, "bcol") for _ in range(2)]
            pp = [ph.ps([128, 512], F32, "pp") for _ in range(4)]
            ot = [ph.sb([128, 512], BF16, "ot") for _ in range(3)]
            otf = [ph.sb([128, 512], F32, "otf") for _ in range(2)]
            wi = 0; pi = 0; oi = 0
            for tb0 in range(0, Tn, NTB):
                nt = min(NTB, Tn - tb0)
                hT_ = hTb[0]
                kb.dma('sp', hT_[:, :, 0:nt], hT_d[:, :, tb0:tb0 + nt], W=[hT_])
                for (gn, dst, dt) in tm_groups:
                    wsrc, c0, cw, BW = WS[l % NWS]['win_tm'][gn]
                    for n0 in range(0, cw, BW):
                        nw = BW
                        w_ = wtm[wi % 2]; br_ = brep[wi % 2]; wi += 1
                        kb.dma('sp', w_[:, :, 0:nw], wsrc[:, n0 // BW, :, :], W=[w_])
                        kb.dma('sp', br_[:, 0:nw], b_in[l:l + 1, c0 + n0:c0 + n0 + nw].to_broadcast([128, nw]), W=[br_])
                        cv_tick()
                        for tt in range(nt // 128):
                            p_ = pp[pi % 4]; pi += 1
                            kb.wait('pe', R=[hT_, w_], W=[p_])
                            for k in range(KC):
                                ins = nc.tensor.matmul(p_[:, 0:nw], lhsT=hT_[:, k, tt * 128:(tt + 1) * 128], rhs=w_[:, k, 0:nw], start=(k == 0), stop=(k == KC - 1))
                            kb.done('pe', ins, R=[hT_, w_], W=[p_])
                            if dt == BF16:
                                o_ = ot[oi % 3]; oi += 1
                            else:
                                o_ = otf[oi % 2]; oi += 1
                            kb.op('dve', lambda v: v.tensor_tensor(out=o_[:, 0:nw], in0=p_[:, 0:nw], in1=br_[:, 0:nw], op=ALU.add), R=[p_, br_], W=[o_])
                            kb.dma('pool', dst[tb0 + tt * 128:tb0 + (tt + 1) * 128, n0:n0 + nw], o_[:, 0:nw], R=[o_])
                for (gn, dst, sg) in fm_groups:
                    wsrc, c0, cw, BW = WS[l % NWS]['win_fm'][gn]
                    for n0 in range(0, cw, 128):
                        w_ = wtm[wi % 2]; bc_ = bcol[wi % 2]; wi += 1
                        kb.dma('sp', w_[:, :, 0:128], wsrc[:, n0 // 128, :, :], W=[w_])
                        kb.dma('sp', bc_[:], b_in[l, c0 + n0:c0 + n0 + 128].rearrange("(p o) -> p o", o=1), W=[bc_])
                        cv_tick()
                        p_ = pp[pi % 4]; pi += 1
                        kb.wait('pe', R=[hT_, w_], W=[p_])
                        for k in range(KC):
                            ins = nc.tensor.matmul(p_[:, 0:nt], lhsT=w_[:, k, 0:128], rhs=hT_[:, k, 0:nt], start=(k == 0), stop=(k == KC - 1))
                        kb.done('pe', ins, R=[hT_, w_], W=[p_])
                        o_ = ot[oi % 3]; oi += 1
                        kb.op('act', lambda a: a.activation(out=o_[:, 0:nt], in_=p_[:, 0:nt], func=(AF.Sigmoid if sg else AF.Identity), bias=bc_[:, 0:1], scale=1.0), R=[p_, bc_], W=[o_])
                        kb.dma('pool', dst[n0:n0 + 128, tb0:tb0 + nt], o_[:, 0:nt], R=[o_])

    def conv_phase(l):
        with Ph() as ph:
            xi = [ph.sb([128, Tn], BF16, "cxi") for _ in range(2)]
            ya = [ph.sb([128, Tn], F32, "cya") for _ in range(2)]
            yo = [ph.sb([128, Tn], BF16, "cyo") for _ in range(2)]
            cw = [ph.sb([128, 4], F32, "ccw") for _ in range(2)]
            for i, c0 in enumerate(range(0, 2 * QKW, 128)):
                x_ = xi[i % 2]; y_ = ya[i % 2]; o_ = yo[i % 2]; w_ = cw[i % 2]
                kb.dma('sp', x_[:], mqk_d[c0:c0 + 128, :], W=[x_])
                kb.dma('sp', w_[:, 0:3], conv_w[l, :, c0:c0 + 128].rearrange("j p -> p j"), W=[w_])
                kb.dma('sp', w_[:, 3:4], conv_b[l, c0:c0 + 128].rearrange("(p o) -> p o", o=1), W=[w_])
                kb.op('dve', lambda v: v.tensor_scalar(out=y_[:], in0=x_[:], scalar1=w_[:, 1:2], scalar2=w_[:, 3:4], op0=ALU.mult, op1=ALU.add), R=[x_, w_], W=[y_])
                for (s0, s1) in [(0, CTX), (CTX, Tn)]:
                    kb.op('dve', lambda v: v.scalar_tensor_tensor(out=y_[:, s0 + 1:s1], in0=x_[:, s0:s1 - 1], scalar=w_[:, 0:1], in1=y_[:, s0 + 1:s1], op0=ALU.mult, op1=ALU.add), R=[x_, w_, y_], W=[y_])
                    kb.op('dve', lambda v: v.scalar_tensor_tensor(out=y_[:, s0:s1 - 1], in0=x_[:, s0 + 1:s1], scalar=w_[:, 2:3], in1=y_[:, s0:s1 - 1], op0=ALU.mult, op1=ALU.add), R=[x_, w_, y_], W=[y_])
                sc = 1.0 if c0 < QKW else DK ** -0.5
                kb.op('act', lambda a: a.activation(out=o_[:], in_=y_[:], func=AF.Sigmoid), R=[y_], W=[o_])
                kb.op('dve', lambda v: v.scalar_tensor_tensor(out=o_[:], in0=y_[:], scalar=sc, in1=o_[:], op0=ALU.mult, op1=ALU.mult), R=[y_, o_], W=[o_])
                kb.dma('pool', mqkc_d[c0:c0 + 128, :], o_[:], R=[o_])

    def qk_phase(l):
        last = (l == DEPTH - 1)
        with Ph() as ph:
            gq = ph.sb([128, HD], F32, "gq"); gk = ph.sb([128, HD], F32, "gk")
            load_rep(gq, g_q[l:l + 1, :], HD); load_rep(gk, g_k[l:l + 1, :], HD)
            NQ = NH + NKV
            qi = [ph.sb([128, NQ, HD], BF16, "qi") for _ in range(2)]
            sq = ph.sb([128, NQ, HD], F32, "sq")
            xn = ph.sb([128, NQ, HD], F32, "xn")
            tmp = ph.sb([128, NQ, 2, 32], F32, "tmp"); tmp2 = ph.sb([128, NQ, 2, 32], F32, "tmp2")
            qo = [ph.sb([128, NQ, HD], BF16, "qo") for _ in range(2)]
            st = [ph.sb([128, 4, NQ], F32, "st") for _ in range(2)]
            cs = [ph.sb([128, 128], F32, "cs") for _ in range(2)]
            ptr = [ph.ps([128, 8, 128], BF16, "ptr") for _ in range(2)]
            oT = [ph.sb([128, NQ, 128], BF16, "oT") for _ in range(2)]
            for t in range(TT):
                q_ = qi[t % 2]; s_ = st[t % 2]; c_ = cs[t % 2]; o_ = qo[t % 2]; oT_ = oT[t % 2]
                kb.dma('sp', q_[:, 0:NH, :], aq_d[t * 128:(t + 1) * 128, :].rearrange("p (h d) -> p h d", d=HD), W=[q_])
                kb.dma('sp', q_[:, NH:NQ, :], ak_d[t * 128:(t + 1) * 128, :].rearrange("p (h d) -> p h d", d=HD), W=[q_])
                kb.op('pool', lambda v: v.tensor_tensor(out=sq[:], in0=q_[:], in1=q_[:], op=ALU.mult), R=[q_], W=[sq])
                kb.op('dve', lambda v: v.tensor_reduce(out=s_[:, 0, :], in_=sq[:], axis=AX.X, op=ALU.add), R=[sq], W=[s_])
                kb.op('dve', lambda v: v.tensor_scalar(out=s_[:, 1, :], in0=s_[:, 0, :], scalar1=1.0 / HD, scalar2=EPS, op0=ALU.mult, op1=ALU.add), R=[s_], W=[s_])
                kb.op('act', lambda a: a.activation(out=s_[:, 2, :], in_=s_[:, 1, :], func=AF.Sqrt), R=[s_], W=[s_])
                kb.op('dve', lambda v: v.reciprocal(out=s_[:, 3, :], in_=s_[:, 2, :]), R=[s_], W=[s_])
                kb.op('dve', lambda v: v.tensor_tensor(out=xn[:], in0=q_[:], in1=s_[:, 3, :].unsqueeze(2).to_broadcast([128, NQ, HD]), op=ALU.mult), R=[q_, s_], W=[xn])
                lat = t >= CT
                dstq = o_ if not lat else xn
                kb.op('dve', lambda v: v.tensor_tensor(out=dstq[:, 0:NH, :], in0=xn[:, 0:NH, :], in1=gq[:].unsqueeze(1).to_broadcast([128, NH, HD]), op=ALU.mult), R=[xn, gq], W=[dstq])
                kb.op('dve', lambda v: v.tensor_tensor(out=dstq[:, NH:NQ, :], in0=xn[:, NH:NQ, :], in1=gk[:].unsqueeze(1).to_broadcast([128, NKV, HD]), op=ALU.mult), R=[xn, gk], W=[dstq])
                if lat:
                    kb.dma('sp', c_[:], rope_cs[(t - CT) * 128:(t - CT + 1) * 128, :], W=[c_])
                    xv = xn[:].rearrange("p h (a b c) -> p h a b c", a=2, b=2)
                    ov = o_[:].rearrange("p h (a b c) -> p h a b c", a=2, b=2)
                    x1 = xv[:, :, :, 0, :]; x2 = xv[:, :, :, 1, :]
                    COS = c_[:, 0:64].rearrange("p (a c) -> p a c", a=2).unsqueeze(1).to_broadcast([128, NQ, 2, 32])
                    SIN = c_[:, 64:128].rearrange("p (a c) -> p a c", a=2).unsqueeze(1).to_broadcast([128, NQ, 2, 32])
                    kb.op('dve', lambda v: v.tensor_tensor(out=tmp[:], in0=x1, in1=COS, op=ALU.mult), R=[xn, c_], W=[tmp])
                    kb.op('pool', lambda v: v.tensor_tensor(out=tmp2[:], in0=x2, in1=SIN, op=ALU.mult), R=[xn, c_], W=[tmp2])
                    kb.op('dve', lambda v: v.tensor_tensor(out=ov[:, :, :, 0, :], in0=tmp[:], in1=tmp2[:], op=ALU.subtract), R=[tmp, tmp2], W=[o_])
                    kb.op('dve', lambda v: v.tensor_tensor(out=tmp[:], in0=x1, in1=SIN, op=ALU.mult), R=[xn, c_], W=[tmp])
                    kb.op('pool', lambda v: v.tensor_tensor(out=tmp2[:], in0=x2, in1=COS, op=ALU.mult), R=[xn, c_], W=[tmp2])
                    kb.op('dve', lambda v: v.tensor_tensor(out=ov[:, :, :, 1, :], in0=tmp[:], in1=tmp2[:], op=ALU.add), R=[tmp, tmp2], W=[o_])
                for k0 in range(0, NQ, 8):
                    p_ = ptr[(k0 // 8) % 2]
                    n = min(NQ, k0 + 8) - k0
                    kb.wait('pe', R=[o_, ident_b], W=[p_])
                    for k in range(k0, k0 + n):
                        ins = nc.tensor.transpose(out=p_[:, k - k0, :], in_=o_[:, k, :], identity=ident_b[:])
                    kb.done('pe', ins, R=[o_, ident_b], W=[p_])
                    kb.op('act', lambda a: a.copy(out=oT_[:, k0:k0 + n, :], in_=p_[:, 0:n, :]), R=[p_], W=[oT_])
                cv_tick(2)
                kb.dma('pool', qT_d[:, :, t * 128:(t + 1) * 128], oT_[:, 0:NH, :], R=[oT_])
                kb.dma('pool', kT_d[:, :, t * 128:(t + 1) * 128], oT_[:, NH:NQ, :], R=[oT_])

    def attn_phase(l):
        last = (l == DEPTH - 1)
        scale = HD ** -0.5
        with Ph() as ph:
            ES = ph.sb([128, NH], F32, "ES")
            load_rep(ES, sink[l:l + 1, :], NH)
            kb.op('act', lambda a: a.activation(out=ES[:], in_=ES[:], func=AF.Exp), R=[ES], W=[ES])
            kc = ph.sb([128, NKV, CTX], BF16, "kc")
            kb.dma('sp', kc[:], kT_d[:, :, 0:CTX], W=[kc])
            vc = ph.sb([128, CT, KW], BF16, "vc")
            kb.dma('sp', vc[:], av_d[0:CTX, :].rearrange("(c p) n -> p c n", p=128), W=[vc])
            qb = [ph.sb([128, NH, 128], BF16, "qb") for _ in range(2)]
            kw = [ph.sb([128, NKV, 384], BF16, "kw") for _ in range(2)]
            vw = [ph.sb([128, 3, KW], BF16, "vw") for _ in range(2)]
            pS = [ph.ps([128, 512], F32, "pS") for _ in range(4)]
            pN = [ph.ps([128, 512], F32, "pN") for _ in range(2)]
            pD = [ph.ps([128, 512], F32, "pD") for _ in range(2)]
            sm = [ph.sb([128, 512], F32, "sm") for _ in range(2)]
            pT = [ph.sb([128, 512], BF16, "pT") for _ in range(3)]
            dn = [ph.sb([128, 512], F32, "dn") for _ in range(2)]
            ob = [ph.sb([128, NH, 128], BF16, "ob") for _ in range(2)]
            blocks = ([] if last else [('c', c) for c in range(CT)]) + [('l', n) for n in range(NB)]
            si = 0; pi = 0
            for bi_, (kind, n) in enumerate(blocks):
                q_ = qb[bi_ % 2]; k_ = kw[bi_ % 2]; v_ = vw[bi_ % 2]; o_ = ob[bi_ % 2]
                tok0 = n * 128 if kind == 'c' else CTX + n * 128
                kb.dma('sp', q_[:], qT_d[:, :, tok0:tok0 + 128], W=[q_])
                chunks = []
                if kind == 'l':
                    lo = max(0, n - 1); hi = min(NB - 1, n + 1)
                    kb.dma('sp', k_[:, :, (lo - n + 1) * 128:(hi - n + 2) * 128], kT_d[:, :, CTX + lo * 128:CTX + (hi + 1) * 128], W=[k_])
                    kb.dma('sp', v_[:, lo - n + 1:hi - n + 2, :], av_d[CTX + lo * 128:CTX + (hi + 1) * 128, :].rearrange("(c p) n -> p c n", p=128), W=[v_])
                    for j in range(lo - n + 1, hi - n + 2):
                        chunks.append((k_, j, v_, j, j))
                for c in range(CT):
                    chunks.append((kc, c, vc, c, None))
                for g in range(NKV):
                    pN_ = pN[g % 2]; pD_ = pD[g % 2]
                    qg = q_[:, g * GRP:(g + 1) * GRP, :].rearrange("p h q -> p (h q)")
                    NQC = GRP * 128
                    nch = len(chunks)
                    psl = {}
                    def score(ci):
                        kt, kj, vt, vj, mj = chunks[ci]
                        pS_ = pS[(si0 + ci) % 4]
                        kb.wait('pe', R=[kt, q_], W=[pS_])
                        ins = nc.tensor.matmul(pS_[:, 0:NQC], lhsT=kt[:, g, kj * 128:(kj + 1) * 128], rhs=qg, start=True, stop=True)
                        kb.done('pe', ins, R=[kt, q_], W=[pS_])
                        psl[ci] = pS_
                    si0 = si; si += nch
                    for ci in range(min(3, nch)):
                        score(ci)
                    for ci, (kt, kj, vt, vj, mj) in enumerate(chunks):
                        if ci + 3 < nch:
                            score(ci + 3)
                        pS_ = psl[ci]
                        p_ = pT[pi % 3]; pi += 1
                        if mj is not None:
                            s_ = sm[ci % 2]
                            kb.op('dve', lambda v: v.tensor_tensor(out=s_[:, 0:NQC].rearrange("p (h q) -> p h q", h=GRP), in0=pS_[:, 0:NQC].rearrange("p (h q) -> p h q", h=GRP), in1=maskT[:, mj * 128:(mj + 1) * 128].unsqueeze(1).to_broadcast([128, GRP, 128]), op=ALU.add), R=[pS_, maskT], W=[s_])
                            kb.op('act', lambda a: a.activation(out=p_[:, 0:NQC], in_=s_[:, 0:NQC], func=AF.Exp, scale=scale), R=[s_], W=[p_])
                        else:
                            kb.op('act', lambda a: a.activation(out=p_[:, 0:NQC], in_=pS_[:, 0:NQC], func=AF.Exp, scale=scale), R=[pS_], W=[p_])
                        first = (ci == 0); lastc = (ci == nch - 1)
                        kb.wait('pe', R=[p_, vt, ones_b], W=[pN_, pD_] if first else [])
                        nc.tensor.matmul(pN_[:, 0:NQC], lhsT=vt[:, vj, g * HD:(g + 1) * HD], rhs=p_[:, 0:NQC], start=first, stop=lastc)
                        ins = nc.tensor.matmul(pD_[:, 0:NQC], lhsT=ones_b[:, :], rhs=p_[:, 0:NQC], start=first, stop=lastc)
                        kb.done('pe', ins, R=[p_, vt, ones_b], W=[pN_, pD_] if lastc else [])
                    d_ = dn[g % 2]
                    kb.op('dve', lambda v: v.tensor_tensor(out=d_[:, 0:NQC].rearrange("p (h q) -> p h q", h=GRP), in0=pD_[:, 0:NQC].rearrange("p (h q) -> p h q", h=GRP), in1=ES[:, g * GRP:(g + 1) * GRP].unsqueeze(2).to_broadcast([128, GRP, 128]), op=ALU.add), R=[pD_, ES], W=[d_])
                    kb.op('dve', lambda v: v.reciprocal(out=d_[:, 0:NQC], in_=d_[:, 0:NQC]), R=[d_], W=[d_])
                    kb.op('dve', lambda v: v.tensor_tensor(out=o_[:, g * GRP:(g + 1) * GRP, :].rearrange("p h q -> p (h q)"), in0=pN_[:, 0:NQC], in1=d_[:, 0:NQC], op=ALU.mult), R=[pN_, d_], W=[o_])
                cv_tick(2)
                kb.dma('pool', atT_d[:, :, tok0:tok0 + 128], o_[:], R=[o_])

    def mlstm_phase(l):
        last = (l == DEPTH - 1)
        NCH = TT
        M2 = 2 * MH
        order_f = list(range(NCH))
        order_b = list(range(CT - 1, -1, -1)) + list(range(NCH - 1, CT - 1, -1))
        with Ph() as ph:
            CS = ph.sb([128, NCH, M2], F32, "CS"); IA = ph.sb([128, NCH, M2], F32, "IA")
            EA = ph.sb([128, NCH, M2], F32, "EA"); WA = ph.sb([128, NCH, M2], F32, "WA")
            Um = [ph.sb([MH, NCH], F32, "Um") for _ in range(2)]; Be = [ph.sb([MH, NCH], F32, "Be") for _ in range(2)]
            Mm = [ph.sb([MH, NCH], F32, "Mm") for _ in range(2)]; Mp = [ph.sb([MH, NCH + 1], F32, "Mp") for _ in range(2)]
            Aa = [ph.sb([MH, NCH], F32, "Aa") for _ in range(2)]
            Mb = ph.sb([128, M2, NCH], F32, "Mb"); Ab = ph.sb([128, M2, NCH], F32, "Ab")
            with ExitStack() as es2:
                def sb2(shape, dt=F32, nm="g"):
                    return T(es2.enter_context(nc.sbuf_tensor(kb.name(nm), list(shape), dt)))
                def ps2(shape, dt=F32, nm="gp"):
                    return TP(es2.enter_context(nc.psum_tensor(kb.name(nm), [128, 512], F32)), list(shape), dt)
                gt = [sb2([128, NGC], F32, "gt") for _ in range(2)]
                spl = [sb2([128, M2], F32, "spl") for _ in range(2)]
                uu = [sb2([128, M2], F32, "uu") for _ in range(2)]
                pc = [ps2([128, M2], F32, "pc") for _ in range(2)]
                pu = [ps2([MH, 2, 128], F32, "pu") for _ in range(2)]
                pb = [ps2([MH, 2], F32, "pb") for _ in range(2)]
                for t in range(NCH):
                    g_ = gt[t % 2]; s_ = spl[t % 2]; u_ = uu[t % 2]; pc_ = pc[t % 2]; pu_ = pu[t % 2]; pb_ = pb[t % 2]
                    kb.dma('sp', g_[:], mg_d[t * 128:(t + 1) * 128, :], W=[g_])
                    kb.op('act', lambda a: a.activation(out=s_[:], in_=g_[:, M2:2 * M2], func=AF.Exp, scale=-1.0), R=[g_], W=[s_])
                    kb.op('act', lambda a: a.activation(out=s_[:], in_=s_[:], func=AF.Ln, bias=1.0, scale=1.0), R=[s_], W=[s_])
                    kb.wait('pe', R=[s_, tri_f, tri_b], W=[pc_])
                    nc.tensor.matmul(pc_[:, 0:MH], lhsT=tri_f[:], rhs=s_[:, 0:MH], start=True, stop=True)
                    ins = nc.tensor.matmul(pc_[:, MH:M2], lhsT=tri_b[:], rhs=s_[:, MH:M2], start=True, stop=True)
                    kb.done('pe', ins, R=[s_, tri_f, tri_b], W=[pc_])
                    kb.op('dve', lambda v: v.tensor_copy(out=CS[:, t, :], in_=pc_[:]), R=[pc_], W=[CS])
                    kb.op('dve', lambda v: v.tensor_copy(out=IA[:, t, :], in_=g_[:, 0:M2]), R=[g_], W=[IA])
                    kb.op('dve', lambda v: v.tensor_tensor(out=u_[:], in0=g_[:, 0:M2], in1=pc_[:], op=ALU.add), R=[g_, pc_], W=[u_])
                    kb.wait('pe', R=[u_, s_, cst_f, ones_f], W=[pu_, pb_])
                    for d in range(2):
                        nc.tensor.matmul(pu_[:, d, :], lhsT=u_[:, d * MH:(d + 1) * MH], rhs=ident_f[:, 0:128], start=True, stop=True)
                    for d in range(2):
                        ins = nc.tensor.matmul(pb_[:, d:d + 1], lhsT=s_[:, d * MH:(d + 1) * MH], rhs=ones_f[:, 0:1], start=True, stop=True)
                    kb.done('pe', ins, R=[u_, s_, cst_f, ones_f], W=[pu_, pb_])
                    for d in range(2):
                        kb.op('dve', lambda v: v.tensor_reduce(out=Um[d][:, t:t + 1], in_=pu_[:, d, :], axis=AX.X, op=ALU.max), R=[pu_], W=[Um[d]])
                        kb.op('dve', lambda v: v.tensor_scalar(out=Be[d][:, t:t + 1], in0=pb_[:, d:d + 1], scalar1=-1.0, scalar2=None, op0=ALU.mult), R=[pb_], W=[Be[d]])
                for d, order in enumerate([order_f, order_b]):
                    kb.op('dve', lambda v: v.memset(Mp[d][:], 0.0), W=[Mp[d]])
                    for k, c in enumerate(order):
                        kb.op('dve', lambda v: v.tensor_tensor(out=Mm[d][:, c:c + 1], in0=Mp[d][:, k:k + 1], in1=Um[d][:, c:c + 1], op=ALU.max), R=[Mp[d], Um[d]], W=[Mm[d]])
                        kb.op('dve', lambda v: v.tensor_tensor(out=Aa[d][:, c:c + 1], in0=Mp[d][:, k:k + 1], in1=Mm[d][:, c:c + 1], op=ALU.subtract), R=[Mp[d], Mm[d]], W=[Aa[d]])
                        kb.op('dve', lambda v: v.tensor_tensor(out=Mp[d][:, k + 1:k + 2], in0=Be[d][:, c:c + 1], in1=Mm[d][:, c:c + 1], op=ALU.add), R=[Be[d], Mm[d]], W=[Mp[d]])
                    kb.op('act', lambda a: a.activation(out=Aa[d][:], in_=Aa[d][:], func=AF.Exp), R=[Aa[d]], W=[Aa[d]])
                X = sb2([MH, MH, NCH], F32, "X")
                pbc = ps2([128, 512], F32, "pbc")
                HPB = max(1, 512 // NCH)
                for d in range(2):
                    for (src, dstb) in [(Mm[d], Mb), (Aa[d], Ab)]:
                        kb.op('dve', lambda v: v.tensor_tensor(out=X[:], in0=ident_f[0:MH, 0:MH].unsqueeze(2).to_broadcast([MH, MH, NCH]), in1=src[:].unsqueeze(1).to_broadcast([MH, MH, NCH]), op=ALU.mult), R=[cst_f, src], W=[X])
                        for r0 in range(0, MH, HPB):
                            nr = min(HPB, MH - r0)
                            kb.wait('pe', R=[X, ones_f], W=[pbc])
                            ins = nc.tensor.matmul(pbc[:, 0:nr * NCH], lhsT=ones_f[0:MH, :], rhs=X[:, r0:r0 + nr, :].rearrange("q r c -> q (r c)"), start=True, stop=True)
                            kb.done('pe', ins, R=[X, ones_f], W=[pbc])
                            kb.op('dve', lambda v: v.tensor_copy(out=dstb[:, d * MH + r0:d * MH + r0 + nr, :].rearrange("p r c -> p (r c)"), in_=pbc[:, 0:nr * NCH]), R=[pbc], W=[dstb])
                kb.op('dve', lambda v: v.tensor_tensor(out=EA[:], in0=CS[:], in1=Mb[:].rearrange("p r c -> p c r"), op=ALU.subtract), R=[CS, Mb], W=[EA])
                kb.op('act', lambda a: a.activation(out=EA[:], in_=EA[:], func=AF.Exp), R=[EA], W=[EA])
                kb.op('act', lambda a: a.activation(out=WA[:], in_=IA[:], func=AF.Exp), R=[IA], W=[WA])
                kb.op('dve', lambda v: v.tensor_tensor(out=WA[:], in0=WA[:], in1=EA[:], op=ALU.mult), R=[WA, EA], W=[WA])
                kb.barrier()
            qT = [ph.sb([128, MH, 128], BF16, "mq") for _ in range(2)]
            kT = [ph.sb([128, MH, 128], BF16, "mk") for _ in range(2)]
            va = [ph.sb([128, MH, DV + 1], BF16, "va") for _ in range(2)]
            for v_ in va:
                kb.op('dve', lambda v: v.memset(v_[:], 1.0), W=[v_])
            Cn = [ph.sb([128, DV + 1], F32, "Cn") for _ in range(MH)]
            Cb = [ph.sb([128, DV + 1], BF16, "Cb") for _ in range(MH)]
            pk = [ph.ps([128, 128], BF16, "pk") for _ in range(2)]
            pst = [ph.ps([128, 128], F32, "pst") for _ in range(2)]
            pnum = [ph.ps([128, DV + 1], F32, "pnum") for _ in range(2)]
            pup = [ph.ps([128, DV + 1], F32, "pup") for _ in range(2)]
            ksc = [ph.sb([128, 128], BF16, "ksc") for _ in range(2)]
            Sp = [ph.sb([128, 128], BF16, "Sp") for _ in range(2)]
            dd = [ph.sb([128, 2], F32, "dd") for _ in range(2)]
            Ho = [ph.sb([128, MH, DV], BF16, "Ho") for _ in range(2)]
            it = 0
            for d, order in enumerate([order_f, order_b]):
                tri = tri_fb if d == 0 else tri_bb
                hdst = hf_d if d == 0 else hb_d
                for h in range(MH):
                    kb.op('dve', lambda v: v.memset(Cn[h][:], 0.0), W=[Cn[h]])
                for k, c in enumerate(order):
                    q_ = qT[k % 2]; k_ = kT[k % 2]; v_ = va[k % 2]; H_ = Ho[k % 2]
                    kb.dma('sp', q_[:], mqkc_d[0:QKW, c * 128:(c + 1) * 128].rearrange("(h d) t -> d h t", d=DK), W=[q_])
                    kb.dma('sp', k_[:], mqkc_d[QKW:2 * QKW, c * 128:(c + 1) * 128].rearrange("(h d) t -> d h t", d=DK), W=[k_])
                    kb.dma('sp', v_[:, :, 0:DV], mv_d[c * 128:(c + 1) * 128, :].rearrange("p (h v) -> p h v", v=DV), W=[v_])
                    skip_out = last and c < CT
                    for h in range(MH):
                        r = d * MH + h
                        i2 = it % 2; it += 1
                        kb.op('dve', lambda v: v.tensor_scalar(out=Cn[h][:], in0=Cn[h][:], scalar1=Ab[:, r, c:c + 1], scalar2=None, op0=ALU.mult), R=[Cn[h], Ab], W=[Cn[h]])
                        kb.op('pool', lambda v: v.tensor_copy(out=Cb[h][:], in_=Cn[h][:]), R=[Cn[h]], W=[Cb[h]])
                        kb.wait('pe', R=[k_, ident_b], W=[pk[i2]])
                        ins = nc.tensor.transpose(out=pk[i2][:], in_=k_[:, h, :], identity=ident_b[:])
                        kb.done('pe', ins, R=[k_, ident_b], W=[pk[i2]])
                        kb.op('dve', lambda v: v.tensor_scalar(out=ksc[i2][:], in0=pk[i2][:], scalar1=WA[:, c, r:r + 1], scalar2=None, op0=ALU.mult), R=[pk[i2], WA], W=[ksc[i2]])
                        if not skip_out:
                            kb.wait('pe', R=[k_, q_], W=[pst[i2]])
                            ins = nc.tensor.matmul(pst[i2][:], lhsT=k_[:, h, :], rhs=q_[:, h, :], start=True, stop=True)
                            kb.done('pe', ins, R=[k_, q_], W=[pst[i2]])
                            kb.op('dve', lambda v: v.scalar_tensor_tensor(out=Sp[i2][:], in0=pst[i2][:], scalar=WA[:, c, r:r + 1], in1=tri[:], op0=ALU.mult, op1=ALU.mult), R=[pst[i2], WA, tri], W=[Sp[i2]])
                            kb.wait('pe', R=[Sp[i2], v_, q_, Cb[h]], W=[pnum[i2]])
                            nc.tensor.matmul(pnum[i2][:], lhsT=Sp[i2][:], rhs=v_[:, h, :], start=True, stop=False)
                            ins = nc.tensor.matmul(pnum[i2][:], lhsT=q_[:, h, :], rhs=Cb[h][:], start=False, stop=True)
                            kb.done('pe', ins, R=[Sp[i2], v_, q_, Cb[h]], W=[pnum[i2]])
                            kb.op('dve', lambda v: v.tensor_scalar(out=dd[i2][:, 0:1], in0=pnum[i2][:, DV:DV + 1], scalar1=-1.0, scalar2=None, op0=ALU.mult), R=[pnum[i2]], W=[dd[i2]])
                            kb.op('dve', lambda v: v.tensor_tensor(out=dd[i2][:, 0:1], in0=dd[i2][:, 0:1], in1=pnum[i2][:, DV:DV + 1], op=ALU.max), R=[pnum[i2], dd[i2]], W=[dd[i2]])
                            kb.op('dve', lambda v: v.tensor_tensor(out=dd[i2][:, 0:1], in0=dd[i2][:, 0:1], in1=EA[:, c, r:r + 1], op=ALU.max), R=[dd[i2], EA], W=[dd[i2]])
                            kb.op('dve', lambda v: v.reciprocal(out=dd[i2][:, 1:2], in_=dd[i2][:, 0:1]), R=[dd[i2]], W=[dd[i2]])
                            kb.op('act', lambda a: a.activation(out=H_[:, h, :], in_=pnum[i2][:, 0:DV], func=AF.Copy, scale=dd[i2][:, 1:2]), R=[pnum[i2], dd[i2]], W=[H_])
                        kb.wait('pe', R=[ksc[i2], v_], W=[pup[i2]])
                        ins = nc.tensor.matmul(pup[i2][:], lhsT=ksc[i2][:], rhs=v_[:, h, :], start=True, stop=True)
                        kb.done('pe', ins, R=[ksc[i2], v_], W=[pup[i2]])
                        kb.op('dve', lambda v: v.tensor_tensor(out=Cn[h][:], in0=Cn[h][:], in1=pup[i2][:], op=ALU.add), R=[Cn[h], pup[i2]], W=[Cn[h]])
                    if not skip_out:
                        kb.dma('pool', hdst[c * 128:(c + 1) * 128, :].rearrange("p (h v) -> p h v", v=DV), H_[:], R=[H_])

    def hm_phase(l):
        last = (l == DEPTH - 1)
        MC = MW // 128
        with Ph() as ph:
            gm = ph.sb([128, MW], F32, "gmh")
            load_rep(gm, g_mh[l:l + 1, :], MW)
            hf = [ph.sb([128, MW], BF16, "hf") for _ in range(2)]; hb = [ph.sb([128, MW], BF16, "hb") for _ in range(2)]
            mo = [ph.sb([128, MW], BF16, "mo") for _ in range(2)]
            sg = ph.sb([128, MW], F32, "sg"); hs = ph.sb([128, MW], F32, "hs"); sq = ph.sb([128, MW], F32, "sq")
            ho = [ph.sb([128, MW], BF16, "ho") for _ in range(2)]
            st = [ph.sb([128, 4, MH], F32, "st") for _ in range(2)]
            ptr = [ph.ps([128, 8, 128], BF16, "ptr") for _ in range(2)]
            oT = [ph.sb([128, MC, 128], BF16, "oT") for _ in range(2)]
            for t in range(CT if last else 0, TT):
                f_ = hf[t % 2]; b_ = hb[t % 2]; m_ = mo[t % 2]; o_ = ho[t % 2]; s_ = st[t % 2]; oT_ = oT[t % 2]
                kb.dma('sp', f_[:], hf_d[t * 128:(t + 1) * 128, :], W=[f_])
                kb.dma('sp', b_[:], hb_d[t * 128:(t + 1) * 128, :], W=[b_])
                kb.dma('sp', m_[:], mo_d[t * 128:(t + 1) * 128, :], W=[m_])
                kb.op('act', lambda a: a.activation(out=sg[:], in_=m_[:], func=AF.Sigmoid), R=[m_], W=[sg])
                kb.op('pool', lambda v: v.tensor_tensor(out=hs[:], in0=f_[:], in1=b_[:], op=ALU.add), R=[f_, b_], W=[hs])
                kb.op('dve', lambda v: v.tensor_tensor(out=hs[:], in0=hs[:], in1=sg[:], op=ALU.mult), R=[hs, sg], W=[hs])
                kb.op('pool', lambda v: v.tensor_tensor(out=sq[:], in0=hs[:], in1=hs[:], op=ALU.mult), R=[hs], W=[sq])
                kb.op('dve', lambda v: v.tensor_reduce(out=s_[:, 0, :], in_=sq[:].rearrange("p (h v) -> p h v", v=DV), axis=AX.X, op=ALU.add), R=[sq], W=[s_])
                kb.op('dve', lambda v: v.tensor_scalar(out=s_[:, 1, :], in0=s_[:, 0, :], scalar1=1.0 / DV, scalar2=EPS, op0=ALU.mult, op1=ALU.add), R=[s_], W=[s_])
                kb.op('act', lambda a: a.activation(out=s_[:, 2, :], in_=s_[:, 1, :], func=AF.Sqrt), R=[s_], W=[s_])
                kb.op('dve', lambda v: v.reciprocal(out=s_[:, 3, :], in_=s_[:, 2, :]), R=[s_], W=[s_])
                kb.op('dve', lambda v: v.tensor_tensor(out=hs[:].rearrange("p (h v) -> p h v", v=DV), in0=hs[:].rearrange("p (h v) -> p h v", v=DV), in1=s_[:, 3, :].unsqueeze(2).to_broadcast([128, MH, DV]), op=ALU.mult), R=[hs, s_], W=[hs])
                kb.op('dve', lambda v: v.tensor_tensor(out=o_[:], in0=hs[:], in1=gm[:], op=ALU.mult), R=[hs, gm], W=[o_])
                for k0 in range(0, MC, 8):
                    p_ = ptr[(k0 // 8) % 2]
                    n = min(MC, k0 + 8) - k0
                    kb.wait('pe', R=[o_, ident_b], W=[p_])
                    for k in range(k0, k0 + n):
                        ins = nc.tensor.transpose(out=p_[:, k - k0, :], in_=o_[:, k * 128:(k + 1) * 128], identity=ident_b[:])
                    kb.done('pe', ins, R=[o_, ident_b], W=[p_])
                    kb.op('act', lambda a: a.copy(out=oT_[:, k0:k0 + n, :], in_=p_[:, 0:n, :]), R=[p_], W=[oT_])
                cv_tick(1)
                kb.dma('pool', hmT_d[:, :, t * 128:(t + 1) * 128], oT_[:], R=[oT_])

    def merge_phase(l):
        last = (l == DEPTH - 1)
        NTB = 512
        AC = AW // 128; MC = MW // 128
        with Ph() as ph:
            gts = [ph.sb([128, 512], F32, "gts") for _ in range(3)]
            aT = ph.sb([128, AC, NTB], BF16, "aT"); mT = ph.sb([128, MC, NTB], BF16, "mT")
            wa = [ph.sb([128, AC, 128], BF16, "wa") for _ in range(2)]; wm = [ph.sb([128, MC, 128], BF16, "wm") for _ in range(2)]
            ga = [ph.sb([128, NTB], BF16, "ga") for _ in range(2)]; gmm = [ph.sb([128, NTB], BF16, "gmm") for _ in range(2)]
            pa = [ph.ps([128, NTB], F32, "pa") for _ in range(2)]; pm = [ph.ps([128, NTB], F32, "pm") for _ in range(2)]
            t1 = [ph.sb([128, NTB], F32, "t1") for _ in range(2)]
            mg = ph.sb([128, KC, NTB], BF16, "mg")
            wo = [ph.sb([128, KC, 512], BF16, "wo") for _ in range(2)]
            po = [ph.ps([128, 512], F32, "po") for _ in range(4)]
            xt = [ph.sb([128, 512], F32, "xt") for _ in range(3)]
            wi = 0; xi = 0; pi = 0
            tbs = list(range(0, Tn, NTB))
            for tb0 in tbs:
                nt = min(NTB, Tn - tb0)
                kb.dma('sp', aT[:, :, 0:nt], atT_d[:, :, tb0:tb0 + nt], W=[aT])
                kb.dma('sp', mT[:, :, 0:nt], hmT_d[:, :, tb0:tb0 + nt], W=[mT])
                for j in range(KC):
                    a_ = wa[j % 2]; m_ = wm[j % 2]; ga_ = ga[j % 2]; gm_ = gmm[j % 2]; pa_ = pa[j % 2]; pm_ = pm[j % 2]; t_ = t1[j % 2]
                    kb.dma('sp', a_[:], WS[l % NWS]['wba_b'][:, j, :, :], W=[a_])
                    kb.dma('sp', m_[:], WS[l % NWS]['wbm_b'][:, j, :, :], W=[m_])
                    kb.dma('sp', ga_[:, 0:nt], sga_d[j * 128:(j + 1) * 128, tb0:tb0 + nt], W=[ga_])
                    kb.dma('sp', gm_[:, 0:nt], sgm_d[j * 128:(j + 1) * 128, tb0:tb0 + nt], W=[gm_])
                    kb.wait('pe', R=[a_, aT], W=[pa_])
                    for k in range(AC):
                        ins = nc.tensor.matmul(pa_[:, 0:nt], lhsT=a_[:, k, :], rhs=aT[:, k, 0:nt], start=(k == 0), stop=(k == AC - 1))
                    kb.done('pe', ins, R=[a_, aT], W=[pa_])
                    kb.wait('pe', R=[m_, mT], W=[pm_])
                    for k in range(MC):
                        ins = nc.tensor.matmul(pm_[:, 0:nt], lhsT=m_[:, k, :], rhs=mT[:, k, 0:nt], start=(k == 0), stop=(k == MC - 1))
                    kb.done('pe', ins, R=[m_, mT], W=[pm_])
                    kb.op('dve', lambda v: v.tensor_tensor(out=t_[:, 0:nt], in0=pa_[:, 0:nt], in1=ga_[:, 0:nt], op=ALU.mult), R=[pa_, ga_], W=[t_])
                    kb.op('dve', lambda v: v.tensor_tensor(out=mg[:, j, 0:nt], in0=pm_[:, 0:nt], in1=gm_[:, 0:nt], op=ALU.mult), R=[pm_, gm_], W=[mg])
                    kb.op('pool', lambda v: v.tensor_tensor(out=mg[:, j, 0:nt], in0=mg[:, j, 0:nt], in1=t_[:, 0:nt], op=ALU.add), R=[mg, t_], W=[mg])
                for n0 in range(0, D, 512):
                    w_ = wo[wi % 2]; wi += 1
                    kb.dma('sp', w_[:], WS[l % NWS]['wo_b'][:, n0 // 512, :, :], W=[w_])
                    for tt in range(nt // 128):
                        tg = (tb0 // 128) + tt
                        if last and tg < CT:
                            continue
                        r = 1 if tg < CT else 0
                        p_ = po[pi % 4]; pi += 1
                        x_ = xt[xi % 3]; xi += 1
                        kb.dma('sp', x_[:], xs[tg * 128:(tg + 1) * 128, n0:n0 + 512], W=[x_])
                        g_ = gts[xi % 3]
                        kb.dma('sp', g_[:], mod_d[l, r:r + 1, 2 * D + n0:2 * D + n0 + 512].to_broadcast([128, 512]), W=[g_])
                        kb.wait('pe', R=[mg, w_], W=[p_])
                        for k in range(KC):
                            ins = nc.tensor.matmul(p_[:], lhsT=mg[:, k, tt * 128:(tt + 1) * 128], rhs=w_[:, k, :], start=(k == 0), stop=(k == KC - 1))
                        kb.done('pe', ins, R=[mg, w_], W=[p_])
                        t_ = t1[pi % 2]
                        kb.op('dve', lambda v: v.tensor_tensor(out=t_[:, 0:512], in0=p_[:], in1=g_[:], op=ALU.mult), R=[p_, g_], W=[t_])
                        kb.op('pool', lambda v: v.tensor_tensor(out=x_[:], in0=x_[:], in1=t_[:, 0:512], op=ALU.add), R=[x_, t_], W=[x_])
                        kb.dma('pool', xs[tg * 128:(tg + 1) * 128, n0:n0 + 512], x_[:], R=[x_])

    def moe_phase(l):
        last = (l == DEPTH - 1)
        NTB = 512
        FC = FF // 128
        with Ph() as ph:
            gts = [ph.sb([128, 512], F32, "gts") for _ in range(2)]
            hTb = ph.sb([128, KC, NTB], BF16, "hTb")
            yacc = ph.sb([128, NTB // 128, D], F32, "yacc")
            wg = [ph.sb([128, KC, 128], BF16, "wg") for _ in range(2)]; wu = [ph.sb([128, KC, 128], BF16, "wu") for _ in range(2)]
            wd = [ph.sb([128, FC, 512], BF16, "wd") for _ in range(2)]
            pg = [ph.ps([128, NTB], F32, "pg") for _ in range(2)]; pu = [ph.ps([128, NTB], F32, "pu") for _ in range(2)]
            pdn = [ph.ps([128, 512], F32, "pdn") for _ in range(4)]
            sg = [ph.sb([128, NTB], F32, "sg") for _ in range(2)]
            aT = [ph.sb([128, FC, NTB], BF16, "aT") for _ in range(2)]
            xt = [ph.sb([128, 512], F32, "xt") for _ in range(2)]
            gi = 0; di = 0; xi = 0
            t_start = CTX if last else 0
            for tb0 in range(t_start, Tn, NTB):
                nt = min(NTB, Tn - tb0)
                ntl = nt // 128
                kb.dma('sp', hTb[:, :, 0:nt], hT_d[:, :, tb0:tb0 + nt], W=[hTb])
                kb.op('pool', lambda v: v.memset(yacc[:], 0.0), W=[yacc])
                for e in range(NE):
                    a_ = aT[e % 2]
                    for j in range(FC):
                        g_ = wg[gi % 2]; u_ = wu[gi % 2]; pg_ = pg[gi % 2]; pu_ = pu[gi % 2]; s_ = sg[gi % 2]; gi += 1
                        kb.dma('sp', g_[:], WS[l % NWS]['wg_b'][e, :, j, :, :], W=[g_])
                        cv_tick()
                        kb.dma('sp', u_[:], WS[l % NWS]['wu_b'][e, :, j, :, :], W=[u_])
                        kb.wait('pe', R=[g_, hTb], W=[pg_])
                        for k in range(KC):
                            ins = nc.tensor.matmul(pg_[:, 0:nt], lhsT=g_[:, k, :], rhs=hTb[:, k, 0:nt], start=(k == 0), stop=(k == KC - 1))
                        kb.done('pe', ins, R=[g_, hTb], W=[pg_])
                        kb.wait('pe', R=[u_, hTb], W=[pu_])
                        for k in range(KC):
                            ins = nc.tensor.matmul(pu_[:, 0:nt], lhsT=u_[:, k, :], rhs=hTb[:, k, 0:nt], start=(k == 0), stop=(k == KC - 1))
                        kb.done('pe', ins, R=[u_, hTb], W=[pu_])
                        kb.op('act', lambda a: a.activation(out=s_[:, 0:nt], in_=pg_[:, 0:nt], func=AF.Sigmoid), R=[pg_], W=[s_])
                        kb.op('dve', lambda v: v.tensor_tensor(out=s_[:, 0:nt], in0=s_[:, 0:nt], in1=pg_[:, 0:nt], op=ALU.mult), R=[s_, pg_], W=[s_])
                        kb.op('dve', lambda v: v.tensor_tensor(out=a_[:, j, 0:nt], in0=s_[:, 0:nt], in1=pu_[:, 0:nt], op=ALU.mult), R=[s_, pu_], W=[a_])
                    for n0 in range(0, D, 512):
                        d_ = wd[(di // max(1, ntl)) % 2]
                        kb.dma('sp', d_[:], WS[l % NWS]['wd_b'][e, :, n0 // 512, :, :], W=[d_])
                        cv_tick()
                        for tt in range(ntl):
                            tg = tb0 // 128 + tt
                            p_ = pdn[di % 4]; di += 1
                            kb.wait('pe', R=[a_, d_], W=[p_])
                            for k in range(FC):
                                ins = nc.tensor.matmul(p_[:], lhsT=a_[:, k, tt * 128:(tt + 1) * 128], rhs=d_[:, k, :], start=(k == 0), stop=(k == FC - 1))
                            kb.done('pe', ins, R=[a_, d_], W=[p_])
                            kb.op('dve', lambda v: v.scalar_tensor_tensor(out=yacc[:, tt, n0:n0 + 512], in0=p_[:], scalar=comb[:, tg, e:e + 1], in1=yacc[:, tt, n0:n0 + 512], op0=ALU.mult, op1=ALU.add), R=[p_, comb, yacc], W=[yacc])
                for tt in range(ntl):
                    tg = tb0 // 128 + tt
                    r = 1 if tg < CT else 0
                    for n0 in range(0, D, 512):
                        x_ = xt[xi % 2]; g_ = gts[xi % 2]; xi += 1
                        kb.dma('sp', x_[:], xs[tg * 128:(tg + 1) * 128, n0:n0 + 512], W=[x_])
                        kb.dma('sp', g_[:], mod_d[l, r:r + 1, 5 * D + n0:5 * D + n0 + 512].to_broadcast([128, 512]), W=[g_])
                        kb.op('dve', lambda v: v.tensor_tensor(out=yacc[:, tt, n0:n0 + 512], in0=yacc[:, tt, n0:n0 + 512], in1=g_[:], op=ALU.mult), R=[yacc, g_], W=[yacc])
                        kb.op('pool', lambda v: v.tensor_tensor(out=x_[:], in0=x_[:], in1=yacc[:, tt, n0:n0 + 512], op=ALU.add), R=[x_, yacc], W=[x_])
                        if last:
                            kb.dma('pool', y_out[(tg - CT) * 128:(tg - CT + 1) * 128, n0:n0 + 512], x_[:], R=[x_])
                        else:
                            kb.dma('pool', xs[tg * 128:(tg + 1) * 128, n0:n0 + 512], x_[:], R=[x_])

    import os
    stop = int(os.environ.get("KSTOP", "999"))
    phs = []
    OVL = os.environ.get("KOVL", "1") == "1"

    def convert_layer(l):
        if l == 0:
            pending.append(convert_gen(0, None, "A" if OVL else "AB"))
            cv_flush()
            if OVL:
                pending.append(convert_gen(0, 'pool', "B"))
        else:
            if not OVL:
                pending.append(convert_gen(l, None, "AB"))
            cv_flush()

    def pre_merge(l):
        if OVL:
            while pending and not ready.get((l, 'm')):
                cv_tick()
            kb.barrier()

    def pre_moe(l):
        if OVL:
            cv_flush()
            if l + 1 < DEPTH:
                pending.append(convert_gen(l + 1, 'pool', "AB"))

    for l in range(DEPTH):
        phs += [lambda l=l: convert_layer(l), lambda l=l: adaln(l), lambda l=l: norm_phase(l, 0), lambda l=l: proj_phase(l),
                lambda l=l: conv_phase(l), lambda l=l: qk_phase(l), lambda l=l: attn_phase(l), lambda l=l: mlstm_phase(l),
                lambda l=l: hm_phase(l), lambda l=l: (pre_merge(l), merge_phase(l)), lambda l=l: norm_phase(l, 1), lambda l=l: (pre_moe(l), moe_phase(l))]
    for i, p in enumerate(phs):
        if i < stop:
            p()
    kb.barrier()
    dbg = [d for d in os.environ.get("KDBG", "").split(",") if d]
    scr = dict(xs=xs, hT_d=hT_d, mod_d=mod_d, aq_d=aq_d, ak_d=ak_d, av_d=av_d, mv_d=mv_d, mo_d=mo_d, mg_d=mg_d, mqk_d=mqk_d,
               mqkc_d=mqkc_d, sga_d=sga_d, sgm_d=sgm_d, qT_d=qT_d, kT_d=kT_d, atT_d=atT_d, hf_d=hf_d, hb_d=hb_d, hmT_d=hmT_d)
    for d in dbg:
        src = scr[d]
        o = nc.dram_tensor("dbg_" + d, list(src.shape), src.dtype, kind="ExternalOutput").ap()
        if len(src.shape) == 3:
            for i in range(src.shape[0]):
                kb.dma('sp', o[i], src[i])
        else:
            kb.dma('sp', o[:, :], src[:, :])
    if "comb" in os.environ.get("KDBG2", ""):
        o = nc.dram_tensor("dbg_comb", [128, TT * NE], F32, kind="ExternalOutput").ap()
        kb.dma('sp', o[:, :], comb[:].rearrange("p t e -> p (t e)"), R=[comb])
    kb.barrier()
    pes.__exit__(None, None, None)
    return nc


def host_consts(cfg):
    NLAT = cfg['NLAT']; GW = cfg['GRID_W']
    rp = 32
    inv = (10000.0 ** (-np.arange(rp, dtype=np.float32) / rp)).astype(np.float32)
    t = np.arange(NLAT)
    ar = (t // GW).astype(np.float32)[:, None] * inv
    ac = (t % GW).astype(np.float32)[:, None] * inv
    rope = np.concatenate([np.cos(ar), np.cos(ac), np.sin(ar), np.sin(ac)], axis=1).astype(np.float32)
    ident = np.eye(128, dtype=np.float32)
    i = np.arange(128)[None, :]; j = np.arange(128)[:, None]
    m = []
    for c in (-1, 0, 1):
        rel = (c * 128 + j) - i
        m.append(np.where(np.abs(rel) <= 128, 0.0, NEG).astype(np.float32))
    trif = (j <= i).astype(np.float32)
    cst = np.concatenate([ident] + m + [trif], axis=1).astype(np.float32)
    return rope, cst


def make_in_maps(cfg, inp):
    D = cfg['D']; MH = cfg['MH']; NH = cfg['NH']; NKV = cfg['NKV']
    AW = NH * 128; KW = NKV * 128; QKW = MH * cfg['DK']; MW = MH * cfg['DV']
    o_mg = AW + 2 * KW + 2 * QKW + 2 * MW
    perm = np.arange(inp['w_in'].shape[-1])
    g = np.arange(4 * MH).reshape(4, MH)
    perm[o_mg:o_mg + 4 * MH] = o_mg + np.concatenate([g[0], g[2], g[1], g[3]])
    w_in = np.ascontiguousarray(inp['w_in'][:, :, perm]); b_in = np.ascontiguousarray(inp['b_in'][:, perm])
    rope, cst = host_consts(cfg)
    f = lambda a: np.ascontiguousarray(np.asarray(a, dtype=np.float32))
    maps = []
    for b in range(inp['x'].shape[0]):
        maps.append({
            'x': f(inp['x'][b]), 'ctx': f(inp['ctx'][b]), 'cc': f(np.stack([inp['c'][b], inp['c_ctx']], 0)),
            'w_ada': f(inp['w_ada']), 'b_ada': f(inp['b_ada']), 'g_mix': f(inp['g_mix']), 'g_ffn': f(inp['g_ffn']),
            'w_in': f(w_in), 'b_in': f(b_in), 'g_q': f(inp['g_q']), 'g_k': f(inp['g_k']), 'sink': f(inp['sink']),
            'conv_w': f(inp['conv_w']), 'conv_b': f(inp['conv_b']), 'g_mh': f(inp['g_mh']),
            'w_br_attn': f(inp['w_br_attn']), 'w_br_mlstm': f(inp['w_br_mlstm']), 'w_out': f(inp['w_out']),
            'w_router': f(inp['w_router']), 'b_router': f(np.asarray(inp['b_router']).reshape(1, -1)),
            'w_gate': f(inp['w_gate']), 'w_up': f(inp['w_up']), 'w_down': f(inp['w_down']),
            'rope_cs': rope, 'cst': cst,
        })
    return maps


def run(cfg, inp, trace=False):
    nc = build(cfg)
    maps = make_in_maps(cfg, inp)
    res = run_bass_kernel_spmd(nc, maps, core_ids=list(range(len(maps))), trace=trace)
    out = np.stack([res.results[b]['y'] for b in range(len(maps))], 0).astype(np.float32)
    return out, res


def kernel(**inputs):
    inp = {k: np.asarray(v) for k, v in inputs.items()}
    out, _ = run(CFG_FULL, inp)
    return out
```
